# Optimizing a Trainium2 kernel written in Bass

```python
import jax
import jax.numpy as jnp
from jax import lax
import numpy as np

D_MODEL = 1024
BATCH = 8
SEQ = 8192
DEPTH = 4

GRID_W = 64
CTX_LEN = 256
EPS = 1e-6

NA_HEAD_DIM = 64
NA_HEADS = D_MODEL // (2 * NA_HEAD_DIM)
NA_WIN_ROWS = 8
NA_WIN_COLS = 16
NA_W = NA_HEADS * NA_HEAD_DIM

GDN_HEAD_DIM = 128
GDN_HEADS = D_MODEL // (2 * GDN_HEAD_DIM)
GDN_W = GDN_HEADS * GDN_HEAD_DIM
GDN_CONV = 5
GDN_CHUNK = 64

EVEN_IN = 3 * NA_W + 4 * GDN_W + 4 * GDN_HEADS
EVEN_MIX = NA_W + GDN_W

MLA_V = 128
MLA_HEADS = D_MODEL // MLA_V
MLA_NOPE = 128
MLA_ROPE = 64
MLA_Q_RANK = 384
MLA_KV_RANK = 256
MLA_IN = MLA_Q_RANK + MLA_KV_RANK + MLA_ROPE
MLA_QBLOCK = 128
ROPE_THETA = 10000.0

PEER_KEYS = 128
PEER_EXPERTS = PEER_KEYS * PEER_KEYS
PEER_HEADS = 8
PEER_TOPK = 16
PEER_DKEY = 256
PEER_BLOCK = 128

N_EVEN = (DEPTH + 1) // 2
N_ODD = DEPTH // 2

kernel_name = 'hybrid_na_gdn_mla_peer_diffusion_trunk'


def rmsnorm(x, g):
    x32 = x.astype(jnp.float32)
    y = x32 * lax.rsqrt(jnp.mean(x32 * x32, axis=-1, keepdims=True) + EPS)
    return (y * g.astype(jnp.float32)).astype(x.dtype)


def l2norm(x):
    x32 = x.astype(jnp.float32)
    return x32 * lax.rsqrt(jnp.sum(x32 * x32, axis=-1, keepdims=True) + EPS)


def modulate(h, shift, scale):
    return h * (1.0 + scale[:, None, :]) + shift[:, None, :]


def split_heads(t, n_heads):
    b, l, w = t.shape
    return t.reshape(b, l, n_heads, w // n_heads).transpose(0, 2, 1, 3)


def merge_heads(t):
    b, h, l, d = t.shape
    return t.transpose(0, 2, 1, 3).reshape(b, l, h * d)


def axial_rope_tables(n_tokens, dim):
    half = dim // 2
    inv_freq = ROPE_THETA ** (-jnp.arange(0, half, 2, dtype=jnp.float32) / half)
    t = jnp.arange(n_tokens, dtype=jnp.int32)
    row = (t // GRID_W).astype(jnp.float32)
    col = (t % GRID_W).astype(jnp.float32)
    ang_r = row[:, None] * inv_freq[None, :]
    ang_c = col[:, None] * inv_freq[None, :]
    ang = jnp.concatenate([ang_r, ang_r, ang_c, ang_c], axis=-1)
    return jnp.cos(ang), jnp.sin(ang)


def apply_axial_rope(x, cos, sin):
    a, b, cq, d = jnp.split(x, 4, axis=-1)
    rot = jnp.concatenate([-b, a, -d, cq], axis=-1)
    return x * cos.astype(x.dtype) + rot * sin.astype(x.dtype)


def softmax_attend(q, k, v, scale):
    s = jnp.einsum('bhqd,bhkd->bhqk', q, k).astype(jnp.float32) * scale
    p = jax.nn.softmax(s, axis=-1).astype(v.dtype)
    return jnp.einsum('bhqk,bhkd->bhqd', p, v)


def blocked_attention(q, k, v, scale):
    b, h, l, dq = q.shape
    qb = jnp.moveaxis(q.reshape(b, h, l // MLA_QBLOCK, MLA_QBLOCK, dq), 2, 0)
    o = lax.map(lambda qi: softmax_attend(qi, k, v, scale), qb)
    return jnp.moveaxis(o, 0, 2).reshape(b, h, l, v.shape[-1])


def neighbourhood_attention(q, k, v, k_ctx, v_ctx, rpb):
    b, h, l, dh = q.shape
    rows = l // GRID_W
    wr = min(NA_WIN_ROWS, rows)
    wc = NA_WIN_COLS
    n_ctx = k_ctx.shape[2]
    scale = dh ** -0.5
    k_grid = k.reshape(b, h, rows, GRID_W, dh)
    v_grid = v.reshape(b, h, rows, GRID_W, dh)
    q_rows = jnp.moveaxis(q.reshape(b, h, rows, GRID_W, dh), 2, 0)
    q_col = jnp.arange(GRID_W)
    c_start = jnp.clip(q_col - wc // 2, 0, GRID_W - wc)
    k_col = jnp.tile(jnp.arange(GRID_W), wr)
    k_row_off = jnp.repeat(jnp.arange(wr), GRID_W)
    col_mask = (k_col[None, :] >= c_start[:, None]) & (k_col[None, :] < c_start[:, None] + wc)
    dc_idx = jnp.clip(k_col[None, :] - q_col[:, None], 1 - wc, wc - 1) + (NA_WIN_COLS - 1)

    def one_row(args):
        q_r, r = args
        r_start = jnp.clip(r - wr // 2, 0, rows - wr)
        k_r = lax.dynamic_slice_in_dim(k_grid, r_start, wr, axis=2).reshape(b, h, wr * GRID_W, dh)
        v_r = lax.dynamic_slice_in_dim(v_grid, r_start, wr, axis=2).reshape(b, h, wr * GRID_W, dh)
        dr_idx = r_start + k_row_off - r + (NA_WIN_ROWS - 1)
        bias = rpb[:, dr_idx[None, :], dc_idx].astype(jnp.float32)
        s_lat = jnp.einsum('bhqd,bhkd->bhqk', q_r, k_r).astype(jnp.float32) * scale + bias
        s_lat = jnp.where(col_mask, s_lat, -jnp.inf)
        s_ctx = jnp.einsum('bhqd,bhkd->bhqk', q_r, k_ctx).astype(jnp.float32) * scale
        p = jax.nn.softmax(jnp.concatenate([s_ctx, s_lat], axis=-1), axis=-1).astype(v.dtype)
        return (jnp.einsum('bhqk,bhkd->bhqd', p[..., :n_ctx], v_ctx)
                + jnp.einsum('bhqk,bhkd->bhqd', p[..., n_ctx:], v_r))

    out = lax.map(one_row, (q_rows, jnp.arange(rows)))
    return jnp.moveaxis(out, 0, 2).reshape(b, h, l, dh)


def short_conv(x, w):
    ch = x.shape[-1]
    return lax.conv_general_dilated(
        x, w.astype(x.dtype)[:, None, :], window_strides=(1,),
        padding=[(GDN_CONV // 2, GDN_CONV // 2)],
        dimension_numbers=('NWC', 'WIO', 'NWC'), feature_group_count=ch)


def gdn_chunk_scan(q, k, v, g, beta, s0, want_out):
    b, h, l, dk = q.shape
    dv = v.shape[-1]
    n = l // GDN_CHUNK
    f32 = jnp.float32
    q = q.astype(f32).reshape(b, h, n, GDN_CHUNK, dk)
    k = k.astype(f32).reshape(b, h, n, GDN_CHUNK, dk)
    v = v.astype(f32).reshape(b, h, n, GDN_CHUNK, dv)
    g = g.astype(f32).reshape(b, h, n, GDN_CHUNK)
    beta = beta.astype(f32).reshape(b, h, n, GDN_CHUNK)
    gc = jnp.cumsum(g, axis=-1)
    idx = jnp.arange(GDN_CHUNK)
    incl = idx[:, None] >= idx[None, :]
    strict = idx[:, None] > idx[None, :]
    dec = jnp.exp(jnp.where(incl, gc[..., :, None] - gc[..., None, :], -jnp.inf))
    a_mat = jnp.where(strict, beta[..., :, None] * jnp.einsum('bhncd,bhnkd->bhnck', k, k) * dec, 0.0)
    w_v = lax.linalg.triangular_solve(a_mat, beta[..., None] * v, left_side=True, lower=True,
                                      unit_diagonal=True)
    w_k = lax.linalg.triangular_solve(a_mat, (beta * jnp.exp(gc))[..., None] * k, left_side=True,
                                      lower=True, unit_diagonal=True)
    k_end = k * jnp.exp(gc[..., -1:] - gc)[..., None]
    b_end = jnp.exp(gc[..., -1])
    mv = lambda t: jnp.moveaxis(t, 2, 0)
    if want_out:
        p_in = jnp.einsum('bhncd,bhnkd->bhnck', q, k) * dec
        q_dec = q * jnp.exp(gc)[..., None]

        def step(s, xs):
            wv, wk, ke, be, qd, pp = xs
            u = wv - jnp.einsum('bhcd,bhde->bhce', wk, s)
            o = jnp.einsum('bhcd,bhde->bhce', qd, s) + jnp.einsum('bhck,bhke->bhce', pp, u)
            s = be[..., None, None] * s + jnp.einsum('bhcd,bhce->bhde', ke, u)
            return s, o

        s_fin, o = lax.scan(step, s0, (mv(w_v), mv(w_k), mv(k_end), mv(b_end), mv(q_dec), mv(p_in)))
        return s_fin, jnp.moveaxis(o, 0, 2).reshape(b, h, l, dv)

    def step_state(s, xs):
        wv, wk, ke, be = xs
        u = wv - jnp.einsum('bhcd,bhde->bhce', wk, s)
        return be[..., None, None] * s + jnp.einsum('bhcd,bhce->bhde', ke, u), None

    s_fin, _ = lax.scan(step_state, s0, (mv(w_v), mv(w_k), mv(k_end), mv(b_end)))
    return s_fin, None


def gdn_inputs(p, conv_w, a_log, dt_bias):
    b, l, _ = p.shape
    qkv = jax.nn.silu(short_conv(p[..., :3 * GDN_W], conv_w))
    q = l2norm(split_heads(qkv[..., :GDN_W], GDN_HEADS)) * (GDN_HEAD_DIM ** -0.5)
    k = l2norm(split_heads(qkv[..., GDN_W:2 * GDN_W], GDN_HEADS))
    v = split_heads(qkv[..., 2 * GDN_W:], GDN_HEADS)
    z = p[..., 3 * GDN_W:4 * GDN_W]
    gr = p[..., 4 * GDN_W:].astype(jnp.float32).reshape(b, l, 2, 2, GDN_HEADS).transpose(2, 3, 0, 4, 1)
    g = -jnp.exp(a_log.astype(jnp.float32))[:, None, :, None] * jax.nn.softplus(
        gr[:, 0] + dt_bias.astype(jnp.float32)[:, None, :, None])
    beta = jax.nn.sigmoid(gr[:, 1])
    return q, k, v, z, g, beta


def gdn_output(o, z, norm_g):
    b, h, l, dv = o.shape
    o = rmsnorm(o.transpose(0, 2, 1, 3), norm_g)
    gate = jax.nn.silu(z.astype(jnp.float32)).reshape(b, l, h, dv)
    return (o * gate).reshape(b, l, h * dv).astype(z.dtype)


def gated_deltanet(p, pc, conv_w, a_log, dt_bias, norm_g, want_ctx):
    q, k, v, z, g, beta = gdn_inputs(p, conv_w, a_log, dt_bias)
    qc, kc, vc, zc, gcx, betac = gdn_inputs(pc, conv_w, a_log, dt_bias)
    flip = lambda t: jnp.flip(t, axis=2)
    zero = jnp.zeros((q.shape[0], GDN_HEADS, GDN_HEAD_DIM, GDN_HEAD_DIM), jnp.float32)
    s_f, oc_f = gdn_chunk_scan(qc, kc, vc, gcx[0], betac[0], zero, want_ctx)
    s_b, oc_b = gdn_chunk_scan(flip(qc), flip(kc), flip(vc), flip(gcx[1]), flip(betac[1]), zero, want_ctx)
    _, o_f = gdn_chunk_scan(q, k, v, g[0], beta[0], s_f, True)
    _, o_b = gdn_chunk_scan(flip(q), flip(k), flip(v), flip(g[1]), flip(beta[1]), s_b, True)
    y = gdn_output(o_f + flip(o_b), z, norm_g)
    if not want_ctx:
        return y, None
    return y, gdn_output(oc_f + flip(oc_b), zc, norm_g)


def even_mixer(h, hc, w_in, conv_w, a_log, dt_bias, gdn_g, rpb, w_out, want_ctx):
    p = h @ w_in
    pc = hc @ w_in
    q = split_heads(p[..., :NA_W], NA_HEADS)
    k = split_heads(p[..., NA_W:2 * NA_W], NA_HEADS)
    v = split_heads(p[..., 2 * NA_W:3 * NA_W], NA_HEADS)
    kc = split_heads(pc[..., NA_W:2 * NA_W], NA_HEADS)
    vc = split_heads(pc[..., 2 * NA_W:3 * NA_W], NA_HEADS)
    y_a = merge_heads(neighbourhood_attention(q, k, v, kc, vc, rpb))
    y_b, yc_b = gated_deltanet(p[..., 3 * NA_W:], pc[..., 3 * NA_W:], conv_w, a_log, dt_bias, gdn_g, want_ctx)
    y = jnp.concatenate([y_a, y_b], axis=-1) @ w_out
    if not want_ctx:
        return y, None
    qc = split_heads(pc[..., :NA_W], NA_HEADS)
    yc_a = merge_heads(softmax_attend(qc, kc, vc, NA_HEAD_DIM ** -0.5))
    return y, jnp.concatenate([yc_a, yc_b], axis=-1) @ w_out


def mla_q(p, q_g, w_uq, rope):
    cq = rmsnorm(p[..., :MLA_Q_RANK], q_g)
    q = split_heads(cq @ w_uq, MLA_HEADS)
    q_nope, q_rope = q[..., :MLA_NOPE], q[..., MLA_NOPE:]
    if rope is not None:
        q_rope = apply_axial_rope(q_rope, rope[0], rope[1])
    return jnp.concatenate([q_nope, q_rope], axis=-1)


def mla_kv(p, kv_g, w_ukv, rope):
    b, l, _ = p.shape
    ckv = rmsnorm(p[..., MLA_Q_RANK:MLA_Q_RANK + MLA_KV_RANK], kv_g)
    kv = split_heads(ckv @ w_ukv, MLA_HEADS)
    k_nope, v = kv[..., :MLA_NOPE], kv[..., MLA_NOPE:]
    k_rope = p[..., MLA_Q_RANK + MLA_KV_RANK:][:, None]
    if rope is not None:
        k_rope = apply_axial_rope(k_rope, rope[0], rope[1])
    k = jnp.concatenate([k_nope, jnp.broadcast_to(k_rope, (b, MLA_HEADS, l, MLA_ROPE))], axis=-1)
    return k, v


def odd_mixer(h, hc, w_in, q_g, kv_g, w_uq, w_ukv, w_out, rope, want_ctx):
    p = h @ w_in
    pc = hc @ w_in
    scale = (MLA_NOPE + MLA_ROPE) ** -0.5
    q = mla_q(p, q_g, w_uq, rope)
    k, v = mla_kv(p, kv_g, w_ukv, rope)
    kc, vc = mla_kv(pc, kv_g, w_ukv, None)
    o = blocked_attention(q, jnp.concatenate([kc, k], axis=2), jnp.concatenate([vc, v], axis=2), scale)
    y = merge_heads(o) @ w_out
    if not want_ctx:
        return y, None
    qc = mla_q(pc, q_g, w_uq, None)
    return y, merge_heads(softmax_attend(qc, kc, vc, scale)) @ w_out


def peer_ffn(h, w_q, sub_keys, u_tab, v_tab):
    b, l, d = h.shape
    kk = PEER_TOPK
    blocks = h.reshape(b * l // PEER_BLOCK, PEER_BLOCK, d)

    def one_block(xb):
        t = xb.shape[0]
        qy = (xb @ w_q).reshape(t, PEER_HEADS, 2, PEER_DKEY // 2)
        s = jnp.einsum('thpd,hpnd->thpn', qy, sub_keys).astype(jnp.float32)
        s_top, i_top = lax.top_k(s, kk)
        comb = (s_top[:, :, 0, :, None] + s_top[:, :, 1, None, :]).reshape(t, PEER_HEADS, kk * kk)
        c_top, c_idx = lax.top_k(comb, kk)
        i1 = jnp.take_along_axis(i_top[:, :, 0], c_idx // kk, axis=-1)
        i2 = jnp.take_along_axis(i_top[:, :, 1], c_idx % kk, axis=-1)
        expert = i1 * PEER_KEYS + i2
        gate = jax.nn.softmax(c_top, axis=-1)
        u = u_tab[expert]
        vv = v_tab[expert]
        act = jax.nn.gelu(jnp.einsum('td,thkd->thk', xb, u).astype(jnp.float32), approximate=False)
        return jnp.einsum('thk,thkd->td', (gate * act).astype(xb.dtype), vv)

    return lax.map(one_block, blocks).reshape(b, l, d)


def setup_inputs(seed: int = 0) -> dict:
    key = jax.random.key(seed)
    ks = iter(jax.random.split(key, 32))
    f32 = jnp.float32
    nrm = lambda shape, s: jax.random.normal(next(ks), shape, f32) * s
    gain = lambda shape: 1.0 + 0.02 * jax.random.normal(next(ks), shape, f32)
    dt = jnp.exp(jax.random.uniform(next(ks), (N_EVEN, 2, GDN_HEADS), f32, minval=-6.9, maxval=-2.3))
    return {
        'x': nrm((BATCH, SEQ, D_MODEL), 1.0),
        'c': nrm((BATCH, D_MODEL), 1.0),
        'ctx': nrm((BATCH, CTX_LEN, D_MODEL), 1.0),
        'c_ctx': nrm((D_MODEL,), 1.0),
        'ada_w': nrm((DEPTH, D_MODEL, 6 * D_MODEL), 0.5 * D_MODEL ** -0.5),
        'ada_b': nrm((DEPTH, 6 * D_MODEL), 0.01),
        'norm1_g': gain((DEPTH, D_MODEL)),
        'norm2_g': gain((DEPTH, D_MODEL)),
        'final_g': gain((D_MODEL,)),
        'even_w_in': nrm((N_EVEN, D_MODEL, EVEN_IN), D_MODEL ** -0.5),
        'even_conv_w': nrm((N_EVEN, GDN_CONV, 3 * GDN_W), GDN_CONV ** -0.5),
        'gdn_a_log': jnp.log(jax.random.uniform(next(ks), (N_EVEN, 2, GDN_HEADS), f32, minval=1.0, maxval=16.0)),
        'gdn_dt_bias': dt + jnp.log(-jnp.expm1(-dt)),
        'gdn_norm_g': gain((N_EVEN, GDN_HEAD_DIM)),
        'na_rpb': nrm((N_EVEN, NA_HEADS, 2 * NA_WIN_ROWS - 1, 2 * NA_WIN_COLS - 1), 0.1),
        'even_w_out': nrm((N_EVEN, EVEN_MIX, D_MODEL), EVEN_MIX ** -0.5),
        'mla_w_in': nrm((N_ODD, D_MODEL, MLA_IN), D_MODEL ** -0.5),
        'mla_q_g': gain((N_ODD, MLA_Q_RANK)),
        'mla_kv_g': gain((N_ODD, MLA_KV_RANK)),
        'mla_w_uq': nrm((N_ODD, MLA_Q_RANK, MLA_HEADS * (MLA_NOPE + MLA_ROPE)), MLA_Q_RANK ** -0.5),
        'mla_w_ukv': nrm((N_ODD, MLA_KV_RANK, MLA_HEADS * (MLA_NOPE + MLA_V)), MLA_KV_RANK ** -0.5),
        'mla_w_out': nrm((N_ODD, MLA_HEADS * MLA_V, D_MODEL), (MLA_HEADS * MLA_V) ** -0.5),
        'peer_w_q': nrm((DEPTH, D_MODEL, PEER_HEADS * PEER_DKEY), D_MODEL ** -0.5),
        'peer_sub_keys': nrm((DEPTH, PEER_HEADS, 2, PEER_KEYS, PEER_DKEY // 2), (PEER_DKEY // 2) ** -0.5),
        'peer_u': nrm((DEPTH, PEER_EXPERTS, D_MODEL), D_MODEL ** -0.5),
        'peer_v': nrm((DEPTH, PEER_EXPERTS, D_MODEL), PEER_HEADS ** -0.5),
    }


def reference(x, c, ctx, c_ctx, ada_w, ada_b, norm1_g, norm2_g, final_g,
              even_w_in, even_conv_w, gdn_a_log, gdn_dt_bias, gdn_norm_g, na_rpb, even_w_out,
              mla_w_in, mla_q_g, mla_kv_g, mla_w_uq, mla_w_ukv, mla_w_out,
              peer_w_q, peer_sub_keys, peer_u, peer_v):
    rope = axial_rope_tables(x.shape[1], MLA_ROPE)
    cond = jax.nn.silu(c)
    cond_ctx = jax.nn.silu(c_ctx)[None, :]
    for layer in range(DEPTH):
        want_ctx = layer < DEPTH - 1
        i = layer // 2
        sh1, sc1, g1, sh2, sc2, g2 = jnp.split(cond @ ada_w[layer] + ada_b[layer], 6, axis=-1)
        csh1, csc1, cg1, csh2, csc2, cg2 = jnp.split(cond_ctx @ ada_w[layer] + ada_b[layer], 6, axis=-1)
        h = modulate(rmsnorm(x, norm1_g[layer]), sh1, sc1)
        hc = modulate(rmsnorm(ctx, norm1_g[layer]), csh1, csc1)
        if layer % 2 == 0:
            y, yc = even_mixer(h, hc, even_w_in[i], even_conv_w[i], gdn_a_log[i], gdn_dt_bias[i],
                               gdn_norm_g[i], na_rpb[i], even_w_out[i], want_ctx)
        else:
            y, yc = odd_mixer(h, hc, mla_w_in[i], mla_q_g[i], mla_kv_g[i], mla_w_uq[i], mla_w_ukv[i],
                              mla_w_out[i], rope, want_ctx)
        x = x + g1[:, None, :] * y
        x = x + g2[:, None, :] * peer_ffn(modulate(rmsnorm(x, norm2_g[layer]), sh2, sc2),
                                          peer_w_q[layer], peer_sub_keys[layer], peer_u[layer], peer_v[layer])
        if want_ctx:
            ctx = ctx + cg1[:, None, :] * yc
            ctx = ctx + cg2[:, None, :] * peer_ffn(modulate(rmsnorm(ctx, norm2_g[layer]), csh2, csc2),
                                                   peer_w_q[layer], peer_sub_keys[layer], peer_u[layer],
                                                   peer_v[layer])
    return rmsnorm(x, final_g)
```

```python
import numpy as np
import concourse.bass as bass
import concourse.mybir as mybir
from concourse.bass_utils import run_bass_kernel_spmd

F32 = mybir.dt.float32
BF16 = mybir.dt.bfloat16
U32 = mybir.dt.uint32
AF = mybir.ActivationFunctionType
ALU = mybir.AluOpType
AX = mybir.AxisListType

D = 1024
SEQ = 8192
CTX = 256
T = SEQ + CTX
NT = T // 128
DEPTH = 4
EPS = 1e-6
ENGS = ("pe", "act", "dve", "pool", "sp")
DMA_SEMS_PER_Q = 24
NEG = -30000.0


class Prog:
    def __init__(self):
        self.nc = bass.Bass("TRN2", target_bir_lowering=False)
        nc = self.nc
        self.E = {"pe": nc.tensor, "act": nc.scalar, "dve": nc.vector, "pool": nc.gpsimd, "sp": nc.sync}
        self.cnt = {e: 0 for e in ENGS}
        self.waited = {e: {} for e in ENGS}
        self.last_w = {}
        self.readers = {}
        self.dma_rr = {e: 0 for e in ENGS}
        self.dma_tot = {}
        self.sems = {}
        self.ctxs = []
        self.n_inst = 0
        self._semctx = set()
        self.uid = 0

    def sb(self, name, shape, dt=F32):
        self.uid += 1; name = "%s_%d" % (name, self.uid)
        c = self.nc.sbuf_tensor(name, list(shape), dt)
        t = c.__enter__(); self.ctxs.append(c)
        return t

    def ps(self, name, shape, dt=F32):
        self.uid += 1; name = "%s_%d" % (name, self.uid)
        c = self.nc.psum_tensor(name, list(shape), dt)
        t = c.__enter__(); self.ctxs.append(c)
        return t

    def psv(self, name, shape, dt=F32):
        esz = 4 if dt == F32 else 2
        per = 1
        for d in shape[1:]:
            per *= d
        nb = (per * esz + 2047) // 2048
        t = self.ps(name, [128, nb * (2048 // esz)], dt)
        v = t[0:shape[0], 0:per]
        if len(shape) > 2:
            names = " ".join("d%d" % i for i in range(1, len(shape)))
            v = v.rearrange("p (%s) -> p %s" % (names, names), **{"d%d" % i: shape[i] for i in range(2, len(shape))})
        return v

    def dram(self, name, shape, dt=F32, kind="Internal"):
        return self.nc.dram_tensor(name, list(shape), dt, kind=kind).ap()

    def _sem(self, n):
        if n not in self.sems:
            c = self.nc.semaphore(n); self.sems[n] = c.__enter__(); self.ctxs.append(c); self._semctx.add(c)
        return self.sems[n]

    def _need(self, eng, sem, val):
        w = self.waited[eng]
        if w.get(sem, 0) >= val:
            return
        w[sem] = val
        self.E[eng].wait_ge(self._sem(sem), val)

    def _deps(self, eng, reads, writes):
        for k in reads:
            lw = self.last_w.get(k)
            if lw is not None:
                self._need(eng, *lw)
        for k in writes:
            lw = self.last_w.get(k)
            if lw is not None:
                self._need(eng, *lw)
            for r in self.readers.get(k, ()):
                self._need(eng, *r)

    def _commit(self, tok, reads, writes):
        for k in reads:
            self.readers.setdefault(k, []).append(tok)
        for k in writes:
            self.last_w[k] = tok
            self.readers[k] = []

    def op(self, eng, fn, reads=(), writes=()):
        self._deps(eng, reads, writes)
        self.cnt[eng] += 1
        tok = ("S_" + eng, self.cnt[eng])
        fn(self.E[eng]).then_inc(self._sem(tok[0]), 1)
        if eng == "pe":
            self.waited[eng][tok[0]] = tok[1]
        self._commit(tok, reads, writes)
        self.n_inst += 1

    def dma(self, q, out, in_, r=(), w=(), **kw):
        reads, writes = r, w
        self._deps(q, reads, writes)
        i = self.dma_rr[q]; self.dma_rr[q] = (i + 1) % DMA_SEMS_PER_Q
        sem = "D_%s_%d" % (q, i)
        prev = self.dma_tot.get(sem, 0)
        if prev:
            self._need(q, sem, prev)
        tot = prev + 16
        self.dma_tot[sem] = tot
        self.E[q].dma_start(out=out, in_=in_, **kw).then_inc(self._sem(sem), 16)
        self._commit((sem, tot), reads, writes)
        self.n_inst += 1

    def mark(self):
        return len(self.ctxs)

    def barrier(self):
        for x in ENGS:
            for e in ENGS:
                if self.cnt[e]:
                    self._need(x, "S_" + e, self.cnt[e])
            for sem, tot in self.dma_tot.items():
                self._need(x, sem, tot)

    def release(self, m):
        self.barrier()
        keep = []
        for c in reversed(self.ctxs[m:]):
            if c in self._semctx:
                keep.append(c)
            else:
                c.__exit__(None, None, None)
        self.ctxs = self.ctxs[:m] + list(reversed(keep))

    def finish(self, final_keys):
        for k in final_keys:
            lw = self.last_w.get(k)
            if lw is not None:
                self._need("sp", *lw)
        for c in reversed(self.ctxs):
            c.__exit__(None, None, None)
        return self.nc

    def mm(self, out, lhsT, rhs, start, stop, r=(), w=()):
        self.op("pe", lambda e: e.matmul(out, lhsT=lhsT, rhs=rhs, start=start, stop=stop), r, w)

    def tr(self, out, in_, ident, r=(), w=()):
        self.op("pe", lambda e: e.transpose(out=out, in_=in_, identity=ident), r, w)

    def act(self, out, in_, func, r=(), w=(), **kw):
        self.op("act", lambda e: e.activation(out=out, in_=in_, func=func, **kw), r, w)

    def tt(self, eng, out, in0, in1, op, r=(), w=()):
        self.op(eng, lambda e: e.tensor_tensor(out=out, in0=in0, in1=in1, op=op), r, w)

    def ts(self, eng, out, in0, s1, op0, s2=None, op1=None, r=(), w=(), **kw):
        if op1 is None:
            self.op(eng, lambda e: e.tensor_scalar(out=out, in0=in0, scalar1=s1, scalar2=None, op0=op0, **kw), r, w)
        else:
            self.op(eng, lambda e: e.tensor_scalar(out=out, in0=in0, scalar1=s1, scalar2=s2, op0=op0, op1=op1, **kw), r, w)

    def stt(self, out, in0, scalar, in1, op0, op1, r=(), w=()):
        self.op("dve", lambda e: e.scalar_tensor_tensor(out=out, in0=in0, scalar=scalar, in1=in1, op0=op0, op1=op1), r, w)

    def cp(self, eng, out, in_, r=(), w=()):
        if eng == "act":
            self.op("act", lambda e: e.copy(out=out, in_=in_), r, w)
        else:
            self.op(eng, lambda e: e.tensor_copy(out=out, in_=in_), r, w)

    def memset(self, eng, ap, val, w=()):
        self.op(eng, lambda e: e.memset(ap, val), (), w)


class Rot:
    def __init__(self, P, name, n, shape, dt, psum=False):
        P.uid += 1
        self.name = "%s#%d" % (name, P.uid); self.n = n; self.i = 0
        if psum:
            self.t = [P.psv("%s%d" % (name, j), shape, dt) for j in range(n)]
        else:
            self.t = [P.sb("%s%d" % (name, j), shape, dt) for j in range(n)]

    def next(self):
        j = self.i; self.i = (j + 1) % self.n
        return self.t[j], (self.name, j)


def bc_row(ap_row, n):
    return ap_row.to_broadcast([128, n])


class K:
    def __init__(self, cfg):
        self.cfg = cfg
        self.P = Prog()
        P = self.P
        dr = lambda n, s, dt=F32: P.dram(n, s, dt, kind="ExternalInput")
        self.I = I = {}
        I["xin"] = dr("xin", [T, D])
        I["cond2"] = dr("cond2", [128, 8, 2])
        I["ada_w"] = dr("ada_w", [DEPTH, D, 6 * D])
        I["ada_b"] = dr("ada_b", [DEPTH, 6 * D])
        I["norm1_g"] = dr("norm1_g", [DEPTH, D])
        I["norm2_g"] = dr("norm2_g", [DEPTH, D])
        I["final_g"] = dr("final_g", [1, D])
        I["mla_w_in"] = dr("mla_w_in", [2, D, 704])
        I["mla_q_g"] = dr("mla_q_g", [2, 384])
        I["mla_kv_g"] = dr("mla_kv_g", [2, 256])
        I["mla_w_uq"] = dr("mla_w_uq", [2, 384, 1536])
        I["mla_w_ukv"] = dr("mla_w_ukv", [2, 256, 2048])
        I["mla_w_out"] = dr("mla_w_out", [2, D, D])
        I["cosT"] = dr("cosT", [64, T])
        I["sinT"] = dr("sinT", [64, T])
        I["peer_w_q"] = dr("peer_w_q", [DEPTH, D, 2048])
        I["skT"] = dr("skT", [DEPTH, 128, 16, 128])
        I["peer_uT"] = dr("peer_uT", [DEPTH, D, 16384])
        I["peer_v"] = dr("peer_v", [DEPTH, 16384, D])
        I["even_w_in"] = dr("even_w_in", [2, D, 3600])
        I["even_w_out"] = dr("even_w_out", [2, D, D])
        I["conv_wT"] = dr("conv_wT", [2, 12, 128, 5])
        I["gdn_ab"] = dr("gdn_ab", [2, 2, 8])
        I["gdn_norm_g"] = dr("gdn_norm_g", [2, 128])
        I["rpbg"] = dr("rpbg", [2, 64, 15, 8, 64])
        I["namask"] = dr("namask", [64, 64])
        self.out = P.dram("out", [SEQ, D], F32, kind="ExternalOutput")
        self.xres = P.dram("xres", [T, D], F32)
        self.ada_s = P.dram("ada_s", [DEPTH, 2, 6 * D], F32)
        self.attn = P.dram("attn", [T, D], F32)
        self.h2T = P.dram("h2T", [D, T], BF16)
        self.UV = P.dram("UV", [128, 128, 2048], BF16)
        self.GTd = [P.dram("GTd0", [34, 128, 128, 128], BF16), P.dram("GTd1", [NT - 34, 128, 128, 128], BF16)]
        self.sel = P.dram("sel", [NT, 128, 3, 128], F32)
        self.dbg = {}
        for name, shape in cfg.get("dbg", {}).items():
            self.dbg[name] = P.dram("dbg_" + name, list(shape), F32, kind="ExternalOutput")
        self.consts()

    def consts(self):
        P = self.P
        self.identf = P.sb("identf", [128, 128], F32)
        self.ident = P.sb("ident", [128, 128], BF16)
        P.memset("pool", self.identf[:], 1.0, w=["identf"])
        P.op("pool", lambda e: e.affine_select(out=self.identf[:], in_=self.identf[:], pattern=[[-1, 128]],
                                               compare_op=ALU.is_equal, fill=0.0, base=0, channel_multiplier=1),
             ["identf"], ["identf"])
        P.cp("dve", self.ident[:], self.identf[:], ["identf"], ["ident"])
        self.iotab = P.sb("iotab", [128, 128], BF16)
        self.iotaf = P.sb("iotaf", [128, 128], F32)
        P.op("pool", lambda e: e.iota(self.iotaf[:], pattern=[[1, 128]], base=0, channel_multiplier=0,
                                      allow_small_or_imprecise_dtypes=True), (), ["iotaf"])
        P.cp("dve", self.iotab[:], self.iotaf[:], ["iotaf"], ["iotab"])
        self.modA = [P.sb("modA%d" % s, [128, D], F32) for s in range(2)]
        self.modS = [P.sb("modS%d" % s, [128, D], F32) for s in range(2)]
        self.modG = [P.sb("modG%d" % s, [128, D], F32) for s in range(2)]
        self.ng = P.sb("ng", [128, D], F32)
        self.xt = Rot(P, "xt", 2, [128, D], F32)
        self.hb = Rot(P, "hb", 2, [128, D], BF16)
        self.tmpf = Rot(P, "tmpf", 2, [128, D], F32)
        self.small = Rot(P, "small", 4, [128, 4], F32)
        self.junk = P.sb("junk", [128, D], F32)

    def phase_ada(self):
        P, I = self.P, self.I
        m = P.mark()
        cs = P.sb("cs", [128, 8, 2], F32)
        P.dma("sp", cs[:], I["cond2"][:, :, :], w=["cs"])
        P.act(cs[:], cs[:], AF.Silu, ["cs"], ["cs"])
        adab = P.sb("adab", [2, 6 * D], F32)
        arow = P.sb("arow", [2, 6 * D], F32)
        wt = Rot(P, "adaw", 2, [128, 8, 512], F32)
        pa = Rot(P, "pada", 2, [128, 512], F32, psum=True)
        for l in self.cfg["layers"]:
            P.dma("sp", adab[:], I["ada_b"][l:l + 1, :].to_broadcast([2, 6 * D]), w=["adab"])
            wv = I["ada_w"][l].rearrange("(kc p) n -> p kc n", p=128)
            for j in range(12):
                w, wk = wt.next()
                P.dma("sp", w[:], wv[:, :, j * 512:(j + 1) * 512], w=[wk])
                ps, pk = pa.next()
                for kc in range(8):
                    P.mm(ps[0:2, :], cs[:, kc, :], w[:, kc, :], kc == 0, kc == 7, ["cs", wk], [pk])
                P.tt("dve", arow[:, j * 512:(j + 1) * 512], ps[0:2, :], adab[:, j * 512:(j + 1) * 512], ALU.add,
                     [pk, "adab"], ["arow"])
            P.dma("sp", self.ada_s[l, :, :], arow[:], ["arow"], [("ada_s", l)])
        P.release(m)

    def load_AS(self, l, which, streams=(0, 1)):
        P, I = self.P, self.I
        off = (which - 1) * 3 * D
        ngd = I["norm1_g"] if which == 1 else I["norm2_g"]
        P.dma("sp", self.ng[:], ngd[l:l + 1, :].to_broadcast([128, D]), w=["ng"])
        for s in streams:
            row = self.ada_s[l, s:s + 1, :]
            P.dma("sp", self.modS[s][:], row[:, off:off + D].to_broadcast([128, D]), [("ada_s", l)], [("modS", s)])
            P.dma("sp", self.modA[s][:], row[:, off + D:off + 2 * D].to_broadcast([128, D]), [("ada_s", l)], [("modA", s)])
            P.stt(self.modA[s][:], self.modA[s][:], 1.0, self.ng[:], ALU.add, ALU.mult, [("modA", s), "ng"], [("modA", s)])

    def load_gate(self, l, which, streams=(0, 1)):
        P = self.P
        off = (which - 1) * 3 * D
        for s in streams:
            row = self.ada_s[l, s:s + 1, :]
            P.dma("sp", self.modG[s][:], row[:, off + 2 * D:off + 3 * D].to_broadcast([128, D]), [("ada_s", l)], [("modG", s)])

    def rstd(self, x_ap, n, xk, eps=EPS):
        P = self.P
        sm, sk = self.small.next()
        P.act(self.junk[:, 0:n], x_ap, AF.Square, [xk], ["junk", sk], accum_out=sm[:, 0:1])
        P.act(sm[:, 1:2], sm[:, 0:1], AF.Sqrt, [sk], [sk], scale=1.0 / n, bias=eps)
        P.op("dve", lambda e: e.reciprocal(out=sm[:, 2:3], in_=sm[:, 1:2]), [sk], [sk])
        return sm[:, 2:3], sk

    def norm_mod_T(self, x_ap, xk, s, hT_dst, hT_key, pst):
        P = self.P
        r, rk = self.rstd(x_ap, D, xk)
        tf, tk = self.tmpf.next()
        P.stt(tf[:], x_ap, r, self.modA[s][:], ALU.mult, ALU.mult, [xk, rk, ("modA", s)], [tk])
        hb, hk = self.hb.next()
        P.tt("dve", hb[:], tf[:], self.modS[s][:], ALU.add, [tk, ("modS", s)], [hk])
        ps, pk = pst.next()
        for kc in range(8):
            P.tr(ps[:, kc, :], hb[:, kc * 128:(kc + 1) * 128], self.ident[:], [hk, "ident"], [pk])
        P.cp("act", hT_dst, ps[:], [pk], [hT_key])


def blocks(want_ctx=True):
    b = [(0, 256)] if want_ctx else []
    return b + [(256 + 512 * i, 512) for i in range(16)]


def rot_cols(P, eng, dst, src, r, w):
    for (d0, s0, sign) in ((0, 16, -1.0), (16, 0, 1.0), (32, 48, -1.0), (48, 32, 1.0)):
        P.ts(eng, dst[:, :, d0:d0 + 16], src[:, :, s0:s0 + 16], sign, ALU.mult, r=r, w=w)


class KM(K):
    def phase_init(self):
        P = self.P
        src = self.I["xin"]
        for i in range(0, NT, 6):
            P.dma("sp", self.xres[i * 128:(i + 6) * 128, :], src[i * 128:(i + 6) * 128, :], (),
                  [("x", j) for j in range(i, i + 6)])

    def phase_mla_proj(self, l):
        P, I = self.P, self.I
        i = l // 2
        m = P.mark()
        self.QnT = P.dram("QnT%d" % l, [8, 128, T], BF16)
        self.QrT = P.dram("QrT%d" % l, [8, 64, T], BF16)
        self.KnT = P.dram("KnT%d" % l, [8, 128, T], BF16)
        self.KrT = P.dram("KrT%d" % l, [64, T], BF16)
        self.Vd = P.dram("Vd%d" % l, [T, D], BF16)
        Win = P.sb("Win", [128, 8, 704], BF16)
        P.dma("pool", Win[:], I["mla_w_in"][i].rearrange("(kc p) n -> p kc n", p=128), w=["Win"])
        WinRot = P.sb("WinRot", [128, 8, 64], BF16)
        rot_cols(P, "dve", WinRot[:, :, :], Win[:, :, 640:704], ["Win"], ["WinRot"])
        Wuq = P.sb("Wuq", [128, 3, 1536], BF16)
        P.dma("pool", Wuq[:], I["mla_w_uq"][i].rearrange("(kc p) n -> p kc n", p=128), w=["Wuq"])
        WuqRot = P.sb("WuqRot", [128, 3, 8, 64], BF16)
        for kc in range(3):
            src = Wuq[:, kc, :].rearrange("p (h x) -> p h x", x=192)[:, :, 128:192]
            rot_cols(P, "dve", WuqRot[:, kc, :, :], src, ["Wuq"], ["WuqRot"])
        Wukv = P.sb("Wukv", [128, 2, 2048], BF16)
        P.dma("pool", Wukv[:], I["mla_w_ukv"][i].rearrange("(kc p) n -> p kc n", p=128), w=["Wukv"])
        qg = P.sb("qg", [128, 384], F32)
        kvg = P.sb("kvg", [128, 256], F32)
        P.dma("sp", qg[:], I["mla_q_g"][i:i + 1, :].to_broadcast([128, 384]), w=["qg"])
        P.dma("sp", kvg[:], I["mla_kv_g"][i:i + 1, :].to_broadcast([128, 256]), w=["kvg"])
        self.load_AS(l, 1)

        pst = Rot(P, "pst", 2, [128, 8, 128], BF16, psum=True)
        psp = Rot(P, "psp", 1, [128, 1024], F32, psum=True)
        psu = Rot(P, "psu", 3, [128, 512], F32, psum=True)
        hTr = Rot(P, "hT", 2, [128, 8, 512], BF16)
        cTr = Rot(P, "cT", 2, [128, 5, 512], BF16)
        cqn = Rot(P, "cqn", 2, [128, 640], BF16)
        cst = Rot(P, "cst", 2, [64, 2, 512], F32)
        stQn = Rot(P, "stQn", 2, [128, 8, 512], BF16)
        stKn = Rot(P, "stKn", 2, [128, 8, 512], BF16)
        stQr = Rot(P, "stQr", 2, [64, 8, 512], BF16)
        stKr = Rot(P, "stKr", 2, [64, 512], BF16)
        rtmp = Rot(P, "rtmp", 2, [64, 2, 512], F32)
        vsb = Rot(P, "vsb", 2, [128, D], BF16)

        def rope_out(ps1, k1, ps2, k2, cs, ck, n, dst, dk):
            rt, rk = rtmp.next()
            P.tt("dve", rt[:, 0, 0:n], ps1[0:64, 0:n], cs[:, 0, 0:n], ALU.mult, [k1, ck], [rk])
            P.tt("dve", rt[:, 1, 0:n], ps2[0:64, 0:n], cs[:, 1, 0:n], ALU.mult, [k2, ck], [rk])
            P.tt("dve", dst, rt[:, 0, 0:n], rt[:, 1, 0:n], ALU.add, [rk], [dk])

        for (t0, n) in blocks(True):
            s = 0 if t0 >= CTX else 1
            nt = n // 128
            hT, hk = hTr.next()
            cT, ck_ = cTr.next()
            cs, csk = cst.next()
            P.dma("sp", cs[:, 0, 0:n], I["cosT"][:, t0:t0 + n], w=[csk])
            P.dma("sp", cs[:, 1, 0:n], I["sinT"][:, t0:t0 + n], w=[csk])
            for j in range(nt):
                ti = t0 // 128 + j
                xt, xk = self.xt.next()
                P.dma("sp", xt[:], self.xres[ti * 128:(ti + 1) * 128, :], [("x", ti)], [xk])
                self.norm_mod_T(xt[:], xk, s, hT[:, :, j * 128:(j + 1) * 128], hk, pst)
                pp, ppk = psp.next()
                for kc in range(8):
                    P.mm(pp[:, 0:512], hT[:, kc, j * 128:(j + 1) * 128], Win[:, kc, 0:512], kc == 0, kc == 7, [hk, "Win"], [ppk])
                for kc in range(8):
                    P.mm(pp[:, 512:640], hT[:, kc, j * 128:(j + 1) * 128], Win[:, kc, 512:640], kc == 0, kc == 7, [hk, "Win"], [ppk])
                r1, r1k = self.rstd(pp[:, 0:384], 384, ppk)
                r2, r2k = self.rstd(pp[:, 384:640], 256, ppk)
                cq, cqk = cqn.next()
                P.stt(cq[:, 0:384], pp[:, 0:384], r1, qg[:], ALU.mult, ALU.mult, [ppk, r1k, "qg"], [cqk])
                P.stt(cq[:, 384:640], pp[:, 384:640], r2, kvg[:], ALU.mult, ALU.mult, [ppk, r2k, "kvg"], [cqk])
                ps, pk = pst.next()
                for kc in range(5):
                    P.tr(ps[:, kc, :], cq[:, kc * 128:(kc + 1) * 128], self.ident[:], [cqk, "ident"], [pk])
                P.cp("act", cT[:, :, j * 128:(j + 1) * 128], ps[:, 0:5, :], [pk], [ck_])
            sQn, sQnk = stQn.next(); sKn, sKnk = stKn.next(); sQr, sQrk = stQr.next(); sKr, sKrk = stKr.next()
            for h in range(8):
                ps, pk = psu.next()
                for kc in range(3):
                    P.mm(ps[:, 0:n], Wuq[:, kc, h * 192:h * 192 + 128], cT[:, kc, 0:n], kc == 0, kc == 2, ["Wuq", ck_], [pk])
                P.cp("act", sQn[:, h, 0:n], ps[:, 0:n], [pk], [sQnk])
                ps, pk = psu.next()
                for kc in range(2):
                    P.mm(ps[:, 0:n], Wukv[:, kc, h * 256:h * 256 + 128], cT[:, 3 + kc, 0:n], kc == 0, kc == 1, ["Wukv", ck_], [pk])
                P.cp("act", sKn[:, h, 0:n], ps[:, 0:n], [pk], [sKnk])
                ps1, k1 = psu.next()
                for kc in range(3):
                    P.mm(ps1[0:64, 0:n], Wuq[:, kc, h * 192 + 128:h * 192 + 192], cT[:, kc, 0:n], kc == 0, kc == 2, ["Wuq", ck_], [k1])
                ps2, k2 = psu.next()
                for kc in range(3):
                    P.mm(ps2[0:64, 0:n], WuqRot[:, kc, h, :], cT[:, kc, 0:n], kc == 0, kc == 2, ["WuqRot", ck_], [k2])
                rope_out(ps1, k1, ps2, k2, cs, csk, n, sQr[:, h, 0:n], sQrk)
            ps1, k1 = psu.next()
            for kc in range(8):
                P.mm(ps1[0:64, 0:n], Win[:, kc, 640:704], hT[:, kc, 0:n], kc == 0, kc == 7, ["Win", hk], [k1])
            ps2, k2 = psu.next()
            for kc in range(8):
                P.mm(ps2[0:64, 0:n], WinRot[:, kc, :], hT[:, kc, 0:n], kc == 0, kc == 7, ["WinRot", hk], [k2])
            rope_out(ps1, k1, ps2, k2, cs, csk, n, sKr[:, 0:n], sKrk)
            bk = ("mlab", t0)
            P.dma("sp", self.QnT[:, :, t0:t0 + n].rearrange("h p t -> p h t"), sQn[:, :, 0:n], [sQnk], [("QnT", t0)])
            P.dma("sp", self.KnT[:, :, t0:t0 + n].rearrange("h p t -> p h t"), sKn[:, :, 0:n], [sKnk], [("KnT", t0)])
            P.dma("sp", self.QrT[:, :, t0:t0 + n].rearrange("h p t -> p h t"), sQr[:, :, 0:n], [sQrk], [("QrT", t0)])
            P.dma("sp", self.KrT[:, t0:t0 + n], sKr[:, 0:n], [sKrk], [("KrT", t0)])
            for j in range(nt):
                vs, vk = vsb.next()
                for hf in range(2):
                    ps, pk = psu.next()
                    for kc in range(2):
                        rhs = Wukv[:, kc, :].rearrange("p (h x) -> p h x", x=256)[:, 4 * hf:4 * hf + 4, 128:256]
                        P.mm(ps[:, :], cT[:, 3 + kc, j * 128:(j + 1) * 128], rhs, kc == 0, kc == 1, ["Wukv", ck_], [pk])
                    P.cp("act", vs[:, hf * 512:(hf + 1) * 512], ps[:, :], [pk], [vk])
                ti = t0 // 128 + j
                P.dma("sp", self.Vd[ti * 128:(ti + 1) * 128, :], vs[:], [vk], [("Vd", ti)])
        P.release(m)

    def phase_mla_attn(self, l, want_ctx):
        P = self.P
        m = P.mark()
        scale = 192.0 ** -0.5
        kn = Rot(P, "kn", 2, [128, T], BF16)
        va = Rot(P, "va", 2, [128, NT, 129], BF16)
        kr = P.sb("kr", [64, T], BF16)
        qn = Rot(P, "qn", 2, [128, 512], BF16)
        qr = Rot(P, "qr", 2, [64, 512], BF16)
        pT = Rot(P, "pT", 4, [128, 512], BF16)
        psS = Rot(P, "psS", 3, [128, 512], F32, psum=True)
        psO = P.ps("psO", [128, 4, 512], F32)
        osb = Rot(P, "osb", 2, [128, 4, 128], F32)
        for t in va.t:
            P.memset("pool", t[:, :, 128:129], 1.0, w=[(va.name, va.t.index(t))])
        allq = [("QnT", b[0]) for b in blocks(True)]
        P.dma("sp", kr[:], self.KrT[:, :], [("KrT", b[0]) for b in blocks(True)], ["kr"])
        for h in range(8):
            k_t, kk = kn.next()
            v_t, vk = va.next()
            P.dma("sp", k_t[:], self.KnT[h, :, :], [("KnT", b[0]) for b in blocks(True)], [kk])
            for q4 in range(0, NT, 11):
                P.dma("sp", v_t[:, q4:q4 + 11, 0:128],
                      self.Vd[q4 * 128:(q4 + 11) * 128, h * 128:(h + 1) * 128].rearrange("(kt p) d -> p kt d", p=128),
                      [("Vd", ti) for ti in range(q4, q4 + 11)], [vk])
            for (t0, n) in blocks(want_ctx):
                nkt = NT if t0 >= CTX else 2
                q_t, qk = qn.next()
                qr_t, qrk = qr.next()
                P.dma("sp", q_t[:, 0:n], self.QnT[h, :, t0:t0 + n], [("QnT", t0)], [qk])
                P.dma("sp", qr_t[:, 0:n], self.QrT[h, :, t0:t0 + n], [("QrT", t0)], [qrk])
                nj = n // 128
                def s_stage(kt):
                    ps, pk = psS.next()
                    P.mm(ps[:, 0:n], k_t[:, kt * 128:(kt + 1) * 128], q_t[:, 0:n], True, False, [kk, qk], [pk])
                    P.mm(ps[:, 0:n], kr[:, kt * 128:(kt + 1) * 128], qr_t[:, 0:n], False, True, ["kr", qrk], [pk])
                    p_t, ptk = pT.next()
                    P.act(p_t[:, 0:n], ps[:, 0:n], AF.Exp, [pk], [ptk], scale=scale)
                    return p_t, ptk

                def pv_stage(kt, p_t, ptk):
                    for j in range(nj):
                        P.mm(psO[:, j, 0:129], p_t[:, j * 128:(j + 1) * 128], v_t[:, kt, :], kt == 0, kt == nkt - 1,
                             [ptk, vk], [("psO", j)])

                prev = s_stage(0)
                for kt in range(nkt):
                    nxt = s_stage(kt + 1) if kt + 1 < nkt else None
                    pv_stage(kt, *prev)
                    prev = nxt
                o_t, ok = osb.next()
                for j in range(nj):
                    sm, sk = self.small.next()
                    P.op("dve", lambda e, sm=sm, j=j: e.reciprocal(out=sm[:, 0:1], in_=psO[:, j, 128:129]), [("psO", j)], [sk])
                    P.ts("dve", o_t[:, j, :], psO[:, j, 0:128], sm[:, 0:1], ALU.mult, r=[("psO", j), sk], w=[ok])
                P.dma("sp", self.attn[t0:t0 + n, h * 128:(h + 1) * 128].rearrange("(j p) d -> p j d", p=128),
                      o_t[:, 0:nj, :], [ok], [("attn", t0, h)])
        P.release(m)

    def phase_outproj(self, l, wout_ap, want_ctx):
        P = self.P
        m = P.mark()
        Wout = P.sb("Wout", [128, 8, D], BF16)
        P.dma("pool", Wout[:], wout_ap.rearrange("(kc p) n -> p kc n", p=128), w=["Wout"])
        self.load_gate(l, 1)
        self.load_AS(l, 2)
        pst = Rot(P, "pst", 2, [128, 8, 128], BF16, psum=True)
        psy = Rot(P, "psy", 2, [128, D], F32, psum=True)
        at = Rot(P, "at", 2, [128, D], F32)
        ab = Rot(P, "ab", 2, [128, D], BF16)
        aT = Rot(P, "aT", 2, [128, 8, 128], BF16)
        xn = Rot(P, "xn", 2, [128, D], F32)
        h2s = Rot(P, "h2s", 2, [128, 8, 128], BF16)
        for ti in range(0 if want_ctx else 2, NT):
            s = 0 if ti >= 2 else 1
            a_t, ak = at.next()
            P.dma("sp", a_t[:], self.attn[ti * 128:(ti + 1) * 128, :], (), [ak])
            b_t, bk = ab.next()
            P.cp("act", b_t[:], a_t[:], [ak], [bk])
            ps, pk = pst.next()
            for kc in range(8):
                P.tr(ps[:, kc, :], b_t[:, kc * 128:(kc + 1) * 128], self.ident[:], [bk, "ident"], [pk])
            aT_t, aTk = aT.next()
            P.cp("act", aT_t[:], ps[:], [pk], [aTk])
            py, pyk = psy.next()
            for hf in range(2):
                for kc in range(8):
                    P.mm(py[:, hf * 512:(hf + 1) * 512], aT_t[:, kc, :], Wout[:, kc, hf * 512:(hf + 1) * 512], kc == 0, kc == 7,
                         [aTk, "Wout"], [pyk])
            xt, xk = self.xt.next()
            P.dma("sp", xt[:], self.xres[ti * 128:(ti + 1) * 128, :], (), [xk])
            tf, tk = self.tmpf.next()
            P.tt("dve", tf[:], py[:], self.modG[s][:], ALU.mult, [pyk, ("modG", s)], [tk])
            x_n, xnk = xn.next()
            P.tt("dve", x_n[:], tf[:], xt[:], ALU.add, [tk, xk], [xnk])
            P.dma("sp", self.xres[ti * 128:(ti + 1) * 128, :], x_n[:], [xnk], [("x", ti)])
            h_t, hk = h2s.next()
            self.norm_mod_T(x_n[:], xnk, s, h_t[:], hk, pst)
            P.dma("sp", self.h2T[:, ti * 128:(ti + 1) * 128].rearrange("(kc p) t -> p kc t", p=128), h_t[:], [hk], [("h2T", ti)])
        P.release(m)

    def phase_peer_prep(self, l):
        P, I = self.P, self.I
        uT = I["peer_uT"][l].rearrange("(kc p) e -> p kc e", p=128)
        for c in range(128):
            P.dma("pool", self.UV[c, :, 0:1024].rearrange("p (kc e) -> p kc e", e=128), uT[:, :, c * 128:(c + 1) * 128], (), [("UV", c)])
            P.dma("pool", self.UV[c, :, 1024:2048], I["peer_v"][l, c * 128:(c + 1) * 128, :], (), [("UV", c)])

    def phase_peer_sel(self, l, want_ctx):
        P, I = self.P, self.I
        m = P.mark()
        Wq = P.sb("Wq", [128, 8, 2048], BF16)
        P.dma("pool", Wq[:], I["peer_w_q"][l].rearrange("(kc p) n -> p kc n", p=128), w=["Wq"])
        skT = P.sb("skT", [128, 16, 128], F32)
        P.dma("sp", skT[:], I["skT"][l], w=["skT"])
        h2 = Rot(P, "h2", 2, [128, 8, 128], BF16)
        qTsR = Rot(P, "qTs", 2, [128, 16, 128], F32)
        s_sbR = Rot(P, "s_sb", 2, [128, 16, 128], F32)
        s_tmp = P.sb("s_tmp", [128, 16, 128], F32)
        stop_ = P.sb("stop", [128, 16, 16], F32)
        sidx = P.sb("sidx", [128, 16, 16], U32)
        sidf = P.sb("sidf", [128, 16, 16], F32)
        comb = P.sb("comb", [128, 8, 256], F32)
        comb2 = P.sb("comb2", [128, 8, 256], F32)
        ctop = P.sb("ctop", [128, 8, 16], F32)
        cidx = P.sb("cidx", [128, 8, 16], U32)
        cj = P.sb("cj", [128, 2, 8, 16], U32)
        cjf = P.sb("cjf", [128, 2, 8, 16], F32)
        oh = P.sb("oh", [128, 8, 16, 16], F32)
        i12 = P.sb("i12", [128, 3, 128], F32)
        i12T = Rot(P, "i12T", 2, [128, 3, 128], F32)
        allA = [("stopA", hp) for hp in range(16)]; allB = [("stopB", hp) for hp in range(16)]
        allI = [("sidxA", hp) for hp in range(16)] + [("sidxB", hp) for hp in range(16)]
        nmx = P.sb("nmx", [128, 8], F32)
        zs = P.sb("zs", [128, 8], F32)
        psA = Rot(P, "psA", 3, [128, 512], F32, psum=True)
        psS = P.ps("psS4", [128, 4, 512], F32)
        iota16 = self.iotaf[:, 0:16]
        for ti in range(0 if want_ctx else 2, NT):
            h_t, hk = h2.next()
            qTs, qTsk = qTsR.next()
            s_sb, ssk = s_sbR.next()
            P.dma("sp", h_t[:], self.h2T[:, ti * 128:(ti + 1) * 128].rearrange("(kc p) t -> p kc t", p=128), (), [hk])
            for hp4 in range(4):
                ps, pk = psA.next()
                for q in range(4):
                    hp = hp4 * 4 + q
                    for kc in range(8):
                        P.mm(ps[:, q * 128:(q + 1) * 128], Wq[:, kc, hp * 128:(hp + 1) * 128], h_t[:, kc, :], kc == 0, kc == 7,
                             ["Wq", hk], [pk])
                P.cp("act", qTs[:, hp4 * 4:hp4 * 4 + 4, :], ps[:, :].rearrange("p (a n) -> p a n", n=128), [pk], [qTsk])
            for hp in range(16):
                P.mm(psS[:, hp // 4, (hp % 4) * 128:(hp % 4 + 1) * 128], qTs[:, hp, :], skT[:, hp, :], True, True,
                     [qTsk, "skT"], [("psS", hp // 4)])
            for g in range(4):
                P.cp("act", s_sb[:, 4 * g:4 * g + 4, :], psS[:, g, :].rearrange("p (a n) -> p a n", n=128), [("psS", g)], [ssk])
            for hp in range(16):
                P.op("dve", lambda e, hp=hp: e.max(out=stop_[:, hp, 0:8], in_=s_sb[:, hp, :]), [ssk], [("stopA", hp)])
            for hp in range(16):
                P.op("dve", lambda e, hp=hp: e.match_replace(out=s_tmp[:, hp, :], in_to_replace=stop_[:, hp, 0:8], in_values=s_sb[:, hp, :],
                                                             imm_value=-1e30), [ssk, ("stopA", hp)], [("s_tmp", hp)])
            for hp in range(16):
                P.op("dve", lambda e, hp=hp: e.max(out=stop_[:, hp, 8:16], in_=s_tmp[:, hp, :]), [("s_tmp", hp)], [("stopB", hp)])
            for hp in range(16):
                P.op("dve", lambda e, hp=hp: e.max_index(out=sidx[:, hp, 0:8], in_max=stop_[:, hp, 0:8], in_values=s_sb[:, hp, :]),
                     [ssk, ("stopA", hp)], [("sidxA", hp)])
            for hp in range(16):
                P.op("dve", lambda e, hp=hp: e.max_index(out=sidx[:, hp, 8:16], in_max=stop_[:, hp, 8:16], in_values=s_sb[:, hp, :]),
                     [ssk, ("stopB", hp)], [("sidxB", hp)])
            P.cp("dve", sidf[:], sidx[:], allI, ["sidf"])
            st4 = stop_[:].rearrange("p (h two) j -> p h two j", two=2)
            in0 = st4[:, :, 0, :].unsqueeze(3).to_broadcast([128, 8, 16, 16])
            in1 = st4[:, :, 1, :].unsqueeze(2).to_broadcast([128, 8, 16, 16])
            P.tt("dve", comb[:].rearrange("p h (a b) -> p h a b", b=16), in0, in1, ALU.add, allA + allB, ["comb"])
            for h in range(8):
                P.op("dve", lambda e, h=h: e.max(out=ctop[:, h, 0:8], in_=comb[:, h, :]), ["comb"], [("ctopA", h)])
            for h in range(8):
                P.op("dve", lambda e, h=h: e.match_replace(out=comb2[:, h, :], in_to_replace=ctop[:, h, 0:8], in_values=comb[:, h, :],
                                                           imm_value=-1e30), ["comb", ("ctopA", h)], [("comb2", h)])
            for h in range(8):
                P.op("dve", lambda e, h=h: e.max(out=ctop[:, h, 8:16], in_=comb2[:, h, :]), [("comb2", h)], [("ctopB", h)])
            for h in range(8):
                P.op("dve", lambda e, h=h: e.max_index(out=cidx[:, h, 0:8], in_max=ctop[:, h, 0:8], in_values=comb[:, h, :]),
                     ["comb", ("ctopA", h)], [("cidxA", h)])
            for h in range(8):
                P.op("dve", lambda e, h=h: e.max_index(out=cidx[:, h, 8:16], in_max=ctop[:, h, 8:16], in_values=comb[:, h, :]),
                     ["comb", ("ctopB", h)], [("cidxB", h)])
            ctk = [("ctopA", h) for h in range(8)] + [("ctopB", h) for h in range(8)]
            cik = [("cidxA", h) for h in range(8)] + [("cidxB", h) for h in range(8)]
            P.ts("dve", cj[:, 0, :, :], cidx[:], 4, ALU.logical_shift_right, r=cik, w=["cj"])
            P.ts("dve", cj[:, 1, :, :], cidx[:], 15, ALU.bitwise_and, r=cik, w=["cj"])
            P.cp("dve", cjf[:], cj[:], ["cj"], ["cjf"])
            sd4 = sidf[:].rearrange("p (h two) j -> p h two j", two=2)
            for w_ in range(2):
                P.tt("dve", oh[:], cjf[:, w_, :, :].unsqueeze(3).to_broadcast([128, 8, 16, 16]),
                     iota16.unsqueeze(1).unsqueeze(1).to_broadcast([128, 8, 16, 16]), ALU.is_equal, ["cjf", "iotaf"], ["oh"])
                P.tt("dve", oh[:], oh[:], sd4[:, :, w_, :].unsqueeze(2).to_broadcast([128, 8, 16, 16]), ALU.mult, ["oh", "sidf"], ["oh"])
                P.op("dve", lambda e, w_=w_: e.tensor_reduce(out=i12[:, w_, :].rearrange("p (h k) -> p h k", k=16), in_=oh[:],
                                                             axis=AX.X, op=ALU.add), ["oh"], ["i12"])
            P.ts("dve", nmx[:], ctop[:, :, 0], -1.0, ALU.mult, r=ctk, w=["nmx"])
            for h in range(8):
                P.act(i12[:, 2, h * 16:(h + 1) * 16], ctop[:, h, :], AF.Exp, ctk + ["nmx"], [("i12g", h), ("zs", h)],
                      bias=nmx[:, h:h + 1], scale=1.0, accum_out=zs[:, h:h + 1])
            zk_ = [("zs", h) for h in range(8)]; gk_ = [("i12g", h) for h in range(8)]
            P.op("dve", lambda e: e.reciprocal(out=zs[:], in_=zs[:]), zk_, zk_)
            g3 = i12[:, 2, :].rearrange("p (h k) -> p h k", k=16)
            P.tt("dve", g3, g3, zs[:].unsqueeze(2).to_broadcast([128, 8, 16]), ALU.mult, ["i12"] + zk_ + gk_, ["i12"] + gk_)
            iT, iTk = i12T.next()
            for w_ in range(3):
                ps, pk = psA.next()
                P.tr(ps[:, 0:128], i12[:, w_, :], self.identf[:], ["i12", "identf"] + gk_, [pk])
                P.cp("act", iT[:, w_, :], ps[:, 0:128], [pk], [iTk])
            P.dma("sp", self.sel[ti], iT[:], [iTk], [("sel", ti)])
        P.release(m)

    def phase_peer_g(self, l, want_ctx):
        P = self.P
        m = P.mark()
        i12T = Rot(P, "i12T", 2, [128, 3, 128], F32)
        i12Tb = Rot(P, "i12Tb", 2, [128, 3, 128], BF16)
        Aoh = Rot(P, "Aoh", 2, [128, 64, 128], BF16)
        Boh = Rot(P, "Boh", 2, [128, 64, 128], BF16)
        GTs = Rot(P, "GTs", 2, [128, 128, 128], BF16)
        ni2r = Rot(P, "ni2", 2, [128, 128], F32)
        abt = Rot(P, "abt", 4, [128, 128], F32)
        NB_DVE = 64
        psA = Rot(P, "psG", 4, [128, 512], F32, psum=True)
        for ti in range(0 if want_ctx else 2, NT):
            iT, iTk = i12T.next()
            P.dma("sp", iT[:], self.sel[ti], (), [iTk])
            G_, Gk = GTs.next()
            ni2, nik = ni2r.next()
            P.ts("dve", ni2[:], iT[:, 1, :], -1.0, ALU.mult, r=[iTk], w=[nik])
            for g64 in range(2):
                A_, Ak = Aoh.next(); B_, Bk = Boh.next()
                for tt_ in range(64):
                    t = g64 * 64 + tt_
                    P.ts("dve", A_[:, tt_, :], self.iotab[:], iT[:, 0, t:t + 1], ALU.is_equal, iT[:, 2, t:t + 1], ALU.mult,
                         r=[iTk, "iotab"], w=[(Ak, tt_)])
                    if tt_ < NB_DVE:
                        P.ts("dve", B_[:, tt_, :], self.iotab[:], iT[:, 1, t:t + 1], ALU.is_equal, r=[iTk, "iotab"], w=[(Bk, tt_)])
                    else:
                        a_t, atk = abt.next()
                        P.act(a_t[:], self.iotaf[:], AF.Abs, ["iotaf", nik], [atk], bias=ni2[:, t:t + 1], scale=1.0)
                        P.act(B_[:, tt_, :], a_t[:], AF.Relu, [atk], [(Bk, tt_)], bias=1.0, scale=-1.0)
                for t4 in range(16):
                    ps, pk = psA.next()
                    for tq in range(4):
                        tt_ = t4 * 4 + tq
                        P.mm(ps[:, tq * 128:(tq + 1) * 128], B_[:, tt_, :], A_[:, tt_, :], True, True,
                             [(Ak, tt_), (Bk, tt_)], [pk])
                    c0 = g64 * 64 + t4 * 4
                    dst = G_[:, :, c0:c0 + 4].rearrange("p c t -> p t c")
                    P.cp("act", dst, ps[:, :].rearrange("p (t c) -> p t c", c=128), [pk], [Gk])
            P.dma("sp", self.GTd[ti // 34][ti % 34], G_[:], [Gk], ())
        P.release(m)

    def phase_peer(self, l, want_ctx):
        P, I = self.P, self.I
        m = P.mark()
        NJ = 3
        TB = 128 * NJ
        self.load_gate(l, 2)
        h2 = Rot(P, "h2", 2, [128, 8, TB], BF16)
        uv = Rot(P, "uv", 8, [128, 2048], BF16)
        gt = Rot(P, "gt", 3, [128, NJ, 8, 128], BF16)
        ga = Rot(P, "ga", 3, [128, TB], BF16)
        wt = Rot(P, "wt", 3, [128, TB], BF16)
        psA = Rot(P, "psA", 2, [128, 512], F32, psum=True)
        psV = P.ps("psV", [128, 2 * NJ, 512], F32)
        for b in range(T // TB):
            t0 = b * TB
            h_t, hk = h2.next()
            P.dma("sp", h_t[:], self.h2T[:, t0:t0 + TB].rearrange("(kc p) t -> p kc t", p=128), (), [hk])
            for c in range(128):
                if c % 8 == 0:
                    g_t8, g8k = gt.next()
                    for j in range(NJ):
                        ti = NJ * b + j
                        P.dma("sp", g_t8[:, j, :, :], self.GTd[ti // 34][ti % 34, :, c:c + 8, :], (), [g8k])
                u_t, uk = uv.next()
                P.dma("sp", u_t[:], self.UV[c], [("UV", c)], [uk])
                ps, pk = psA.next()
                for kc in range(8):
                    P.mm(ps[:, 0:TB], u_t[:, kc * 128:(kc + 1) * 128], h_t[:, kc, :], kc == 0, kc == 7, [uk, hk], [pk])
                g_t, gk = ga.next()
                P.act(g_t[:], ps[:, 0:TB], AF.Gelu, [pk], [gk])
                w_t, wk = wt.next()
                P.tt("dve", w_t[:].rearrange("p (j t) -> p j t", j=NJ), g_t[:].rearrange("p (j t) -> p j t", j=NJ),
                     g_t8[:, :, c % 8, :], ALU.mult, [gk, g8k], [wk])
                for j in range(NJ):
                    for hf in range(2):
                        P.mm(psV[:, j * 2 + hf, :], w_t[:, j * 128:(j + 1) * 128], u_t[:, 1024 + hf * 512:1024 + (hf + 1) * 512],
                             c == 0, c == 127, [wk, uk], [("psV", j * 2 + hf)])
            for j in range(NJ):
                ti = t0 // 128 + j
                s_ = 0 if ti >= 2 else 1
                xt, xk = self.xt.next()
                P.dma("sp", xt[:], self.xres[ti * 128:(ti + 1) * 128, :], (), [xk])
                tf, tk = self.tmpf.next()
                P.tt("dve", tf[:], psV[:, 2 * j:2 * j + 2, :].rearrange("p a n -> p (a n)"), self.modG[s_][:], ALU.mult,
                     [("psV", 2 * j), ("psV", 2 * j + 1), ("modG", s_)], [tk])
                P.tt("dve", tf[:], tf[:], xt[:], ALU.add, [tk, xk], [tk])
                P.dma("sp", self.xres[ti * 128:(ti + 1) * 128, :], tf[:], [tk], [("x", ti)])
        P.release(m)

    def phase_final(self):
        P, I = self.P, self.I
        m = P.mark()
        fg = P.sb("fg", [128, D], F32)
        P.dma("sp", fg[:], I["final_g"][0:1, :].to_broadcast([128, D]), w=["fg"])
        ot = Rot(P, "ot", 2, [128, D], F32)
        for ti in range(2, NT):
            xt, xk = self.xt.next()
            P.dma("sp", xt[:], self.xres[ti * 128:(ti + 1) * 128, :], (), [xk])
            r, rk = self.rstd(xt[:], D, xk)
            o, ok = ot.next()
            P.stt(o[:], xt[:], r, fg[:], ALU.mult, ALU.mult, [xk, rk, "fg"], [ok])
            P.dma("sp", self.out[(ti - 2) * 128:(ti - 1) * 128, :], o[:], [ok], ["out"])
        P.release(m)


def build(cfg):
    k = KE(cfg)
    P = k.P
    if cfg.get("scopes"):
        for nm in [n for n in dir(k) if n.startswith("phase_") and n != "phase_even"]:
            def wrap(f, nm=nm):
                def g(*a, **kw):
                    with P.nc.named_scope(nm + "_" + str(a[0] if a else "")):
                        return f(*a, **kw)
                return g
            setattr(k, nm, wrap(getattr(k, nm)))
    layers = cfg["layers"]
    stop_after = cfg.get("stop_after")
    k.phase_init()
    k.phase_ada()
    P.barrier()

    def dump(name, src):
        if name in k.dbg:
            P.barrier()
            n = src.shape[0]
            for r0 in range(0, n, 1024):
                r1 = min(n, r0 + 1024)
                P.dma("sp", k.dbg[name][r0:r1, :], src[r0:r1, :], (), ["dbgout"])
            P.barrier()

    done = False
    for l in layers:
        want_ctx = l < DEPTH - 1
        k.phase_peer_prep(l)
        if l % 2 == 0:
            k.phase_even(l, want_ctx)
            wout = k.I["even_w_out"][l // 2]
        else:
            k.phase_mla_proj(l)
            k.phase_mla_attn(l, want_ctx)
            wout = k.I["mla_w_out"][l // 2]
        dump("attn%d" % l, k.attn)
        if stop_after == ("attn", l):
            done = True; break
        k.phase_outproj(l, wout, want_ctx)
        dump("xm%d" % l, k.xres)
        if stop_after == ("outproj", l):
            done = True; break
        k.phase_peer_sel(l, True)
        k.phase_peer_g(l, True)
        k.phase_peer(l, True)
        dump("x%d" % l, k.xres)
    if not done:
        k.phase_final()
    P.barrier()
    return P.finish([]), k


def rope_tables():
    half = 32
    inv = 10000.0 ** (-np.arange(0, half, 2, dtype=np.float32) / half)
    t = np.arange(SEQ)
    row = (t // 64).astype(np.float32); col = (t % 64).astype(np.float32)
    ar = row[:, None] * inv[None, :]; ac = col[:, None] * inv[None, :]
    ang = np.concatenate([ar, ar, ac, ac], axis=-1).astype(np.float32)
    cosT = np.ones((64, T), np.float32); sinT = np.zeros((64, T), np.float32)
    cosT[:, CTX:] = np.cos(ang).T; sinT[:, CTX:] = np.sin(ang).T
    return cosT, sinT


def na_tables(rpb):
    q = np.arange(64); kc = np.arange(64)
    dc = np.clip(kc[:, None] - q[None, :], -15, 15) + 15
    g = rpb[:, :, :, dc]
    g = np.ascontiguousarray(np.transpose(g, (0, 3, 2, 1, 4))).astype(np.float32)
    cstart = np.clip(q - 8, 0, 48)
    ok = (kc[:, None] >= cstart[None, :]) & (kc[:, None] < cstart[None, :] + 16)
    mask = np.where(ok, 0.0, NEG).astype(np.float32)
    return g, mask


def host_inputs(inp, b):
    f = lambda a: np.ascontiguousarray(np.asarray(a, dtype=np.float32))
    cosT, sinT = rope_tables()
    rpbg, namask = na_tables(np.asarray(inp["na_rpb"], np.float32))
    c2 = np.stack([np.asarray(inp["c"][b]).reshape(8, 128).T, np.asarray(inp["c_ctx"]).reshape(8, 128).T], axis=-1)
    m = {
        "xin": f(np.concatenate([inp["ctx"][b], inp["x"][b]], axis=0)),
        "cond2": f(c2),
        "cosT": cosT, "sinT": sinT, "rpbg": rpbg, "namask": namask,
        "final_g": f(np.asarray(inp["final_g"]).reshape(1, D)),
        "conv_wT": f(np.transpose(np.asarray(inp["even_conv_w"]).reshape(2, 5, 12, 128), (0, 2, 3, 1))),
        "gdn_ab": f(np.stack([np.asarray(inp["gdn_a_log"]).reshape(2, 8), np.asarray(inp["gdn_dt_bias"]).reshape(2, 8)], axis=1)),
    }
    return m


_SHARED = None


def shared_inputs(inp):
    f = lambda a: np.ascontiguousarray(np.asarray(a, dtype=np.float32))
    sk = np.asarray(inp["peer_sub_keys"], np.float32).reshape(DEPTH, 16, 128, 128)
    m = {k: f(inp[k]) for k in ("ada_w", "ada_b", "norm1_g", "norm2_g", "mla_w_in", "mla_q_g", "mla_kv_g", "mla_w_uq",
                                "mla_w_ukv", "mla_w_out", "peer_w_q", "peer_v", "even_w_in", "even_w_out", "gdn_norm_g")}
    m["skT"] = f(np.transpose(sk, (0, 3, 1, 2)))
    m["peer_uT"] = f(np.transpose(np.asarray(inp["peer_u"], np.float32), (0, 2, 1)))
    return m


def kernel(**inputs):
    cfg = {"layers": [0, 1, 2, 3]}
    nc, k = build(cfg)
    sh = shared_inputs(inputs)
    in_maps = []
    for b in range(8):
        m = dict(sh); m.update(host_inputs(inputs, b))
        in_maps.append(m)
    res = run_bass_kernel_spmd(nc, in_maps, core_ids=list(range(8)))
    return np.stack([np.asarray(r["out"], np.float32) for r in res.results], axis=0)


class KE(KM):
    def phase_even_proj(self, l):
        P, I = self.P, self.I
        i = l // 2
        m = P.mark()
        if not hasattr(self, "qaT"):
            self.qaT = P.dram("qaT", [8, 64, T], BF16)
            self.kaT = P.dram("kaT", [8, 64, T], BF16)
            self.va = P.dram("va", [T, 8, 65], BF16)
            self.gpre = P.dram("gpre", [12, 128, T], F32)
            self.zd = P.dram("zd", [T, 512], F32)
            self.grd = P.dram("grd", [T, 16], F32)
        Win = P.sb("WinE", [128, 8, 3600], BF16)
        wv = I["even_w_in"][i].rearrange("(kc p) n -> p kc n", p=128)
        for kc in range(8):
            P.dma("pool", Win[:, kc, :], wv[:, kc, :], w=["Win"])
        self.load_AS(l, 1)
        pst = Rot(P, "pst", 2, [128, 8, 128], BF16, psum=True)
        psu = Rot(P, "psu", 4, [128, 512], F32, psum=True)
        hTr = Rot(P, "hT", 2, [128, 8, 512], BF16)
        stq = Rot(P, "stq", 2, [64, 8, 512], BF16)
        stk = Rot(P, "stk", 2, [64, 8, 512], BF16)
        stg = Rot(P, "stg", 3, [128, 512], F32)
        vsb = Rot(P, "vsbE", 2, [128, 8, 65], BF16)
        for t_ in vsb.t:
            P.memset("pool", t_[:, :, 64:65], 1.0, w=[(vsb.name, vsb.t.index(t_))])
        zsb = Rot(P, "zsbE", 2, [128, 512], F32)
        gsb = Rot(P, "gsbE", 2, [128, 16], F32)
        for (t0, n) in blocks(True):
            s = 0 if t0 >= CTX else 1
            nt = n // 128
            hT, hk = hTr.next()
            for j in range(nt):
                ti = t0 // 128 + j
                xt, xk = self.xt.next()
                P.dma("sp", xt[:], self.xres[ti * 128:(ti + 1) * 128, :], (), [xk])
                self.norm_mod_T(xt[:], xk, s, hT[:, :, j * 128:(j + 1) * 128], hk, pst)
            sq, sqk = stq.next(); sk_, skk = stk.next()
            for h in range(8):
                for (dst, dk, c0) in ((sq, sqk, 0), (sk_, skk, 512)):
                    ps, pk = psu.next()
                    for kc in range(8):
                        P.mm(ps[0:64, 0:n], Win[:, kc, c0 + h * 64:c0 + (h + 1) * 64], hT[:, kc, 0:n], kc == 0, kc == 7, ["Win", hk], [pk])
                    P.cp("act", dst[:, h, 0:n], ps[0:64, 0:n], [pk], [dk])
            P.dma("sp", self.qaT[:, :, t0:t0 + n].rearrange("h p t -> p h t"), sq[:, :, 0:n], [sqk], ())
            P.dma("sp", self.kaT[:, :, t0:t0 + n].rearrange("h p t -> p h t"), sk_[:, :, 0:n], [skk], ())
            for c in range(12):
                ps, pk = psu.next()
                for kc in range(8):
                    P.mm(ps[:, 0:n], Win[:, kc, 1536 + c * 128:1536 + (c + 1) * 128], hT[:, kc, 0:n], kc == 0, kc == 7, ["Win", hk], [pk])
                sg, sgk = stg.next()
                P.cp("act" if c % 2 else "dve", sg[:, 0:n], ps[:, 0:n], [pk], [sgk])
                P.dma("sp", self.gpre[c, :, t0:t0 + n], sg[:, 0:n], [sgk], ())
            for j in range(nt):
                ti = t0 // 128 + j
                tsl = slice(j * 128, (j + 1) * 128)
                ps, pk = psu.next()
                for kc in range(8):
                    P.mm(ps[:, :], hT[:, kc, tsl], Win[:, kc, 1024:1536], kc == 0, kc == 7, ["Win", hk], [pk])
                v_, vk = vsb.next()
                P.cp("act", v_[:, :, 0:64], ps[:, :].rearrange("p (h d) -> p h d", d=64), [pk], [vk])
                P.dma("sp", self.va[ti * 128:(ti + 1) * 128, :, :], v_[:], [vk], ())
                ps, pk = psu.next()
                for kc in range(8):
                    P.mm(ps[:, :], hT[:, kc, tsl], Win[:, kc, 3072:3584], kc == 0, kc == 7, ["Win", hk], [pk])
                z_, zk = zsb.next()
                P.cp("dve", z_[:], ps[:, :], [pk], [zk])
                P.dma("sp", self.zd[ti * 128:(ti + 1) * 128, :], z_[:], [zk], ())
                ps, pk = psu.next()
                for kc in range(8):
                    P.mm(ps[:, 0:16], hT[:, kc, tsl], Win[:, kc, 3584:3600], kc == 0, kc == 7, ["Win", hk], [pk])
                g_, gk = gsb.next()
                P.cp("act", g_[:], ps[:, 0:16], [pk], [gk])
                P.dma("sp", self.grd[ti * 128:(ti + 1) * 128, :], g_[:], [gk], ())
        P.release(m)

    def phase_na(self, l, want_ctx):
        P, I = self.P, self.I
        i = l // 2
        m = P.mark()
        scale = 64.0 ** -0.5
        BB = P.sb("BB", [64, 15, 8, 64], F32)
        msk = P.sb("msk", [64, 64], F32)
        P.dma("sp", BB[:], I["rpbg"][i], w=["BB"])
        P.dma("sp", msk[:], I["namask"][:, :], w=["msk"])
        for dr in range(15):
            P.tt("pool", BB[:, dr, :, :], BB[:, dr, :, :], msk[:].unsqueeze(1).to_broadcast([64, 8, 64]), ALU.add, ["BB", "msk"], ["BB"])
            P.ts("pool", BB[:, dr, :, :], BB[:, dr, :, :], 1.0 / scale, ALU.mult, r=["BB"], w=["BB"])
        kcT = P.sb("kcT", [64, 8, 256], BF16)
        vcx = P.sb("vcx", [128, 2, 8, 65], BF16)
        P.dma("sp", kcT[:], self.kaT[:, :, 0:256].rearrange("h p t -> p h t"), w=["kcT"])
        P.dma("sp", vcx[:], self.va[0:256, :, :].rearrange("(kt p) h d -> p kt h d", p=128), (), ["vcx"])
        m2 = P.mark()
        kw = Rot(P, "kw", 2, [64, 8, 1024], BF16)
        vw = Rot(P, "vw", 2, [64, 16, 8, 65], BF16)
        qb = Rot(P, "qb", 2, [64, 8, 512], BF16)
        psS = Rot(P, "psS", 2, [64, 2, 8, 64], F32, psum=True)
        psC = Rot(P, "psC", 2, [128, 2, 2, 64], F32, psum=True)
        psO = Rot(P, "psO", 1, [128, 2, 65], F32, psum=True)
        stmp = Rot(P, "stmp", 2, [64, 2, 8, 64], F32)
        pb = Rot(P, "pb", 2, [64, 2, 8, 64], BF16)
        pcb = Rot(P, "pcb", 2, [128, 2, 2, 64], BF16)
        yo = Rot(P, "yo", 1, [64, 8, 8, 64], F32)
        for b8 in range(16):
            w0 = min(max(8 * b8 - 4, 0), 112)
            k_w, kk = kw.next(); v_w, vk = vw.next(); q_b, qk = qb.next()
            tk0 = CTX + w0 * 64
            P.dma("sp", k_w[:], self.kaT[:, :, tk0:tk0 + 1024].rearrange("h p t -> p h t"), (), [kk])
            P.dma("sp", v_w[:], self.va[tk0:tk0 + 1024, :, :].rearrange("(r p) h d -> p r h d", p=64), (), [vk])
            tq0 = CTX + b8 * 512
            P.dma("sp", q_b[:], self.qaT[:, :, tq0:tq0 + 512].rearrange("h p t -> p h t"), (), [qk])
            y_o, yk = yo.next()
            for rr in range(8):
                r = 8 * b8 + rr
                rs = min(max(r - 4, 0), 120)
                dr0 = rs - r + 7
                for hp2 in range(4):
                    ps, pk = psS.next()
                    pc, pck = psC.next()
                    for hh in range(2):
                        h = 2 * hp2 + hh
                        for i8 in range(8):
                            kr = rs + i8 - w0
                            P.mm(ps[:, hh, i8, :], k_w[:, h, kr * 64:(kr + 1) * 64], q_b[:, h, rr * 64:(rr + 1) * 64], True, True,
                                 [kk, qk], [pk])
                        for ct in range(2):
                            P.mm(pc[:, ct, hh, :], kcT[:, h, ct * 128:(ct + 1) * 128], q_b[:, h, rr * 64:(rr + 1) * 64], True, True,
                                 ["kcT", qk], [pck])
                    st, stk_ = stmp.next()
                    bias = BB[:, dr0:dr0 + 8, 2 * hp2:2 * hp2 + 2, :].rearrange("p r h q -> p h r q")
                    P.tt("dve", st[:], ps[:], bias, ALU.add, [pk, "BB"], [stk_])
                    p_b, pbk = pb.next()
                    P.act(p_b[:], st[:], AF.Exp, [stk_], [pbk], scale=scale)
                    p_c, pcbk = pcb.next()
                    P.act(p_c[:], pc[:], AF.Exp, [pck], [pcbk], scale=scale)
                    po, pok = psO.next()
                    for hh in range(2):
                        h = 2 * hp2 + hh
                        for i8 in range(8):
                            kr = rs + i8 - w0
                            P.mm(po[0:64, hh, :], p_b[:, hh, i8, :], v_w[:, kr, h, :], i8 == 0, False, [pbk, vk], [pok])
                        for ct in range(2):
                            P.mm(po[0:64, hh, :], p_c[:, ct, hh, :], vcx[:, ct, h, :], False, ct == 1, [pcbk, "vcx"], [pok])
                    sm, smk = self.small.next()
                    P.op("dve", lambda e, sm=sm, po=po: e.reciprocal(out=sm[0:64, 0:2], in_=po[0:64, :, 64]), [pok], [smk])
                    P.tt("dve", y_o[:, rr, 2 * hp2:2 * hp2 + 2, :], po[0:64, :, 0:64], sm[0:64, 0:2].unsqueeze(2).to_broadcast([64, 2, 64]),
                         ALU.mult, [pok, smk], [yk])
            P.dma("sp", self.attn[tq0:tq0 + 512, 0:512].rearrange("(r p) (h d) -> p r h d", p=64, d=64), y_o[:], [yk], ())
        P.release(m2)
        if want_ctx:
            psO = Rot(P, "psO", 1, [128, 2, 65], F32, psum=True)
            qc = P.sb("qc", [64, 8, 256], BF16)
            P.dma("sp", qc[:], self.qaT[:, :, 0:256].rearrange("h p t -> p h t"), w=["qc"])
            psX = Rot(P, "psX", 1, [128, 2, 256], F32, psum=True)
            pxb = Rot(P, "pxb", 2, [128, 2, 256], BF16)
            yc = P.sb("yc", [128, 2, 8, 64], F32)
            for h in range(8):
                ps, pk = psX.next()
                for kt in range(2):
                    P.mm(ps[:, kt, :], kcT[:, h, kt * 128:(kt + 1) * 128], qc[:, h, :], True, True, ["kcT", "qc"], [pk])
                px, pxk = pxb.next()
                P.act(px[:], ps[:], AF.Exp, [pk], [pxk], scale=scale)
                po, pok = psO.next()
                for qt in range(2):
                    for kt in range(2):
                        P.mm(po[:, qt, :], px[:, kt, qt * 128:(qt + 1) * 128], vcx[:, kt, h, :], kt == 0, kt == 1, [pxk, "vcx"], [pok])
                sm, smk = self.small.next()
                P.op("dve", lambda e, sm=sm, po=po: e.reciprocal(out=sm[:, 0:2], in_=po[:, :, 64]), [pok], [smk])
                P.tt("dve", yc[:, :, h, :], po[:, :, 0:64], sm[:, 0:2].unsqueeze(2).to_broadcast([128, 2, 64]), ALU.mult, [pok, smk], ["yc"])
            P.dma("sp", self.attn[0:256, 0:512].rearrange("(qt p) (h d) -> p qt h d", p=128, d=64), yc[:], ["yc"], ())
        P.release(m)

    def phase_gdn_pre(self, l):
        P, I = self.P, self.I
        i = l // 2
        m = P.mark()
        if not hasattr(self, "gT"):
            self.gT = P.dram("gT", [12, 128, T], F32)
            self.od = P.dram("od", [2, T, 512], F32)
        cw = P.sb("cw", [128, 12, 5], F32)
        P.dma("sp", cw[:], I["conv_wT"][i].rearrange("c p k -> p c k"), w=["cw"])
        onesf = P.sb("onesf", [128, 128], F32)
        P.memset("pool", onesf[:], 1.0, w=["onesf"])
        NP = 2048
        xp = Rot(P, "xp", 2, [128, NP + 4], F32)
        acc = Rot(P, "acc", 2, [128, NP], F32)
        yv = Rot(P, "yv", 2, [128, NP], F32)
        sq = Rot(P, "sq", 2, [128, NP], F32)
        rn = Rot(P, "rn", 2, [128, 512], F32)
        psn = Rot(P, "psn", 2, [128, 512], F32, psum=True)
        pieces = [(0, 256, 0, 256)] + [(256 + NP * k, 256 + NP * (k + 1), 256, T) for k in range(SEQ // NP)]
        for c in range(12):
            for (a, b, lo_, hi_) in pieces:
                n = b - a
                x_, xk = xp.next()
                lo = max(a - 2, lo_); hi = min(b + 2, hi_)
                if lo > a - 2:
                    P.memset("pool", x_[:, 0:2], 0.0, w=[xk])
                if hi < b + 2:
                    P.memset("pool", x_[:, n + 2:n + 4], 0.0, w=[xk])
                P.dma("sp", x_[:, lo - (a - 2):hi - (a - 2)], self.gpre[c, :, lo:hi], (), [xk])
                a_, ak = acc.next()
                P.ts("dve", a_[:, 0:n], x_[:, 0:n], cw[:, c, 0:1], ALU.mult, r=[xk, "cw"], w=[ak])
                for k in range(1, 5):
                    P.stt(a_[:, 0:n], x_[:, k:k + n], cw[:, c, k:k + 1], a_[:, 0:n], ALU.mult, ALU.add, [xk, "cw", ak], [ak])
                y_, yk = yv.next()
                P.act(y_[:, 0:n], a_[:, 0:n], AF.Silu, [ak], [yk])
                if c < 8:
                    s_, sk = sq.next()
                    P.act(s_[:, 0:n], y_[:, 0:n], AF.Square, [yk], [sk])
                    for q0 in range(0, n, 512):
                        w_ = min(512, n - q0)
                        ps, pk = psn.next()
                        P.mm(ps[:, 0:w_], onesf[:], s_[:, q0:q0 + w_], True, True, ["onesf", sk], [pk])
                        r_, rk = rn.next()
                        mul = 128.0 if c < 4 else 1.0
                        P.act(r_[:, 0:w_], ps[:, 0:w_], AF.Sqrt, [pk], [rk], scale=mul, bias=mul * EPS)
                        P.op("dve", lambda e, r_=r_, w_=w_: e.reciprocal(out=r_[:, 0:w_], in_=r_[:, 0:w_]), [rk], [rk])
                        P.tt("dve", y_[:, q0:q0 + w_], y_[:, q0:q0 + w_], r_[:, 0:w_], ALU.mult, [yk, rk], [yk])
                P.dma("sp", self.gT[c, :, a:b], y_[:, 0:n], [yk], ())
        P.release(m)

    def phase_gdn(self, l):
        P, I = self.P, self.I
        i = l // 2
        m = P.mark()
        sbc = lambda n: P.sb(n, [128, 128], F32)
        Ball = sbc("Ball"); P.memset("pool", Ball[:], 0.0, w=["Ball"])
        P.memset("pool", Ball[0:64, 0:64], 1.0, w=["Ball"]); P.memset("pool", Ball[64:128, 64:128], 1.0, w=["Ball"])
        INf = sbc("INf"); INb = sbc("INb"); STf = sbc("STf"); STb = sbc("STb")
        for (dst, coef, cm, base) in ((INf, -1, 1, 0), (INb, 1, -1, 0), (STf, -1, 1, -1), (STb, 1, -1, -1)):
            P.op("pool", lambda e, dst=dst, coef=coef, cm=cm, base=base: e.affine_select(
                out=dst[:], in_=Ball[:], pattern=[[coef, 128]], compare_op=ALU.is_ge, fill=0.0, base=base, channel_multiplier=cm),
                ["Ball"], [dst.name])
        MNf = sbc("MNf"); MNb = sbc("MNb")
        P.ts("dve", MNf[:], INf[:], -1.0, ALU.add, -NEG, ALU.mult, r=[INf.name], w=["MNf"])
        P.ts("dve", MNb[:], INb[:], -1.0, ALU.add, -NEG, ALU.mult, r=[INb.name], w=["MNb"])
        CM = [sbc("CM0"), sbc("CM1")]
        BM = [sbc("BM0"), sbc("BM1")]
        SEL = [sbc("SEL0"), sbc("SEL1")]
        for c in range(2):
            for t_ in (CM[c], BM[c], SEL[c]):
                P.memset("pool", t_[:], 0.0, w=[t_.name])
            P.memset("pool", CM[c][:, c * 64:(c + 1) * 64], 1.0, w=[CM[c].name])
            P.memset("pool", BM[c][c * 64:(c + 1) * 64, c * 64:(c + 1) * 64], 1.0, w=[BM[c].name])
            P.memset("pool", SEL[c][c * 64:(c + 1) * 64, :], 1.0 / 64, w=[SEL[c].name])
        cnames = [Ball.name, INf.name, INb.name, STf.name, STb.name, "MNf", "MNb"] + [t_.name for t_ in CM + BM + SEL]
        MN = [MNf, MNb]; ST = [STf, STb]
        Lc = [INb, INf]
        ab = P.sb("ab", [128, 2, 8], F32)
        P.dma("sp", ab[:], I["gdn_ab"][i:i + 1, :, :].to_broadcast([128, 2, 8]), w=["ab"])
        negA = P.sb("negA", [128, 8], F32)
        P.act(negA[:], ab[:, 0, :], AF.Exp, ["ab"], ["negA"])
        P.ts("dve", negA[:], negA[:], -1.0, ALU.mult, r=["negA"], w=["negA"])

        psg = Rot(P, "psg", 8, [128, 128], F32, psum=True)
        grt = Rot(P, "grt", 2, [128, 16], F32)
        gtmp = Rot(P, "gtmp", 3, [128, 16], F32)
        gsc = Rot(P, "gsc", 6, [128, 6, 8], F32)
        bnd = Rot(P, "bnd", 6, [128, 2, 8], F32)

        def gates(ti):
            g_, gk = grt.next()
            P.dma("sp", g_[:], self.grd[ti * 128:(ti + 1) * 128, :], (), [gk])
            g4 = g_[:].rearrange("p (d k h) -> p d k h", d=2, k=2)
            t_, tk = gtmp.next()
            t4 = t_[:].rearrange("p (d k h) -> p d k h", d=2, k=2)
            sc, sk = gsc.next()
            P.tt("dve", t4[:, :, 0, :], g4[:, :, 0, :], ab[:, 1, :].rearrange("p (d h) -> p d h", d=2), ALU.add, [gk, "ab"], [tk])
            P.act(t4[:, :, 0, :], t4[:, :, 0, :], AF.Exp, [tk], [tk])
            P.act(t4[:, :, 0, :], t4[:, :, 0, :], AF.Ln, [tk], [tk], bias=1.0)
            P.tt("dve", t4[:, :, 0, :], t4[:, :, 0, :], negA[:].rearrange("p (d h) -> p d h", d=2), ALU.mult, [tk, "negA"], [tk])
            P.act(sc[:, 4, :].rearrange("p (d h) -> p d h", d=2), g4[:, :, 1, :], AF.Sigmoid, [gk], [sk])
            gg = t_[:].rearrange("p (d k h) -> p d k h", d=2, k=2)
            gcomp, gck = gtmp.next()
            P.cp("dve", gcomp[:, 0:8].rearrange("p (d h) -> p d h", d=2), gg[:, :, 0, :], [tk], [gck])
            ps, pk = psg.t[0], (psg.name, 0)
            P.mm(ps[:, 0:4], Lc[0][:], gcomp[:, 0:4], True, True, [Lc[0].name, gck], [pk])
            P.mm(ps[:, 4:8], Lc[1][:], gcomp[:, 4:8], True, True, [Lc[1].name, gck], [pk])
            P.mm(ps[:, 8:16], Ball[:], gcomp[:, 0:8], True, True, [Ball.name, gck], [pk])
            gps_sb, gpk = gtmp.next()
            P.cp("act", gps_sb[:], ps[:, 0:16], [pk], [gpk])
            P.cp("act", sc[:, 0, :], gps_sb[:, 0:8], [gpk], [sk])
            P.ts("dve", sc[:, 1, :], gps_sb[:, 0:8], -1.0, ALU.mult, r=[gpk], w=[sk])
            P.act(sc[:, 2, :], gps_sb[:, 0:8], AF.Exp, [gpk], [sk])
            P.tt("dve", sc[:, 5, :], gps_sb[:, 8:16], sc[:, 0, :], ALU.subtract, [gpk, sk], [sk])
            P.act(sc[:, 5, :], sc[:, 5, :], AF.Exp, [sk], [sk])
            P.stt(sc[:, 3, :], sc[:, 2, :], -1.0, sc[:, 4, :], ALU.mult, ALU.mult, [sk], [sk])
            P.cp("act", gcomp[:, 8:16], gps_sb[:, 8:16], [gpk], [gck])
            ps2, pk2 = psg.t[1], (psg.name, 1)
            for c in range(2):
                P.mm(ps2[:, c * 8:(c + 1) * 8], SEL[c][:], gcomp[:, 8:16], True, True, [SEL[c].name, gck], [pk2])
            b_, bk = bnd.next()
            P.act(b_[:].rearrange("p c e -> p (c e)"), ps2[:, 0:16], AF.Exp, [pk2], [bk])
            return sc, sk, b_, bk

        NCH = 8
        wk4 = [[P.sb("gwk%d_%d" % (ch, j), [128, 128], F32) for j in range(4)] for ch in range(NCH)]
        PAs = [[P.sb("PA%d_%d" % (ch, j), [128, 128], F32) for j in range(2)] for ch in range(NCH)]
        PTs = [[P.sb("PT%d_%d" % (ch, j), [128, 128], F32) for j in range(2)] for ch in range(NCH)]
        TTs = [P.sb("TT%d" % ch, [128, 128], F32) for ch in range(NCH)]
        lds = [P.sb("ld%d" % ch, [128, 3, 128], F32) for ch in range(NCH)]
        prep = [[P.sb("prep%d_%d" % (ch, j), [128, 9, 128], F32) for j in range(2)] for ch in range(NCH)]
        prep_i = [0] * NCH
        Sst = [[P.sb("S%d_%d" % (ch, j), [128, 128], F32) for j in range(2)] for ch in range(NCH)]
        S_i = [0] * NCH
        for ch in range(NCH):
            P.memset("pool", Sst[ch][0][:], 0.0, w=[("S", ch, 0)])
        rr_ = [P.sb("rr%d" % ch, [128, 128], F32) for ch in range(NCH)]
        uu = [P.sb("uu%d" % ch, [128, 128], F32) for ch in range(NCH)]
        osb = [P.sb("gosb%d" % ch, [128, 128], F32) for ch in range(NCH)]

        def prep_tile(ti, h, d, sc, sk):
            ch = d * 4 + h; c8 = ch
            j = prep_i[ch]; prep_i[ch] ^= 1
            pr = prep[ch][j]; prk = ("prep", ch, j)
            return pr, prk, prep_gen(ti, h, d, sc, sk, ch, pr, prk)

        free_ps = list(zip(psg.t, [(psg.name, j) for j in range(psg.n)]))

        def grab(n):
            while len(free_ps) < n:
                yield
            return [free_ps.pop(0) for _ in range(n)]

        def give(*tiles):
            free_ps.extend(tiles)

        def prep_gen(ti, h, d, sc, sk, ch, pr, prk):
            c8 = ch
            ld, ldk = lds[ch], ("ld", ch)
            for w_, cc in enumerate((h, 4 + h, 8 + h)):
                P.dma("sp", ld[:, w_, :], self.gT[cc, :, ti * 128:(ti + 1) * 128], (), [ldk])
            qT, kT, vT = ld[:, 0, :], ld[:, 1, :], ld[:, 2, :]
            gcol = lambda w_: sc[:, w_, c8:c8 + 1]
            (gcB, Dm, dT, EG) = wk4[ch]
            gcBk, Dmk, dTk, EGk = [("wk", ch, q) for q in range(4)]
            P.cp("pool", gcB[:], gcol(0).to_broadcast([128, 128]), [sk], [gcBk])
            yield
            (g_,) = yield from grab(1)
            Gps, Gk = g_
            P.mm(Gps[:], gcB[:], self.identf[:], True, True, [gcBk, "identf"], [Gk])
            yield
            P.stt(Dm[:], Gps[:], -1.0, MN[d][:], ALU.mult, ALU.add, [Gk, "MNf" if d == 0 else "MNb"], [Dmk])
            P.tt("dve", dT[:], Gps[:], MN[1 - d][:], ALU.add, [Gk, "MNf" if d == 1 else "MNb"], [dTk])
            P.act(EG[:], Gps[:], AF.Exp, [Gk, Dmk, dTk], [EGk])
            give(g_)
            yield
            P.act(Dm[:], Dm[:], AF.Exp, [Dmk, sk], [Dmk], bias=gcol(0))
            P.act(dT[:], dT[:], AF.Exp, [dTk, sk], [dTk], bias=gcol(1))
            yield
            P.tt("pool", Dm[:], Dm[:], ST[d][:], ALU.mult, [Dmk, ST[d].name], [Dmk])
            (g_,) = yield from grab(1)
            KKp, KKk = g_
            P.mm(KKp[:], kT, kT, True, True, [ldk], [KKk])
            yield
            pa = PAs[ch]; pt = PTs[ch]
            A_, Ak = pa[0], ("PA", ch, 0)
            P.stt(A_[:], KKp[:], gcol(4), Dm[:], ALU.mult, ALU.mult, [KKk, sk, Dmk], [Ak])
            give(g_)
            yield
            (g_,) = yield from grab(1)
            ATp, ATpk = g_
            P.tr(ATp[:], A_[:], self.identf[:], [Ak, "identf"], [ATpk])
            yield
            AT, ATk = pt[0], ("PT", ch, 0)
            P.cp("act", AT[:], ATp[:], [ATpk], [ATk])
            give(g_)
            yield
            TT, TTk = TTs[ch], ("TT", ch)
            P.tt("dve", TT[:], self.identf[:], AT[:], ALU.subtract, ["identf", ATk], [TTk])
            Pn, Pnk, PTn, PTnk = A_, Ak, AT, ATk
            for lev in range(5):
                gs = yield from grab(2 if lev < 4 else 1)
                p2, p2k = gs[0]
                P.mm(p2[:], PTn[:], Pn[:], True, True, [PTnk, Pnk], [p2k])
                if lev < 4:
                    p2t, p2tk = gs[1]
                    P.mm(p2t[:], Pn[:], PTn[:], True, True, [PTnk, Pnk], [p2tk])
                yield
                q_ = (lev + 1) % 2
                Pn2, Pn2k = pa[q_], ("PA", ch, q_)
                P.cp("act", Pn2[:], p2[:], [p2k], [Pn2k])
                if lev < 4:
                    PTn2, PTn2k = pt[q_], ("PT", ch, q_)
                    P.cp("dve", PTn2[:], p2t[:], [p2tk], [PTn2k])
                give(*gs)
                yield
                (g_,) = yield from grab(1)
                up, upk = g_
                P.mm(up[:], Pn2[:], TT[:], True, True, [Pn2k, TTk], [upk])
                yield
                P.tt("dve", TT[:], TT[:], up[:], ALU.add, [TTk, upk], [TTk])
                give(g_)
                Pn, Pnk = Pn2, Pn2k
                if lev < 4:
                    PTn, PTnk = PTn2, PTn2k
            yield
            for c in range(2):
                P.tt("pool", pr[:, c, :], TT[:], BM[c][:], ALU.mult, [TTk, BM[c].name], [prk])
            P.tt("pool", EG[:], EG[:], qT, ALU.mult, [EGk, ldk], [EGk])
            gs = yield from grab(3)
            (Pp, Ppk), (kp, kpk), (vp, vpk) = gs
            P.mm(Pp[:], kT, qT, True, True, [ldk], [Ppk])
            P.tr(kp[:], kT, self.identf[:], [ldk, "identf"], [kpk])
            P.tr(vp[:], vT, self.identf[:], [ldk, "identf"], [vpk])
            yield
            P.tt("dve", pr[:, 2, :], Pp[:], dT[:], ALU.mult, [Ppk, dTk], [prk])
            P.act(pr[:, 7, :], kp[:], AF.Copy, [kpk, sk], [prk], scale=gcol(5))
            P.act(pr[:, 8, :], vp[:], AF.Copy, [vpk, sk], [prk], scale=gcol(4))
            give(*gs)
            for c in range(2):
                P.tt("pool", pr[:, 3 + c, :], EG[:], CM[c][:], ALU.mult, [EGk, CM[c].name], [prk])
                P.tt("pool", pr[:, 5 + c, :], kT, CM[c][:], ALU.mult, [ldk, CM[c].name], [prk])
            yield

        def chain_gen(ti, h, d, pr, prk, sc, sk, b_, bk):
            ch = d * 4 + h; c8 = ch
            o_, ok = osb[ch], ("osb", ch)
            r_, rk = rr_[ch], ("rr", ch)
            u_, uk = uu[ch], ("uu", ch)
            order = (0, 1) if d == 0 else (1, 0)
            for n_, c in enumerate(order):
                si = S_i[ch]; S_i[ch] ^= 1
                S = Sst[ch][si]; Sk = ("S", ch, si)
                S2 = Sst[ch][si ^ 1]; S2k = ("S", ch, si ^ 1)
                (g_,) = yield from grab(1)
                ks, ksk = g_
                P.mm(ks[:], pr[:, 5 + c, :], S[:], True, True, [prk, Sk], [ksk])
                yield
                P.stt(r_[:], ks[:], sc[:, 3, c8:c8 + 1], pr[:, 8, :], ALU.mult, ALU.add, [ksk, sk, prk], [rk])
                give(g_)
                yield
                (g_,) = yield from grab(1)
                up, upk = g_
                P.mm(up[:], pr[:, c, :], r_[:], True, True, [prk, rk], [upk])
                yield
                P.cp("act", u_[:], up[:], [upk], [uk])
                give(g_)
                yield
                gs = yield from grab(2)
                (op_, opk), (ds, dsk) = gs
                P.mm(op_[:], pr[:, 3 + c, :], S[:], True, False, [prk, Sk], [opk])
                P.mm(op_[:], pr[:, 2, :], u_[:], False, True, [prk, uk], [opk])
                P.mm(ds[:], pr[:, 7, :], u_[:], True, True, [prk, uk], [dsk])
                yield
                P.stt(S2[:], S[:], b_[:, c, c8:c8 + 1], ds[:], ALU.mult, ALU.add, [Sk, bk, dsk], [S2k])
                if n_ == 0:
                    P.cp("act", o_[:], op_[:], [opk], [ok])
                else:
                    P.tt("dve", o_[:], o_[:], op_[:], ALU.add, [ok, opk], [ok])
                give(*gs)
                yield
            P.dma("sp", self.od[d, ti * 128:(ti + 1) * 128, h * 128:(h + 1) * 128], o_[:], [ok], ())

        def run_rr(gens):
            gens = list(gens)
            while gens:
                alive = []
                for g in gens:
                    try:
                        next(g); alive.append(g)
                    except StopIteration:
                        pass
                gens = alive

        fwd_order = list(range(NT))
        bwd_order = [1, 0] + list(range(NT - 1, 1, -1))

        def make_preps(s_):
            tf_, tb_ = fwd_order[s_], bwd_order[s_]
            gf = gates(tf_)
            gb = gates(tb_)
            out = []
            for h in range(4):
                out.append((tf_, h, 0, prep_tile(tf_, h, 0, gf[0], gf[1]), gf))
                out.append((tb_, h, 1, prep_tile(tb_, h, 1, gb[0], gb[1]), gb))
            return out

        cur = make_preps(0)
        run_rr([p[3][2] for p in cur])
        for s_ in range(NT):
            nxt = make_preps(s_ + 1) if s_ + 1 < NT else []
            chains = [chain_gen(ti, h, d, pr[0], pr[1], g[0], g[1], g[2], g[3]) for (ti, h, d, pr, g) in cur]
            run_rr([p[3][2] for p in nxt] + chains)
            cur = nxt
        P.release(m)

    def phase_gdn_out(self, l):
        P, I = self.P, self.I
        i = l // 2
        m = P.mark()
        gng = P.sb("gng", [128, 128], F32)
        P.dma("sp", gng[:], I["gdn_norm_g"][i:i + 1, :].to_broadcast([128, 128]), w=["gng"])
        of = Rot(P, "of", 2, [128, 512], F32)
        ob = Rot(P, "ob", 2, [128, 512], F32)
        zt = Rot(P, "zt", 2, [128, 512], F32)
        yt = Rot(P, "yt", 2, [128, 512], F32)
        for ti in range(NT):
            o1, k1 = of.next(); o2, k2 = ob.next(); z_, zk = zt.next()
            P.dma("sp", o1[:], self.od[0, ti * 128:(ti + 1) * 128, :], (), [k1])
            P.dma("sp", o2[:], self.od[1, ti * 128:(ti + 1) * 128, :], (), [k2])
            P.dma("sp", z_[:], self.zd[ti * 128:(ti + 1) * 128, :], (), [zk])
            P.tt("dve", o1[:], o1[:], o2[:], ALU.add, [k1, k2], [k1])
            P.act(z_[:], z_[:], AF.Silu, [zk], [zk])
            y_, yk = yt.next()
            for h in range(4):
                hs = slice(h * 128, (h + 1) * 128)
                r, rk = self.rstd(o1[:, hs], 128, k1)
                P.stt(y_[:, hs], o1[:, hs], r, gng[:], ALU.mult, ALU.mult, [k1, rk, "gng"], [yk])
            P.tt("dve", y_[:], y_[:], z_[:], ALU.mult, [yk, zk], [yk])
            P.dma("sp", self.attn[ti * 128:(ti + 1) * 128, 512:1024], y_[:], [yk], ())
        P.release(m)

    def phase_even(self, l, want_ctx):
        self.phase_even_proj(l)
        self.phase_na(l, want_ctx)
        self.phase_gdn_pre(l)
        self.phase_gdn(l)
        self.phase_gdn_out(l)
```

```python
import numpy as np
import concourse.bass as bass
import concourse.mybir as mybir
from concourse.bass_utils import run_bass_kernel_spmd

F32 = mybir.dt.float32
BF16 = mybir.dt.bfloat16
U32 = mybir.dt.uint32
AF = mybir.ActivationFunctionType
ALU = mybir.AluOpType
AX = mybir.AxisListType

D = 1024
SEQ = 8192
CTX = 256
T = SEQ + CTX
NT = T // 128
DEPTH = 4
EPS = 1e-6
ENGS = ("pe", "act", "dve", "pool", "sp")
DMA_SEMS_PER_Q = 24
NEG = -30000.0


class Prog:
    def __init__(self):
        self.nc = bass.Bass("TRN2", target_bir_lowering=False)
        nc = self.nc
        self.E = {"pe": nc.tensor, "act": nc.scalar, "dve": nc.vector, "pool": nc.gpsimd, "sp": nc.sync}
        self.cnt = {e: 0 for e in ENGS}
        self.waited = {e: {} for e in ENGS}
        self.last_w = {}
        self.readers = {}
        self.dma_rr = {e: 0 for e in ENGS}
        self.dma_tot = {}
        self.sems = {}
        self.ctxs = []
        self.n_inst = 0
        self._semctx = set()
        self.uid = 0

    def sb(self, name, shape, dt=F32):
        self.uid += 1; name = "%s_%d" % (name, self.uid)
        c = self.nc.sbuf_tensor(name, list(shape), dt)
        t = c.__enter__(); self.ctxs.append(c)
        return t

    def ps(self, name, shape, dt=F32):
        self.uid += 1; name = "%s_%d" % (name, self.uid)
        c = self.nc.psum_tensor(name, list(shape), dt)
        t = c.__enter__(); self.ctxs.append(c)
        return t

    def psv(self, name, shape, dt=F32):
        esz = 4 if dt == F32 else 2
        per = 1
        for d in shape[1:]:
            per *= d
        nb = (per * esz + 2047) // 2048
        t = self.ps(name, [128, nb * (2048 // esz)], dt)
        v = t[0:shape[0], 0:per]
        if len(shape) > 2:
            names = " ".join("d%d" % i for i in range(1, len(shape)))
            v = v.rearrange("p (%s) -> p %s" % (names, names), **{"d%d" % i: shape[i] for i in range(2, len(shape))})
        return v

    def dram(self, name, shape, dt=F32, kind="Internal"):
        return self.nc.dram_tensor(name, list(shape), dt, kind=kind).ap()

    def _sem(self, n):
        if n not in self.sems:
            c = self.nc.semaphore(n); self.sems[n] = c.__enter__(); self.ctxs.append(c); self._semctx.add(c)
        return self.sems[n]

    def _need(self, eng, sem, val):
        w = self.waited[eng]
        if w.get(sem, 0) >= val:
            return
        w[sem] = val
        self.E[eng].wait_ge(self._sem(sem), val)

    def _deps(self, eng, reads, writes):
        for k in reads:
            lw = self.last_w.get(k)
            if lw is not None:
                self._need(eng, *lw)
        for k in writes:
            lw = self.last_w.get(k)
            if lw is not None:
                self._need(eng, *lw)
            for r in self.readers.get(k, ()):
                self._need(eng, *r)

    def _commit(self, tok, reads, writes):
        for k in reads:
            self.readers.setdefault(k, []).append(tok)
        for k in writes:
            self.last_w[k] = tok
            self.readers[k] = []

    def op(self, eng, fn, reads=(), writes=()):
        self._deps(eng, reads, writes)
        self.cnt[eng] += 1
        tok = ("S_" + eng, self.cnt[eng])
        fn(self.E[eng]).then_inc(self._sem(tok[0]), 1)
        if eng == "pe":
            self.waited[eng][tok[0]] = tok[1]
        self._commit(tok, reads, writes)
        self.n_inst += 1

    def dma(self, q, out, in_, r=(), w=(), **kw):
        reads, writes = r, w
        self._deps(q, reads, writes)
        i = self.dma_rr[q]; self.dma_rr[q] = (i + 1) % DMA_SEMS_PER_Q
        sem = "D_%s_%d" % (q, i)
        prev = self.dma_tot.get(sem, 0)
        if prev:
            self._need(q, sem, prev)
        tot = prev + 16
        self.dma_tot[sem] = tot
        self.E[q].dma_start(out=out, in_=in_, **kw).then_inc(self._sem(sem), 16)
        self._commit((sem, tot), reads, writes)
        self.n_inst += 1

    def mark(self):
        return len(self.ctxs)

    def barrier(self):
        for x in ENGS:
            for e in ENGS:
                if self.cnt[e]:
                    self._need(x, "S_" + e, self.cnt[e])
            for sem, tot in self.dma_tot.items():
                self._need(x, sem, tot)

    def release(self, m):
        self.barrier()
        keep = []
        for c in reversed(self.ctxs[m:]):
            if c in self._semctx:
                keep.append(c)
            else:
                c.__exit__(None, None, None)
        self.ctxs = self.ctxs[:m] + list(reversed(keep))

    def finish(self, final_keys):
        for k in final_keys:
            lw = self.last_w.get(k)
            if lw is not None:
                self._need("sp", *lw)
        for c in reversed(self.ctxs):
            c.__exit__(None, None, None)
        return self.nc

    def mm(self, out, lhsT, rhs, start, stop, r=(), w=()):
        self.op("pe", lambda e: e.matmul(out, lhsT=lhsT, rhs=rhs, start=start, stop=stop), r, w)

    def tr(self, out, in_, ident, r=(), w=()):
        self.op("pe", lambda e: e.transpose(out=out, in_=in_, identity=ident), r, w)

    def act(self, out, in_, func, r=(), w=(), **kw):
        self.op("act", lambda e: e.activation(out=out, in_=in_, func=func, **kw), r, w)

    def tt(self, eng, out, in0, in1, op, r=(), w=()):
        self.op(eng, lambda e: e.tensor_tensor(out=out, in0=in0, in1=in1, op=op), r, w)

    def ts(self, eng, out, in0, s1, op0, s2=None, op1=None, r=(), w=(), **kw):
        if op1 is None:
            self.op(eng, lambda e: e.tensor_scalar(out=out, in0=in0, scalar1=s1, scalar2=None, op0=op0, **kw), r, w)
        else:
            self.op(eng, lambda e: e.tensor_scalar(out=out, in0=in0, scalar1=s1, scalar2=s2, op0=op0, op1=op1, **kw), r, w)

    def stt(self, out, in0, scalar, in1, op0, op1, r=(), w=()):
        self.op("dve", lambda e: e.scalar_tensor_tensor(out=out, in0=in0, scalar=scalar, in1=in1, op0=op0, op1=op1), r, w)

    def cp(self, eng, out, in_, r=(), w=()):
        if eng == "act":
            self.op("act", lambda e: e.copy(out=out, in_=in_), r, w)
        else:
            self.op(eng, lambda e: e.tensor_copy(out=out, in_=in_), r, w)

    def memset(self, eng, ap, val, w=()):
        self.op(eng, lambda e: e.memset(ap, val), (), w)


class Rot:
    def __init__(self, P, name, n, shape, dt, psum=False):
        P.uid += 1
        self.name = "%s#%d" % (name, P.uid); self.n = n; self.i = 0
        if psum:
            self.t = [P.psv("%s%d" % (name, j), shape, dt) for j in range(n)]
        else:
            self.t = [P.sb("%s%d" % (name, j), shape, dt) for j in range(n)]

    def next(self):
        j = self.i; self.i = (j + 1) % self.n
        return self.t[j], (self.name, j)


def bc_row(ap_row, n):
    return ap_row.to_broadcast([128, n])


class K:
    def __init__(self, cfg):
        self.cfg = cfg
        self.P = Prog()
        P = self.P
        dr = lambda n, s, dt=F32: P.dram(n, s, dt, kind="ExternalInput")
        self.I = I = {}
        I["xin"] = dr("xin", [T, D])
        I["cond2"] = dr("cond2", [128, 8, 2])
        I["ada_w"] = dr("ada_w", [DEPTH, D, 6 * D])
        I["ada_b"] = dr("ada_b", [DEPTH, 6 * D])
        I["norm1_g"] = dr("norm1_g", [DEPTH, D])
        I["norm2_g"] = dr("norm2_g", [DEPTH, D])
        I["final_g"] = dr("final_g", [1, D])
        I["mla_w_in"] = dr("mla_w_in", [2, D, 704])
        I["mla_q_g"] = dr("mla_q_g", [2, 384])
        I["mla_kv_g"] = dr("mla_kv_g", [2, 256])
        I["mla_w_uq"] = dr("mla_w_uq", [2, 384, 1536])
        I["mla_w_ukv"] = dr("mla_w_ukv", [2, 256, 2048])
        I["mla_w_out"] = dr("mla_w_out", [2, D, D])
        I["cosT"] = dr("cosT", [64, T])
        I["sinT"] = dr("sinT", [64, T])
        I["peer_w_q"] = dr("peer_w_q", [DEPTH, D, 2048])
        I["skT"] = dr("skT", [DEPTH, 128, 16, 128])
        I["peer_uT"] = dr("peer_uT", [DEPTH, D, 16384])
        I["peer_v"] = dr("peer_v", [DEPTH, 16384, D])
        I["even_w_in"] = dr("even_w_in", [2, D, 3600])
        I["even_w_out"] = dr("even_w_out", [2, D, D])
        I["conv_wT"] = dr("conv_wT", [2, 12, 128, 5])
        I["gdn_ab"] = dr("gdn_ab", [2, 2, 8])
        I["gdn_norm_g"] = dr("gdn_norm_g", [2, 128])
        I["rpbg"] = dr("rpbg", [2, 64, 15, 8, 64])
        I["namask"] = dr("namask", [64, 64])
        self.out = P.dram("out", [SEQ, D], F32, kind="ExternalOutput")
        self.xres = P.dram("xres", [T, D], F32)
        self.ada_s = P.dram("ada_s", [DEPTH, 2, 6 * D], F32)
        self.attn = P.dram("attn", [T, D], F32)
        self.h2T = P.dram("h2T", [D, T], BF16)
        self.UV = P.dram("UV", [128, 128, 2048], BF16)
        self.GTd = [P.dram("GTd0", [34, 128, 128, 128], BF16), P.dram("GTd1", [NT - 34, 128, 128, 128], BF16)]
        self.sel = P.dram("sel", [NT, 128, 3, 128], F32)
        self.dbg = {}
        for name, shape in cfg.get("dbg", {}).items():
            self.dbg[name] = P.dram("dbg_" + name, list(shape), F32, kind="ExternalOutput")
        self.consts()

    def consts(self):
        P = self.P
        self.identf = P.sb("identf", [128, 128], F32)
        self.ident = P.sb("ident", [128, 128], BF16)
        P.memset("pool", self.identf[:], 1.0, w=["identf"])
        P.op("pool", lambda e: e.affine_select(out=self.identf[:], in_=self.identf[:], pattern=[[-1, 128]],
                                               compare_op=ALU.is_equal, fill=0.0, base=0, channel_multiplier=1),
             ["identf"], ["identf"])
        P.cp("dve", self.ident[:], self.identf[:], ["identf"], ["ident"])
        self.iotab = P.sb("iotab", [128, 128], BF16)
        self.iotaf = P.sb("iotaf", [128, 128], F32)
        P.op("pool", lambda e: e.iota(self.iotaf[:], pattern=[[1, 128]], base=0, channel_multiplier=0,
                                      allow_small_or_imprecise_dtypes=True), (), ["iotaf"])
        P.cp("dve", self.iotab[:], self.iotaf[:], ["iotaf"], ["iotab"])
        self.modA = [P.sb("modA%d" % s, [128, D], F32) for s in range(2)]
        self.modS = [P.sb("modS%d" % s, [128, D], F32) for s in range(2)]
        self.modG = [P.sb("modG%d" % s, [128, D], F32) for s in range(2)]
        self.ng = P.sb("ng", [128, D], F32)
        self.xt = Rot(P, "xt", 2, [128, D], F32)
        self.hb = Rot(P, "hb", 2, [128, D], BF16)
        self.tmpf = Rot(P, "tmpf", 2, [128, D], F32)
        self.small = Rot(P, "small", 4, [128, 4], F32)
        self.junk = P.sb("junk", [128, D], F32)

    def phase_ada(self):
        P, I = self.P, self.I
        m = P.mark()
        cs = P.sb("cs", [128, 8, 2], F32)
        P.dma("sp", cs[:], I["cond2"][:, :, :], w=["cs"])
        P.act(cs[:], cs[:], AF.Silu, ["cs"], ["cs"])
        adab = P.sb("adab", [2, 6 * D], F32)
        arow = P.sb("arow", [2, 6 * D], F32)
        wt = Rot(P, "adaw", 2, [128, 8, 512], F32)
        pa = Rot(P, "pada", 2, [128, 512], F32, psum=True)
        for l in self.cfg["layers"]:
            P.dma("sp", adab[:], I["ada_b"][l:l + 1, :].to_broadcast([2, 6 * D]), w=["adab"])
            wv = I["ada_w"][l].rearrange("(kc p) n -> p kc n", p=128)
            for j in range(12):
                w, wk = wt.next()
                P.dma("sp", w[:], wv[:, :, j * 512:(j + 1) * 512], w=[wk])
                ps, pk = pa.next()
                for kc in range(8):
                    P.mm(ps[0:2, :], cs[:, kc, :], w[:, kc, :], kc == 0, kc == 7, ["cs", wk], [pk])
                P.tt("dve", arow[:, j * 512:(j + 1) * 512], ps[0:2, :], adab[:, j * 512:(j + 1) * 512], ALU.add,
                     [pk, "adab"], ["arow"])
            P.dma("sp", self.ada_s[l, :, :], arow[:], ["arow"], [("ada_s", l)])
        P.release(m)

    def load_AS(self, l, which, streams=(0, 1)):
        P, I = self.P, self.I
        off = (which - 1) * 3 * D
        ngd = I["norm1_g"] if which == 1 else I["norm2_g"]
        P.dma("sp", self.ng[:], ngd[l:l + 1, :].to_broadcast([128, D]), w=["ng"])
        for s in streams:
            row = self.ada_s[l, s:s + 1, :]
            P.dma("sp", self.modS[s][:], row[:, off:off + D].to_broadcast([128, D]), [("ada_s", l)], [("modS", s)])
            P.dma("sp", self.modA[s][:], row[:, off + D:off + 2 * D].to_broadcast([128, D]), [("ada_s", l)], [("modA", s)])
            P.stt(self.modA[s][:], self.modA[s][:], 1.0, self.ng[:], ALU.add, ALU.mult, [("modA", s), "ng"], [("modA", s)])

    def load_gate(self, l, which, streams=(0, 1)):
        P = self.P
        off = (which - 1) * 3 * D
        for s in streams:
            row = self.ada_s[l, s:s + 1, :]
            P.dma("sp", self.modG[s][:], row[:, off + 2 * D:off + 3 * D].to_broadcast([128, D]), [("ada_s", l)], [("modG", s)])

    def rstd(self, x_ap, n, xk, eps=EPS):
        P = self.P
        sm, sk = self.small.next()
        P.act(self.junk[:, 0:n], x_ap, AF.Square, [xk], ["junk", sk], accum_out=sm[:, 0:1])
        P.act(sm[:, 1:2], sm[:, 0:1], AF.Sqrt, [sk], [sk], scale=1.0 / n, bias=eps)
        P.op("dve", lambda e: e.reciprocal(out=sm[:, 2:3], in_=sm[:, 1:2]), [sk], [sk])
        return sm[:, 2:3], sk

    def norm_mod_T(self, x_ap, xk, s, hT_dst, hT_key, pst):
        P = self.P
        r, rk = self.rstd(x_ap, D, xk)
        tf, tk = self.tmpf.next()
        P.stt(tf[:], x_ap, r, self.modA[s][:], ALU.mult, ALU.mult, [xk, rk, ("modA", s)], [tk])
        hb, hk = self.hb.next()
        P.tt("dve", hb[:], tf[:], self.modS[s][:], ALU.add, [tk, ("modS", s)], [hk])
        ps, pk = pst.next()
        for kc in range(8):
            P.tr(ps[:, kc, :], hb[:, kc * 128:(kc + 1) * 128], self.ident[:], [hk, "ident"], [pk])
        P.cp("act", hT_dst, ps[:], [pk], [hT_key])


def blocks(want_ctx=True):
    b = [(0, 256)] if want_ctx else []
    return b + [(256 + 512 * i, 512) for i in range(16)]


def rot_cols(P, eng, dst, src, r, w):
    for (d0, s0, sign) in ((0, 16, -1.0), (16, 0, 1.0), (32, 48, -1.0), (48, 32, 1.0)):
        P.ts(eng, dst[:, :, d0:d0 + 16], src[:, :, s0:s0 + 16], sign, ALU.mult, r=r, w=w)


class KM(K):
    def phase_init(self):
        P = self.P
        src = self.I["xin"]
        for i in range(0, NT, 6):
            P.dma("sp", self.xres[i * 128:(i + 6) * 128, :], src[i * 128:(i + 6) * 128, :], (),
                  [("x", j) for j in range(i, i + 6)])

    def phase_mla_proj(self, l):
        P, I = self.P, self.I
        i = l // 2
        m = P.mark()
        self.QnT = P.dram("QnT%d" % l, [8, 128, T], BF16)
        self.QrT = P.dram("QrT%d" % l, [8, 64, T], BF16)
        self.KnT = P.dram("KnT%d" % l, [8, 128, T], BF16)
        self.KrT = P.dram("KrT%d" % l, [64, T], BF16)
        self.Vd = P.dram("Vd%d" % l, [T, D], BF16)
        Win = P.sb("Win", [128, 8, 704], BF16)
        P.dma("pool", Win[:], I["mla_w_in"][i].rearrange("(kc p) n -> p kc n", p=128), w=["Win"])
        WinRot = P.sb("WinRot", [128, 8, 64], BF16)
        rot_cols(P, "dve", WinRot[:, :, :], Win[:, :, 640:704], ["Win"], ["WinRot"])
        Wuq = P.sb("Wuq", [128, 3, 1536], BF16)
        P.dma("pool", Wuq[:], I["mla_w_uq"][i].rearrange("(kc p) n -> p kc n", p=128), w=["Wuq"])
        WuqRot = P.sb("WuqRot", [128, 3, 8, 64], BF16)
        for kc in range(3):
            src = Wuq[:, kc, :].rearrange("p (h x) -> p h x", x=192)[:, :, 128:192]
            rot_cols(P, "dve", WuqRot[:, kc, :, :], src, ["Wuq"], ["WuqRot"])
        Wukv = P.sb("Wukv", [128, 2, 2048], BF16)
        P.dma("pool", Wukv[:], I["mla_w_ukv"][i].rearrange("(kc p) n -> p kc n", p=128), w=["Wukv"])
        qg = P.sb("qg", [128, 384], F32)
        kvg = P.sb("kvg", [128, 256], F32)
        P.dma("sp", qg[:], I["mla_q_g"][i:i + 1, :].to_broadcast([128, 384]), w=["qg"])
        P.dma("sp", kvg[:], I["mla_kv_g"][i:i + 1, :].to_broadcast([128, 256]), w=["kvg"])
        self.load_AS(l, 1)

        pst = Rot(P, "pst", 2, [128, 8, 128], BF16, psum=True)
        psp = Rot(P, "psp", 1, [128, 1024], F32, psum=True)
        psu = Rot(P, "psu", 3, [128, 512], F32, psum=True)
        hTr = Rot(P, "hT", 2, [128, 8, 512], BF16)
        cTr = Rot(P, "cT", 2, [128, 5, 512], BF16)
        cqn = Rot(P, "cqn", 2, [128, 640], BF16)
        cst = Rot(P, "cst", 2, [64, 2, 512], F32)
        stQn = Rot(P, "stQn", 2, [128, 8, 512], BF16)
        stKn = Rot(P, "stKn", 2, [128, 8, 512], BF16)
        stQr = Rot(P, "stQr", 2, [64, 8, 512], BF16)
        stKr = Rot(P, "stKr", 2, [64, 512], BF16)
        rtmp = Rot(P, "rtmp", 2, [64, 2, 512], F32)
        vsb = Rot(P, "vsb", 2, [128, D], BF16)

        def rope_out(ps1, k1, ps2, k2, cs, ck, n, dst, dk):
            rt, rk = rtmp.next()
            P.tt("dve", rt[:, 0, 0:n], ps1[0:64, 0:n], cs[:, 0, 0:n], ALU.mult, [k1, ck], [rk])
            P.tt("dve", rt[:, 1, 0:n], ps2[0:64, 0:n], cs[:, 1, 0:n], ALU.mult, [k2, ck], [rk])
            P.tt("dve", dst, rt[:, 0, 0:n], rt[:, 1, 0:n], ALU.add, [rk], [dk])

        for (t0, n) in blocks(True):
            s = 0 if t0 >= CTX else 1
            nt = n // 128
            hT, hk = hTr.next()
            cT, ck_ = cTr.next()
            cs, csk = cst.next()
            P.dma("sp", cs[:, 0, 0:n], I["cosT"][:, t0:t0 + n], w=[csk])
            P.dma("sp", cs[:, 1, 0:n], I["sinT"][:, t0:t0 + n], w=[csk])
            for j in range(nt):
                ti = t0 // 128 + j
                xt, xk = self.xt.next()
                P.dma("sp", xt[:], self.xres[ti * 128:(ti + 1) * 128, :], [("x", ti)], [xk])
                self.norm_mod_T(xt[:], xk, s, hT[:, :, j * 128:(j + 1) * 128], hk, pst)
                pp, ppk = psp.next()
                for kc in range(8):
                    P.mm(pp[:, 0:512], hT[:, kc, j * 128:(j + 1) * 128], Win[:, kc, 0:512], kc == 0, kc == 7, [hk, "Win"], [ppk])
                for kc in range(8):
                    P.mm(pp[:, 512:640], hT[:, kc, j * 128:(j + 1) * 128], Win[:, kc, 512:640], kc == 0, kc == 7, [hk, "Win"], [ppk])
                r1, r1k = self.rstd(pp[:, 0:384], 384, ppk)
                r2, r2k = self.rstd(pp[:, 384:640], 256, ppk)
                cq, cqk = cqn.next()
                P.stt(cq[:, 0:384], pp[:, 0:384], r1, qg[:], ALU.mult, ALU.mult, [ppk, r1k, "qg"], [cqk])
                P.stt(cq[:, 384:640], pp[:, 384:640], r2, kvg[:], ALU.mult, ALU.mult, [ppk, r2k, "kvg"], [cqk])
                ps, pk = pst.next()
                for kc in range(5):
                    P.tr(ps[:, kc, :], cq[:, kc * 128:(kc + 1) * 128], self.ident[:], [cqk, "ident"], [pk])
                P.cp("act", cT[:, :, j * 128:(j + 1) * 128], ps[:, 0:5, :], [pk], [ck_])
            sQn, sQnk = stQn.next(); sKn, sKnk = stKn.next(); sQr, sQrk = stQr.next(); sKr, sKrk = stKr.next()
            for h in range(8):
                ps, pk = psu.next()
                for kc in range(3):
                    P.mm(ps[:, 0:n], Wuq[:, kc, h * 192:h * 192 + 128], cT[:, kc, 0:n], kc == 0, kc == 2, ["Wuq", ck_], [pk])
                P.cp("act", sQn[:, h, 0:n], ps[:, 0:n], [pk], [sQnk])
                ps, pk = psu.next()
                for kc in range(2):
                    P.mm(ps[:, 0:n], Wukv[:, kc, h * 256:h * 256 + 128], cT[:, 3 + kc, 0:n], kc == 0, kc == 1, ["Wukv", ck_], [pk])
                P.cp("act", sKn[:, h, 0:n], ps[:, 0:n], [pk], [sKnk])
                ps1, k1 = psu.next()
                for kc in range(3):
                    P.mm(ps1[0:64, 0:n], Wuq[:, kc, h * 192 + 128:h * 192 + 192], cT[:, kc, 0:n], kc == 0, kc == 2, ["Wuq", ck_], [k1])
                ps2, k2 = psu.next()
                for kc in range(3):
                    P.mm(ps2[0:64, 0:n], WuqRot[:, kc, h, :], cT[:, kc, 0:n], kc == 0, kc == 2, ["WuqRot", ck_], [k2])
                rope_out(ps1, k1, ps2, k2, cs, csk, n, sQr[:, h, 0:n], sQrk)
            ps1, k1 = psu.next()
            for kc in range(8):
                P.mm(ps1[0:64, 0:n], Win[:, kc, 640:704], hT[:, kc, 0:n], kc == 0, kc == 7, ["Win", hk], [k1])
            ps2, k2 = psu.next()
            for kc in range(8):
                P.mm(ps2[0:64, 0:n], WinRot[:, kc, :], hT[:, kc, 0:n], kc == 0, kc == 7, ["WinRot", hk], [k2])
            rope_out(ps1, k1, ps2, k2, cs, csk, n, sKr[:, 0:n], sKrk)
            bk = ("mlab", t0)
            P.dma("sp", self.QnT[:, :, t0:t0 + n].rearrange("h p t -> p h t"), sQn[:, :, 0:n], [sQnk], [("QnT", t0)])
            P.dma("sp", self.KnT[:, :, t0:t0 + n].rearrange("h p t -> p h t"), sKn[:, :, 0:n], [sKnk], [("KnT", t0)])
            P.dma("sp", self.QrT[:, :, t0:t0 + n].rearrange("h p t -> p h t"), sQr[:, :, 0:n], [sQrk], [("QrT", t0)])
            P.dma("sp", self.KrT[:, t0:t0 + n], sKr[:, 0:n], [sKrk], [("KrT", t0)])
            for j in range(nt):
                vs, vk = vsb.next()
                for hf in range(2):
                    ps, pk = psu.next()
                    for kc in range(2):
                        rhs = Wukv[:, kc, :].rearrange("p (h x) -> p h x", x=256)[:, 4 * hf:4 * hf + 4, 128:256]
                        P.mm(ps[:, :], cT[:, 3 + kc, j * 128:(j + 1) * 128], rhs, kc == 0, kc == 1, ["Wukv", ck_], [pk])
                    P.cp("act", vs[:, hf * 512:(hf + 1) * 512], ps[:, :], [pk], [vk])
                ti = t0 // 128 + j
                P.dma("sp", self.Vd[ti * 128:(ti + 1) * 128, :], vs[:], [vk], [("Vd", ti)])
        P.release(m)

    def phase_mla_attn(self, l, want_ctx):
        P = self.P
        m = P.mark()
        scale = 192.0 ** -0.5
        kn = Rot(P, "kn", 2, [128, T], BF16)
        va = Rot(P, "va", 2, [128, NT, 129], BF16)
        kr = P.sb("kr", [64, T], BF16)
        qn = Rot(P, "qn", 2, [128, 512], BF16)
        qr = Rot(P, "qr", 2, [64, 512], BF16)
        pT = Rot(P, "pT", 4, [128, 512], BF16)
        psS = Rot(P, "psS", 3, [128, 512], F32, psum=True)
        psO = P.ps("psO", [128, 4, 512], F32)
        osb = Rot(P, "osb", 2, [128, 4, 128], F32)
        for t in va.t:
            P.memset("pool", t[:, :, 128:129], 1.0, w=[(va.name, va.t.index(t))])
        allq = [("QnT", b[0]) for b in blocks(True)]
        P.dma("sp", kr[:], self.KrT[:, :], [("KrT", b[0]) for b in blocks(True)], ["kr"])
        for h in range(8):
            k_t, kk = kn.next()
            v_t, vk = va.next()
            P.dma("sp", k_t[:], self.KnT[h, :, :], [("KnT", b[0]) for b in blocks(True)], [kk])
            for q4 in range(0, NT, 11):
                P.dma("sp", v_t[:, q4:q4 + 11, 0:128],
                      self.Vd[q4 * 128:(q4 + 11) * 128, h * 128:(h + 1) * 128].rearrange("(kt p) d -> p kt d", p=128),
                      [("Vd", ti) for ti in range(q4, q4 + 11)], [vk])
            for (t0, n) in blocks(want_ctx):
                nkt = NT if t0 >= CTX else 2
                q_t, qk = qn.next()
                qr_t, qrk = qr.next()
                P.dma("sp", q_t[:, 0:n], self.QnT[h, :, t0:t0 + n], [("QnT", t0)], [qk])
                P.dma("sp", qr_t[:, 0:n], self.QrT[h, :, t0:t0 + n], [("QrT", t0)], [qrk])
                nj = n // 128
                def s_stage(kt):
                    ps, pk = psS.next()
                    P.mm(ps[:, 0:n], k_t[:, kt * 128:(kt + 1) * 128], q_t[:, 0:n], True, False, [kk, qk], [pk])
                    P.mm(ps[:, 0:n], kr[:, kt * 128:(kt + 1) * 128], qr_t[:, 0:n], False, True, ["kr", qrk], [pk])
                    p_t, ptk = pT.next()
                    P.act(p_t[:, 0:n], ps[:, 0:n], AF.Exp, [pk], [ptk], scale=scale)
                    return p_t, ptk

                def pv_stage(kt, p_t, ptk):
                    for j in range(nj):
                        P.mm(psO[:, j, 0:129], p_t[:, j * 128:(j + 1) * 128], v_t[:, kt, :], kt == 0, kt == nkt - 1,
                             [ptk, vk], [("psO", j)])

                q_ = [s_stage(0)]
                if nkt > 1:
                    q_.append(s_stage(1))
                for kt in range(nkt):
                    if kt + 2 < nkt:
                        q_.append(s_stage(kt + 2))
                    pv_stage(kt, *q_.pop(0))
                o_t, ok = osb.next()
                for j in range(nj):
                    sm, sk = self.small.next()
                    P.op("dve", lambda e, sm=sm, j=j: e.reciprocal(out=sm[:, 0:1], in_=psO[:, j, 128:129]), [("psO", j)], [sk])
                    P.ts("dve", o_t[:, j, :], psO[:, j, 0:128], sm[:, 0:1], ALU.mult, r=[("psO", j), sk], w=[ok])
                P.dma("sp", self.attn[t0:t0 + n, h * 128:(h + 1) * 128].rearrange("(j p) d -> p j d", p=128),
                      o_t[:, 0:nj, :], [ok], [("attn", t0, h)])
        P.release(m)

    def phase_outproj(self, l, wout_ap, want_ctx):
        P = self.P
        m = P.mark()
        Wout = P.sb("Wout", [128, 8, D], BF16)
        P.dma("pool", Wout[:], wout_ap.rearrange("(kc p) n -> p kc n", p=128), w=["Wout"])
        self.load_gate(l, 1)
        self.load_AS(l, 2)
        pst = Rot(P, "pst", 2, [128, 8, 128], BF16, psum=True)
        psy = Rot(P, "psy", 2, [128, D], F32, psum=True)
        at = Rot(P, "at", 2, [128, D], F32)
        ab = Rot(P, "ab", 2, [128, D], BF16)
        aT = Rot(P, "aT", 2, [128, 8, 128], BF16)
        xn = Rot(P, "xn", 2, [128, D], F32)
        h2s = Rot(P, "h2s", 2, [128, 8, 128], BF16)
        for ti in range(0 if want_ctx else 2, NT):
            s = 0 if ti >= 2 else 1
            a_t, ak = at.next()
            P.dma("sp", a_t[:], self.attn[ti * 128:(ti + 1) * 128, :], (), [ak])
            b_t, bk = ab.next()
            P.cp("act", b_t[:], a_t[:], [ak], [bk])
            ps, pk = pst.next()
            for kc in range(8):
                P.tr(ps[:, kc, :], b_t[:, kc * 128:(kc + 1) * 128], self.ident[:], [bk, "ident"], [pk])
            aT_t, aTk = aT.next()
            P.cp("act", aT_t[:], ps[:], [pk], [aTk])
            py, pyk = psy.next()
            for hf in range(2):
                for kc in range(8):
                    P.mm(py[:, hf * 512:(hf + 1) * 512], aT_t[:, kc, :], Wout[:, kc, hf * 512:(hf + 1) * 512], kc == 0, kc == 7,
                         [aTk, "Wout"], [pyk])
            xt, xk = self.xt.next()
            P.dma("sp", xt[:], self.xres[ti * 128:(ti + 1) * 128, :], (), [xk])
            tf, tk = self.tmpf.next()
            P.tt("dve", tf[:], py[:], self.modG[s][:], ALU.mult, [pyk, ("modG", s)], [tk])
            x_n, xnk = xn.next()
            P.tt("dve", x_n[:], tf[:], xt[:], ALU.add, [tk, xk], [xnk])
            P.dma("sp", self.xres[ti * 128:(ti + 1) * 128, :], x_n[:], [xnk], [("x", ti)])
            h_t, hk = h2s.next()
            self.norm_mod_T(x_n[:], xnk, s, h_t[:], hk, pst)
            P.dma("sp", self.h2T[:, ti * 128:(ti + 1) * 128].rearrange("(kc p) t -> p kc t", p=128), h_t[:], [hk], [("h2T", ti)])
        P.release(m)

    def phase_peer_prep(self, l):
        P, I = self.P, self.I
        uT = I["peer_uT"][l].rearrange("(kc p) e -> p kc e", p=128)
        for c in range(128):
            P.dma("pool", self.UV[c, :, 0:1024].rearrange("p (kc e) -> p kc e", e=128), uT[:, :, c * 128:(c + 1) * 128], (), [("UV", c)])
            P.dma("pool", self.UV[c, :, 1024:2048], I["peer_v"][l, c * 128:(c + 1) * 128, :], (), [("UV", c)])

    def phase_peer_sel(self, l, want_ctx):
        P, I = self.P, self.I
        m = P.mark()
        Wq = P.sb("Wq", [128, 8, 2048], BF16)
        P.dma("pool", Wq[:], I["peer_w_q"][l].rearrange("(kc p) n -> p kc n", p=128), w=["Wq"])
        skT = P.sb("skT", [128, 16, 128], F32)
        P.dma("sp", skT[:], I["skT"][l], w=["skT"])
        h2 = Rot(P, "h2", 2, [128, 8, 128], BF16)
        qTsR = Rot(P, "qTs", 2, [128, 16, 128], F32)
        s_sbR = Rot(P, "s_sb", 2, [128, 16, 128], F32)
        s_tmp = P.sb("s_tmp", [128, 16, 128], F32)
        stop_ = P.sb("stop", [128, 16, 16], F32)
        sidx = P.sb("sidx", [128, 16, 16], U32)
        sidf = P.sb("sidf", [128, 16, 16], F32)
        comb = P.sb("comb", [128, 8, 256], F32)
        comb2 = P.sb("comb2", [128, 8, 256], F32)
        ctop = P.sb("ctop", [128, 8, 16], F32)
        cidx = P.sb("cidx", [128, 8, 16], U32)
        cj = P.sb("cj", [128, 2, 8, 16], U32)
        cjf = P.sb("cjf", [128, 2, 8, 16], F32)
        oh = P.sb("oh", [128, 8, 16, 16], F32)
        i12 = P.sb("i12", [128, 3, 128], F32)
        i12T = Rot(P, "i12T", 2, [128, 3, 128], F32)
        allA = [("stopA", hp) for hp in range(16)]; allB = [("stopB", hp) for hp in range(16)]
        allI = [("sidxA", hp) for hp in range(16)] + [("sidxB", hp) for hp in range(16)]
        nmx = P.sb("nmx", [128, 8], F32)
        zs = P.sb("zs", [128, 8], F32)
        psA = Rot(P, "psA", 3, [128, 512], F32, psum=True)
        psS = P.ps("psS4", [128, 4, 512], F32)
        iota16 = self.iotaf[:, 0:16]
        for ti in range(0 if want_ctx else 2, NT):
            h_t, hk = h2.next()
            qTs, qTsk = qTsR.next()
            s_sb, ssk = s_sbR.next()
            P.dma("sp", h_t[:], self.h2T[:, ti * 128:(ti + 1) * 128].rearrange("(kc p) t -> p kc t", p=128), (), [hk])
            for hp4 in range(4):
                ps, pk = psA.next()
                for q in range(4):
                    hp = hp4 * 4 + q
                    for kc in range(8):
                        P.mm(ps[:, q * 128:(q + 1) * 128], Wq[:, kc, hp * 128:(hp + 1) * 128], h_t[:, kc, :], kc == 0, kc == 7,
                             ["Wq", hk], [pk])
                P.cp("act", qTs[:, hp4 * 4:hp4 * 4 + 4, :], ps[:, :].rearrange("p (a n) -> p a n", n=128), [pk], [qTsk])
            for hp in range(16):
                P.mm(psS[:, hp // 4, (hp % 4) * 128:(hp % 4 + 1) * 128], qTs[:, hp, :], skT[:, hp, :], True, True,
                     [qTsk, "skT"], [("psS", hp // 4)])
            for g in range(4):
                P.cp("act", s_sb[:, 4 * g:4 * g + 4, :], psS[:, g, :].rearrange("p (a n) -> p a n", n=128), [("psS", g)], [ssk])
            for hp in range(16):
                P.op("dve", lambda e, hp=hp: e.max(out=stop_[:, hp, 0:8], in_=s_sb[:, hp, :]), [ssk], [("stopA", hp)])
            for hp in range(16):
                P.op("dve", lambda e, hp=hp: e.match_replace(out=s_tmp[:, hp, :], in_to_replace=stop_[:, hp, 0:8], in_values=s_sb[:, hp, :],
                                                             imm_value=-1e30), [ssk, ("stopA", hp)], [("s_tmp", hp)])
            for hp in range(16):
                P.op("dve", lambda e, hp=hp: e.max(out=stop_[:, hp, 8:16], in_=s_tmp[:, hp, :]), [("s_tmp", hp)], [("stopB", hp)])
            for hp in range(16):
                P.op("dve", lambda e, hp=hp: e.max_index(out=sidx[:, hp, 0:8], in_max=stop_[:, hp, 0:8], in_values=s_sb[:, hp, :]),
                     [ssk, ("stopA", hp)], [("sidxA", hp)])
            for hp in range(16):
                P.op("dve", lambda e, hp=hp: e.max_index(out=sidx[:, hp, 8:16], in_max=stop_[:, hp, 8:16], in_values=s_sb[:, hp, :]),
                     [ssk, ("stopB", hp)], [("sidxB", hp)])
            P.cp("dve", sidf[:], sidx[:], allI, ["sidf"])
            st4 = stop_[:].rearrange("p (h two) j -> p h two j", two=2)
            in0 = st4[:, :, 0, :].unsqueeze(3).to_broadcast([128, 8, 16, 16])
            in1 = st4[:, :, 1, :].unsqueeze(2).to_broadcast([128, 8, 16, 16])
            P.tt("dve", comb[:].rearrange("p h (a b) -> p h a b", b=16), in0, in1, ALU.add, allA + allB, ["comb"])
            for h in range(8):
                P.op("dve", lambda e, h=h: e.max(out=ctop[:, h, 0:8], in_=comb[:, h, :]), ["comb"], [("ctopA", h)])
            for h in range(8):
                P.op("dve", lambda e, h=h: e.match_replace(out=comb2[:, h, :], in_to_replace=ctop[:, h, 0:8], in_values=comb[:, h, :],
                                                           imm_value=-1e30), ["comb", ("ctopA", h)], [("comb2", h)])
            for h in range(8):
                P.op("dve", lambda e, h=h: e.max(out=ctop[:, h, 8:16], in_=comb2[:, h, :]), [("comb2", h)], [("ctopB", h)])
            for h in range(8):
                P.op("dve", lambda e, h=h: e.max_index(out=cidx[:, h, 0:8], in_max=ctop[:, h, 0:8], in_values=comb[:, h, :]),
                     ["comb", ("ctopA", h)], [("cidxA", h)])
            for h in range(8):
                P.op("dve", lambda e, h=h: e.max_index(out=cidx[:, h, 8:16], in_max=ctop[:, h, 8:16], in_values=comb[:, h, :]),
                     ["comb", ("ctopB", h)], [("cidxB", h)])
            ctk = [("ctopA", h) for h in range(8)] + [("ctopB", h) for h in range(8)]
            cik = [("cidxA", h) for h in range(8)] + [("cidxB", h) for h in range(8)]
            P.ts("dve", cj[:, 0, :, :], cidx[:], 4, ALU.logical_shift_right, r=cik, w=["cj"])
            P.ts("dve", cj[:, 1, :, :], cidx[:], 15, ALU.bitwise_and, r=cik, w=["cj"])
            P.cp("dve", cjf[:], cj[:], ["cj"], ["cjf"])
            sd4 = sidf[:].rearrange("p (h two) j -> p h two j", two=2)
            for w_ in range(2):
                P.tt("dve", oh[:], cjf[:, w_, :, :].unsqueeze(3).to_broadcast([128, 8, 16, 16]),
                     iota16.unsqueeze(1).unsqueeze(1).to_broadcast([128, 8, 16, 16]), ALU.is_equal, ["cjf", "iotaf"], ["oh"])
                P.tt("dve", oh[:], oh[:], sd4[:, :, w_, :].unsqueeze(2).to_broadcast([128, 8, 16, 16]), ALU.mult, ["oh", "sidf"], ["oh"])
                P.op("dve", lambda e, w_=w_: e.tensor_reduce(out=i12[:, w_, :].rearrange("p (h k) -> p h k", k=16), in_=oh[:],
                                                             axis=AX.X, op=ALU.add), ["oh"], ["i12"])
            P.ts("dve", nmx[:], ctop[:, :, 0], -1.0, ALU.mult, r=ctk, w=["nmx"])
            for h in range(8):
                P.act(i12[:, 2, h * 16:(h + 1) * 16], ctop[:, h, :], AF.Exp, ctk + ["nmx"], [("i12g", h), ("zs", h)],
                      bias=nmx[:, h:h + 1], scale=1.0, accum_out=zs[:, h:h + 1])
            zk_ = [("zs", h) for h in range(8)]; gk_ = [("i12g", h) for h in range(8)]
            P.op("dve", lambda e: e.reciprocal(out=zs[:], in_=zs[:]), zk_, zk_)
            g3 = i12[:, 2, :].rearrange("p (h k) -> p h k", k=16)
            P.tt("dve", g3, g3, zs[:].unsqueeze(2).to_broadcast([128, 8, 16]), ALU.mult, ["i12"] + zk_ + gk_, ["i12"] + gk_)
            iT, iTk = i12T.next()
            for w_ in range(3):
                ps, pk = psA.next()
                P.tr(ps[:, 0:128], i12[:, w_, :], self.identf[:], ["i12", "identf"] + gk_, [pk])
                P.cp("act", iT[:, w_, :], ps[:, 0:128], [pk], [iTk])
            P.dma("sp", self.sel[ti], iT[:], [iTk], [("sel", ti)])
        P.release(m)

    def phase_peer_g(self, l, want_ctx):
        P = self.P
        m = P.mark()
        i12T = Rot(P, "i12T", 2, [128, 3, 128], F32)
        i12Tb = Rot(P, "i12Tb", 2, [128, 3, 128], BF16)
        Aoh = Rot(P, "Aoh", 2, [128, 64, 128], BF16)
        Boh = Rot(P, "Boh", 2, [128, 64, 128], BF16)
        GTs = Rot(P, "GTs", 2, [128, 128, 128], BF16)
        ni2r = Rot(P, "ni2", 2, [128, 128], F32)
        abt = Rot(P, "abt", 4, [128, 128], F32)
        NB_DVE = 64
        psA = Rot(P, "psG", 4, [128, 512], F32, psum=True)
        for ti in range(0 if want_ctx else 2, NT):
            iT, iTk = i12T.next()
            P.dma("sp", iT[:], self.sel[ti], (), [iTk])
            G_, Gk = GTs.next()
            ni2, nik = ni2r.next()
            P.ts("dve", ni2[:], iT[:, 1, :], -1.0, ALU.mult, r=[iTk], w=[nik])
            for g64 in range(2):
                A_, Ak = Aoh.next(); B_, Bk = Boh.next()
                for tt_ in range(64):
                    t = g64 * 64 + tt_
                    P.ts("dve", A_[:, tt_, :], self.iotab[:], iT[:, 0, t:t + 1], ALU.is_equal, iT[:, 2, t:t + 1], ALU.mult,
                         r=[iTk, "iotab"], w=[(Ak, tt_)])
                    if tt_ < NB_DVE:
                        P.ts("dve", B_[:, tt_, :], self.iotab[:], iT[:, 1, t:t + 1], ALU.is_equal, r=[iTk, "iotab"], w=[(Bk, tt_)])
                    else:
                        a_t, atk = abt.next()
                        P.act(a_t[:], self.iotaf[:], AF.Abs, ["iotaf", nik], [atk], bias=ni2[:, t:t + 1], scale=1.0)
                        P.act(B_[:, tt_, :], a_t[:], AF.Relu, [atk], [(Bk, tt_)], bias=1.0, scale=-1.0)
                for t4 in range(16):
                    ps, pk = psA.next()
                    for tq in range(4):
                        tt_ = t4 * 4 + tq
                        P.mm(ps[:, tq * 128:(tq + 1) * 128], B_[:, tt_, :], A_[:, tt_, :], True, True,
                             [(Ak, tt_), (Bk, tt_)], [pk])
                    c0 = g64 * 64 + t4 * 4
                    dst = G_[:, :, c0:c0 + 4].rearrange("p c t -> p t c")
                    P.cp("act", dst, ps[:, :].rearrange("p (t c) -> p t c", c=128), [pk], [Gk])
            P.dma("sp", self.GTd[ti // 34][ti % 34], G_[:], [Gk], ())
        P.release(m)

    def phase_peer(self, l, want_ctx):
        P, I = self.P, self.I
        m = P.mark()
        NJ = 3
        TB = 128 * NJ
        self.load_gate(l, 2)
        h2 = Rot(P, "h2", 2, [128, 8, TB], BF16)
        uv = Rot(P, "uv", 8, [128, 2048], BF16)
        gt = Rot(P, "gt", 3, [128, NJ, 8, 128], BF16)
        ga = Rot(P, "ga", 3, [128, TB], BF16)
        wt = Rot(P, "wt", 3, [128, TB], BF16)
        psA = Rot(P, "psA", 2, [128, 512], F32, psum=True)
        psV = P.ps("psV", [128, 2 * NJ, 512], F32)
        for b in range(T // TB):
            t0 = b * TB
            h_t, hk = h2.next()
            P.dma("sp", h_t[:], self.h2T[:, t0:t0 + TB].rearrange("(kc p) t -> p kc t", p=128), (), [hk])
            def stage_a(c):
                nonlocal g_t8, g8k
                if c % 8 == 0:
                    g_t8, g8k = gt.next()
                    for j in range(NJ):
                        ti = NJ * b + j
                        P.dma("sp", g_t8[:, j, :, :], self.GTd[ti // 34][ti % 34, :, c:c + 8, :], (), [g8k])
                u_t, uk = uv.next()
                P.dma("sp", u_t[:], self.UV[c], [("UV", c)], [uk])
                ps, pk = psA.next()
                for kc in range(8):
                    P.mm(ps[:, 0:TB], u_t[:, kc * 128:(kc + 1) * 128], h_t[:, kc, :], kc == 0, kc == 7, [uk, hk], [pk])
                g_t, gk = ga.next()
                P.act(g_t[:], ps[:, 0:TB], AF.Gelu, [pk], [gk])
                w_t, wk = wt.next()
                P.tt("dve", w_t[:].rearrange("p (j t) -> p j t", j=NJ), g_t[:].rearrange("p (j t) -> p j t", j=NJ),
                     g_t8[:, :, c % 8, :], ALU.mult, [gk, g8k], [wk])
                return w_t, wk, u_t, uk

            def stage_b(c, w_t, wk, u_t, uk):
                for j in range(NJ):
                    for hf in range(2):
                        P.mm(psV[:, j * 2 + hf, :], w_t[:, j * 128:(j + 1) * 128], u_t[:, 1024 + hf * 512:1024 + (hf + 1) * 512],
                             c == 0, c == 127, [wk, uk], [("psV", j * 2 + hf)])

            g_t8 = g8k = None
            prev = stage_a(0)
            for c in range(128):
                nxt = stage_a(c + 1) if c + 1 < 128 else None
                stage_b(c, *prev)
                prev = nxt
            for j in range(NJ):
                ti = t0 // 128 + j
                s_ = 0 if ti >= 2 else 1
                xt, xk = self.xt.next()
                P.dma("sp", xt[:], self.xres[ti * 128:(ti + 1) * 128, :], (), [xk])
                tf, tk = self.tmpf.next()
                P.tt("dve", tf[:], psV[:, 2 * j:2 * j + 2, :].rearrange("p a n -> p (a n)"), self.modG[s_][:], ALU.mult,
                     [("psV", 2 * j), ("psV", 2 * j + 1), ("modG", s_)], [tk])
                P.tt("dve", tf[:], tf[:], xt[:], ALU.add, [tk, xk], [tk])
                P.dma("sp", self.xres[ti * 128:(ti + 1) * 128, :], tf[:], [tk], [("x", ti)])
        P.release(m)

    def phase_final(self):
        P, I = self.P, self.I
        m = P.mark()
        fg = P.sb("fg", [128, D], F32)
        P.dma("sp", fg[:], I["final_g"][0:1, :].to_broadcast([128, D]), w=["fg"])
        ot = Rot(P, "ot", 2, [128, D], F32)
        for ti in range(2, NT):
            xt, xk = self.xt.next()
            P.dma("sp", xt[:], self.xres[ti * 128:(ti + 1) * 128, :], (), [xk])
            r, rk = self.rstd(xt[:], D, xk)
            o, ok = ot.next()
            P.stt(o[:], xt[:], r, fg[:], ALU.mult, ALU.mult, [xk, rk, "fg"], [ok])
            P.dma("sp", self.out[(ti - 2) * 128:(ti - 1) * 128, :], o[:], [ok], ["out"])
        P.release(m)


def build(cfg):
    k = KE(cfg)
    P = k.P
    if cfg.get("scopes"):
        for nm in [n for n in dir(k) if n.startswith("phase_") and n != "phase_even"]:
            def wrap(f, nm=nm):
                def g(*a, **kw):
                    with P.nc.named_scope(nm + "_" + str(a[0] if a else "")):
                        return f(*a, **kw)
                return g
            setattr(k, nm, wrap(getattr(k, nm)))
    layers = cfg["layers"]
    stop_after = cfg.get("stop_after")
    k.phase_init()
    k.phase_ada()
    P.barrier()

    def dump(name, src):
        if name in k.dbg:
            P.barrier()
            n = src.shape[0]
            for r0 in range(0, n, 1024):
                r1 = min(n, r0 + 1024)
                P.dma("sp", k.dbg[name][r0:r1, :], src[r0:r1, :], (), ["dbgout"])
            P.barrier()

    done = False
    for l in layers:
        want_ctx = l < DEPTH - 1
        k.phase_peer_prep(l)
        if l % 2 == 0:
            k.phase_even(l, want_ctx)
            wout = k.I["even_w_out"][l // 2]
        else:
            k.phase_mla_proj(l)
            k.phase_mla_attn(l, want_ctx)
            wout = k.I["mla_w_out"][l // 2]
        dump("attn%d" % l, k.attn)
        if stop_after == ("attn", l):
            done = True; break
        k.phase_outproj(l, wout, want_ctx)
        dump("xm%d" % l, k.xres)
        if stop_after == ("outproj", l):
            done = True; break
        k.phase_peer_sel(l, True)
        k.phase_peer_g(l, True)
        k.phase_peer(l, True)
        dump("x%d" % l, k.xres)
    if not done:
        k.phase_final()
    P.barrier()
    return P.finish([]), k


def rope_tables():
    half = 32
    inv = 10000.0 ** (-np.arange(0, half, 2, dtype=np.float32) / half)
    t = np.arange(SEQ)
    row = (t // 64).astype(np.float32); col = (t % 64).astype(np.float32)
    ar = row[:, None] * inv[None, :]; ac = col[:, None] * inv[None, :]
    ang = np.concatenate([ar, ar, ac, ac], axis=-1).astype(np.float32)
    cosT = np.ones((64, T), np.float32); sinT = np.zeros((64, T), np.float32)
    cosT[:, CTX:] = np.cos(ang).T; sinT[:, CTX:] = np.sin(ang).T
    return cosT, sinT


def na_tables(rpb):
    q = np.arange(64); kc = np.arange(64)
    dc = np.clip(kc[:, None] - q[None, :], -15, 15) + 15
    g = rpb[:, :, :, dc]
    g = np.ascontiguousarray(np.transpose(g, (0, 3, 2, 1, 4))).astype(np.float32)
    cstart = np.clip(q - 8, 0, 48)
    ok = (kc[:, None] >= cstart[None, :]) & (kc[:, None] < cstart[None, :] + 16)
    mask = np.where(ok, 0.0, NEG).astype(np.float32)
    return g, mask


def host_inputs(inp, b):
    f = lambda a: np.ascontiguousarray(np.asarray(a, dtype=np.float32))
    cosT, sinT = rope_tables()
    rpbg, namask = na_tables(np.asarray(inp["na_rpb"], np.float32))
    c2 = np.stack([np.asarray(inp["c"][b]).reshape(8, 128).T, np.asarray(inp["c_ctx"]).reshape(8, 128).T], axis=-1)
    m = {
        "xin": f(np.concatenate([inp["ctx"][b], inp["x"][b]], axis=0)),
        "cond2": f(c2),
        "cosT": cosT, "sinT": sinT, "rpbg": rpbg, "namask": namask,
        "final_g": f(np.asarray(inp["final_g"]).reshape(1, D)),
        "conv_wT": f(np.transpose(np.asarray(inp["even_conv_w"]).reshape(2, 5, 12, 128), (0, 2, 3, 1))),
        "gdn_ab": f(np.stack([np.asarray(inp["gdn_a_log"]).reshape(2, 8), np.asarray(inp["gdn_dt_bias"]).reshape(2, 8)], axis=1)),
    }
    return m


_SHARED = None


def shared_inputs(inp):
    f = lambda a: np.ascontiguousarray(np.asarray(a, dtype=np.float32))
    sk = np.asarray(inp["peer_sub_keys"], np.float32).reshape(DEPTH, 16, 128, 128)
    m = {k: f(inp[k]) for k in ("ada_w", "ada_b", "norm1_g", "norm2_g", "mla_w_in", "mla_q_g", "mla_kv_g", "mla_w_uq",
                                "mla_w_ukv", "mla_w_out", "peer_w_q", "peer_v", "even_w_in", "even_w_out", "gdn_norm_g")}
    m["skT"] = f(np.transpose(sk, (0, 3, 1, 2)))
    m["peer_uT"] = f(np.transpose(np.asarray(inp["peer_u"], np.float32), (0, 2, 1)))
    return m


def kernel(**inputs):
    cfg = {"layers": [0, 1, 2, 3]}
    nc, k = build(cfg)
    sh = shared_inputs(inputs)
    in_maps = []
    for b in range(8):
        m = dict(sh); m.update(host_inputs(inputs, b))
        in_maps.append(m)
    res = run_bass_kernel_spmd(nc, in_maps, core_ids=list(range(8)))
    return np.stack([np.asarray(r["out"], np.float32) for r in res.results], axis=0)


class KE(KM):
    def phase_even_proj(self, l):
        P, I = self.P, self.I
        i = l // 2
        m = P.mark()
        if not hasattr(self, "qaT"):
            self.qaT = P.dram("qaT", [8, 64, T], BF16)
            self.kaT = P.dram("kaT", [8, 64, T], BF16)
            self.va = P.dram("va", [T, 8, 65], BF16)
            self.gpre = P.dram("gpre", [12, 128, T], F32)
            self.zd = P.dram("zd", [T, 512], F32)
            self.grd = P.dram("grd", [T, 16], F32)
        Win = P.sb("WinE", [128, 8, 3600], BF16)
        wv = I["even_w_in"][i].rearrange("(kc p) n -> p kc n", p=128)
        for kc in range(8):
            P.dma("pool", Win[:, kc, :], wv[:, kc, :], w=["Win"])
        self.load_AS(l, 1)
        pst = Rot(P, "pst", 2, [128, 8, 128], BF16, psum=True)
        psu = Rot(P, "psu", 4, [128, 512], F32, psum=True)
        hTr = Rot(P, "hT", 2, [128, 8, 512], BF16)
        stq = Rot(P, "stq", 2, [64, 8, 512], BF16)
        stk = Rot(P, "stk", 2, [64, 8, 512], BF16)
        stg = Rot(P, "stg", 3, [128, 512], F32)
        vsb = Rot(P, "vsbE", 2, [128, 8, 65], BF16)
        for t_ in vsb.t:
            P.memset("pool", t_[:, :, 64:65], 1.0, w=[(vsb.name, vsb.t.index(t_))])
        zsb = Rot(P, "zsbE", 2, [128, 512], F32)
        gsb = Rot(P, "gsbE", 2, [128, 16], F32)
        for (t0, n) in blocks(True):
            s = 0 if t0 >= CTX else 1
            nt = n // 128
            hT, hk = hTr.next()
            for j in range(nt):
                ti = t0 // 128 + j
                xt, xk = self.xt.next()
                P.dma("sp", xt[:], self.xres[ti * 128:(ti + 1) * 128, :], (), [xk])
                self.norm_mod_T(xt[:], xk, s, hT[:, :, j * 128:(j + 1) * 128], hk, pst)
            sq, sqk = stq.next(); sk_, skk = stk.next()
            for h in range(8):
                for (dst, dk, c0) in ((sq, sqk, 0), (sk_, skk, 512)):
                    ps, pk = psu.next()
                    for kc in range(8):
                        P.mm(ps[0:64, 0:n], Win[:, kc, c0 + h * 64:c0 + (h + 1) * 64], hT[:, kc, 0:n], kc == 0, kc == 7, ["Win", hk], [pk])
                    P.cp("act", dst[:, h, 0:n], ps[0:64, 0:n], [pk], [dk])
            P.dma("sp", self.qaT[:, :, t0:t0 + n].rearrange("h p t -> p h t"), sq[:, :, 0:n], [sqk], ())
            P.dma("sp", self.kaT[:, :, t0:t0 + n].rearrange("h p t -> p h t"), sk_[:, :, 0:n], [skk], ())
            for c in range(12):
                ps, pk = psu.next()
                for kc in range(8):
                    P.mm(ps[:, 0:n], Win[:, kc, 1536 + c * 128:1536 + (c + 1) * 128], hT[:, kc, 0:n], kc == 0, kc == 7, ["Win", hk], [pk])
                sg, sgk = stg.next()
                P.cp("act" if c % 2 else "dve", sg[:, 0:n], ps[:, 0:n], [pk], [sgk])
                P.dma("sp", self.gpre[c, :, t0:t0 + n], sg[:, 0:n], [sgk], ())
            for j in range(nt):
                ti = t0 // 128 + j
                tsl = slice(j * 128, (j + 1) * 128)
                ps, pk = psu.next()
                for kc in range(8):
                    P.mm(ps[:, :], hT[:, kc, tsl], Win[:, kc, 1024:1536], kc == 0, kc == 7, ["Win", hk], [pk])
                v_, vk = vsb.next()
                P.cp("act", v_[:, :, 0:64], ps[:, :].rearrange("p (h d) -> p h d", d=64), [pk], [vk])
                P.dma("sp", self.va[ti * 128:(ti + 1) * 128, :, :], v_[:], [vk], ())
                ps, pk = psu.next()
                for kc in range(8):
                    P.mm(ps[:, :], hT[:, kc, tsl], Win[:, kc, 3072:3584], kc == 0, kc == 7, ["Win", hk], [pk])
                z_, zk = zsb.next()
                P.cp("dve", z_[:], ps[:, :], [pk], [zk])
                P.dma("sp", self.zd[ti * 128:(ti + 1) * 128, :], z_[:], [zk], ())
                ps, pk = psu.next()
                for kc in range(8):
                    P.mm(ps[:, 0:16], hT[:, kc, tsl], Win[:, kc, 3584:3600], kc == 0, kc == 7, ["Win", hk], [pk])
                g_, gk = gsb.next()
                P.cp("act", g_[:], ps[:, 0:16], [pk], [gk])
                P.dma("sp", self.grd[ti * 128:(ti + 1) * 128, :], g_[:], [gk], ())
        P.release(m)

    def phase_na(self, l, want_ctx):
        P, I = self.P, self.I
        i = l // 2
        m = P.mark()
        scale = 64.0 ** -0.5
        BB = P.sb("BB", [64, 15, 8, 64], F32)
        msk = P.sb("msk", [64, 64], F32)
        P.dma("sp", BB[:], I["rpbg"][i], w=["BB"])
        P.dma("sp", msk[:], I["namask"][:, :], w=["msk"])
        for dr in range(15):
            P.tt("pool", BB[:, dr, :, :], BB[:, dr, :, :], msk[:].unsqueeze(1).to_broadcast([64, 8, 64]), ALU.add, ["BB", "msk"], ["BB"])
            P.ts("pool", BB[:, dr, :, :], BB[:, dr, :, :], 1.0 / scale, ALU.mult, r=["BB"], w=["BB"])
        kcT = P.sb("kcT", [64, 8, 256], BF16)
        vcx = P.sb("vcx", [128, 2, 8, 65], BF16)
        P.dma("sp", kcT[:], self.kaT[:, :, 0:256].rearrange("h p t -> p h t"), w=["kcT"])
        P.dma("sp", vcx[:], self.va[0:256, :, :].rearrange("(kt p) h d -> p kt h d", p=128), (), ["vcx"])
        m2 = P.mark()
        kw = Rot(P, "kw", 2, [64, 8, 1024], BF16)
        vw = Rot(P, "vw", 2, [64, 16, 8, 65], BF16)
        qb = Rot(P, "qb", 2, [64, 8, 512], BF16)
        psS = Rot(P, "psS", 2, [64, 2, 8, 64], F32, psum=True)
        psC = Rot(P, "psC", 2, [128, 2, 2, 64], F32, psum=True)
        psO = Rot(P, "psO", 1, [128, 2, 65], F32, psum=True)
        stmp = Rot(P, "stmp", 2, [64, 2, 8, 64], F32)
        pb = Rot(P, "pb", 2, [64, 2, 8, 64], BF16)
        pcb = Rot(P, "pcb", 2, [128, 2, 2, 64], BF16)
        yo = Rot(P, "yo", 1, [64, 8, 8, 64], F32)
        for b8 in range(16):
            w0 = min(max(8 * b8 - 4, 0), 112)
            k_w, kk = kw.next(); v_w, vk = vw.next(); q_b, qk = qb.next()
            tk0 = CTX + w0 * 64
            P.dma("sp", k_w[:], self.kaT[:, :, tk0:tk0 + 1024].rearrange("h p t -> p h t"), (), [kk])
            P.dma("sp", v_w[:], self.va[tk0:tk0 + 1024, :, :].rearrange("(r p) h d -> p r h d", p=64), (), [vk])
            tq0 = CTX + b8 * 512
            P.dma("sp", q_b[:], self.qaT[:, :, tq0:tq0 + 512].rearrange("h p t -> p h t"), (), [qk])
            y_o, yk = yo.next()
            for rr in range(8):
                r = 8 * b8 + rr
                rs = min(max(r - 4, 0), 120)
                dr0 = rs - r + 7
                for hp2 in range(4):
                    ps, pk = psS.next()
                    pc, pck = psC.next()
                    for hh in range(2):
                        h = 2 * hp2 + hh
                        for i8 in range(8):
                            kr = rs + i8 - w0
                            P.mm(ps[:, hh, i8, :], k_w[:, h, kr * 64:(kr + 1) * 64], q_b[:, h, rr * 64:(rr + 1) * 64], True, True,
                                 [kk, qk], [pk])
                        for ct in range(2):
                            P.mm(pc[:, ct, hh, :], kcT[:, h, ct * 128:(ct + 1) * 128], q_b[:, h, rr * 64:(rr + 1) * 64], True, True,
                                 ["kcT", qk], [pck])
                    st, stk_ = stmp.next()
                    bias = BB[:, dr0:dr0 + 8, 2 * hp2:2 * hp2 + 2, :].rearrange("p r h q -> p h r q")
                    P.tt("dve", st[:], ps[:], bias, ALU.add, [pk, "BB"], [stk_])
                    p_b, pbk = pb.next()
                    P.act(p_b[:], st[:], AF.Exp, [stk_], [pbk], scale=scale)
                    p_c, pcbk = pcb.next()
                    P.act(p_c[:], pc[:], AF.Exp, [pck], [pcbk], scale=scale)
                    po, pok = psO.next()
                    for hh in range(2):
                        h = 2 * hp2 + hh
                        for i8 in range(8):
                            kr = rs + i8 - w0
                            P.mm(po[0:64, hh, :], p_b[:, hh, i8, :], v_w[:, kr, h, :], i8 == 0, False, [pbk, vk], [pok])
                        for ct in range(2):
                            P.mm(po[0:64, hh, :], p_c[:, ct, hh, :], vcx[:, ct, h, :], False, ct == 1, [pcbk, "vcx"], [pok])
                    sm, smk = self.small.next()
                    P.op("dve", lambda e, sm=sm, po=po: e.reciprocal(out=sm[0:64, 0:2], in_=po[0:64, :, 64]), [pok], [smk])
                    P.tt("dve", y_o[:, rr, 2 * hp2:2 * hp2 + 2, :], po[0:64, :, 0:64], sm[0:64, 0:2].unsqueeze(2).to_broadcast([64, 2, 64]),
                         ALU.mult, [pok, smk], [yk])
            P.dma("sp", self.attn[tq0:tq0 + 512, 0:512].rearrange("(r p) (h d) -> p r h d", p=64, d=64), y_o[:], [yk], ())
        P.release(m2)
        if want_ctx:
            psO = Rot(P, "psO", 1, [128, 2, 65], F32, psum=True)
            qc = P.sb("qc", [64, 8, 256], BF16)
            P.dma("sp", qc[:], self.qaT[:, :, 0:256].rearrange("h p t -> p h t"), w=["qc"])
            psX = Rot(P, "psX", 1, [128, 2, 256], F32, psum=True)
            pxb = Rot(P, "pxb", 2, [128, 2, 256], BF16)
            yc = P.sb("yc", [128, 2, 8, 64], F32)
            for h in range(8):
                ps, pk = psX.next()
                for kt in range(2):
                    P.mm(ps[:, kt, :], kcT[:, h, kt * 128:(kt + 1) * 128], qc[:, h, :], True, True, ["kcT", "qc"], [pk])
                px, pxk = pxb.next()
                P.act(px[:], ps[:], AF.Exp, [pk], [pxk], scale=scale)
                po, pok = psO.next()
                for qt in range(2):
                    for kt in range(2):
                        P.mm(po[:, qt, :], px[:, kt, qt * 128:(qt + 1) * 128], vcx[:, kt, h, :], kt == 0, kt == 1, [pxk, "vcx"], [pok])
                sm, smk = self.small.next()
                P.op("dve", lambda e, sm=sm, po=po: e.reciprocal(out=sm[:, 0:2], in_=po[:, :, 64]), [pok], [smk])
                P.tt("dve", yc[:, :, h, :], po[:, :, 0:64], sm[:, 0:2].unsqueeze(2).to_broadcast([128, 2, 64]), ALU.mult, [pok, smk], ["yc"])
            P.dma("sp", self.attn[0:256, 0:512].rearrange("(qt p) (h d) -> p qt h d", p=128, d=64), yc[:], ["yc"], ())
        P.release(m)

    def phase_gdn_pre(self, l):
        P, I = self.P, self.I
        i = l // 2
        m = P.mark()
        if not hasattr(self, "gT"):
            self.gT = P.dram("gT", [12, 128, T], F32)
            self.od = P.dram("od", [2, T, 512], F32)
        cw = P.sb("cw", [128, 12, 5], F32)
        P.dma("sp", cw[:], I["conv_wT"][i].rearrange("c p k -> p c k"), w=["cw"])
        onesf = P.sb("onesf", [128, 128], F32)
        P.memset("pool", onesf[:], 1.0, w=["onesf"])
        NP = 2048
        xp = Rot(P, "xp", 2, [128, NP + 4], F32)
        acc = Rot(P, "acc", 2, [128, NP], F32)
        yv = Rot(P, "yv", 2, [128, NP], F32)
        sq = Rot(P, "sq", 2, [128, NP], F32)
        rn = Rot(P, "rn", 2, [128, 512], F32)
        psn = Rot(P, "psn", 2, [128, 512], F32, psum=True)
        pieces = [(0, 256, 0, 256)] + [(256 + NP * k, 256 + NP * (k + 1), 256, T) for k in range(SEQ // NP)]
        for c in range(12):
            for (a, b, lo_, hi_) in pieces:
                n = b - a
                x_, xk = xp.next()
                lo = max(a - 2, lo_); hi = min(b + 2, hi_)
                if lo > a - 2:
                    P.memset("pool", x_[:, 0:2], 0.0, w=[xk])
                if hi < b + 2:
                    P.memset("pool", x_[:, n + 2:n + 4], 0.0, w=[xk])
                P.dma("sp", x_[:, lo - (a - 2):hi - (a - 2)], self.gpre[c, :, lo:hi], (), [xk])
                a_, ak = acc.next()
                P.ts("dve", a_[:, 0:n], x_[:, 0:n], cw[:, c, 0:1], ALU.mult, r=[xk, "cw"], w=[ak])
                for k in range(1, 5):
                    P.stt(a_[:, 0:n], x_[:, k:k + n], cw[:, c, k:k + 1], a_[:, 0:n], ALU.mult, ALU.add, [xk, "cw", ak], [ak])
                y_, yk = yv.next()
                P.act(y_[:, 0:n], a_[:, 0:n], AF.Silu, [ak], [yk])
                if c < 8:
                    s_, sk = sq.next()
                    P.act(s_[:, 0:n], y_[:, 0:n], AF.Square, [yk], [sk])
                    for q0 in range(0, n, 512):
                        w_ = min(512, n - q0)
                        ps, pk = psn.next()
                        P.mm(ps[:, 0:w_], onesf[:], s_[:, q0:q0 + w_], True, True, ["onesf", sk], [pk])
                        r_, rk = rn.next()
                        mul = 128.0 if c < 4 else 1.0
                        P.act(r_[:, 0:w_], ps[:, 0:w_], AF.Sqrt, [pk], [rk], scale=mul, bias=mul * EPS)
                        P.op("dve", lambda e, r_=r_, w_=w_: e.reciprocal(out=r_[:, 0:w_], in_=r_[:, 0:w_]), [rk], [rk])
                        P.tt("dve", y_[:, q0:q0 + w_], y_[:, q0:q0 + w_], r_[:, 0:w_], ALU.mult, [yk, rk], [yk])
                P.dma("sp", self.gT[c, :, a:b], y_[:, 0:n], [yk], ())
        P.release(m)

    def phase_gdn(self, l):
        P, I = self.P, self.I
        i = l // 2
        m = P.mark()
        sbc = lambda n: P.sb(n, [128, 128], F32)
        Ball = sbc("Ball"); P.memset("pool", Ball[:], 0.0, w=["Ball"])
        P.memset("pool", Ball[0:64, 0:64], 1.0, w=["Ball"]); P.memset("pool", Ball[64:128, 64:128], 1.0, w=["Ball"])
        INf = sbc("INf"); INb = sbc("INb"); STf = sbc("STf"); STb = sbc("STb")
        for (dst, coef, cm, base) in ((INf, -1, 1, 0), (INb, 1, -1, 0), (STf, -1, 1, -1), (STb, 1, -1, -1)):
            P.op("pool", lambda e, dst=dst, coef=coef, cm=cm, base=base: e.affine_select(
                out=dst[:], in_=Ball[:], pattern=[[coef, 128]], compare_op=ALU.is_ge, fill=0.0, base=base, channel_multiplier=cm),
                ["Ball"], [dst.name])
        MNf = sbc("MNf"); MNb = sbc("MNb")
        P.ts("dve", MNf[:], INf[:], -1.0, ALU.add, -NEG, ALU.mult, r=[INf.name], w=["MNf"])
        P.ts("dve", MNb[:], INb[:], -1.0, ALU.add, -NEG, ALU.mult, r=[INb.name], w=["MNb"])
        CM = [sbc("CM0"), sbc("CM1")]
        BM = [sbc("BM0"), sbc("BM1")]
        SEL = [sbc("SEL0"), sbc("SEL1")]
        for c in range(2):
            for t_ in (CM[c], BM[c], SEL[c]):
                P.memset("pool", t_[:], 0.0, w=[t_.name])
            P.memset("pool", CM[c][:, c * 64:(c + 1) * 64], 1.0, w=[CM[c].name])
            P.memset("pool", BM[c][c * 64:(c + 1) * 64, c * 64:(c + 1) * 64], 1.0, w=[BM[c].name])
            P.memset("pool", SEL[c][c * 64:(c + 1) * 64, :], 1.0 / 64, w=[SEL[c].name])
        cnames = [Ball.name, INf.name, INb.name, STf.name, STb.name, "MNf", "MNb"] + [t_.name for t_ in CM + BM + SEL]
        MN = [MNf, MNb]; ST = [STf, STb]
        Lc = [INb, INf]
        ab = P.sb("ab", [128, 2, 8], F32)
        P.dma("sp", ab[:], I["gdn_ab"][i:i + 1, :, :].to_broadcast([128, 2, 8]), w=["ab"])
        negA = P.sb("negA", [128, 8], F32)
        P.act(negA[:], ab[:, 0, :], AF.Exp, ["ab"], ["negA"])
        P.ts("dve", negA[:], negA[:], -1.0, ALU.mult, r=["negA"], w=["negA"])

        psg = Rot(P, "psg", 8, [128, 128], F32, psum=True)
        grt = Rot(P, "grt", 2, [128, 16], F32)
        gtmp = Rot(P, "gtmp", 3, [128, 16], F32)
        gsc = Rot(P, "gsc", 6, [128, 6, 8], F32)
        bnd = Rot(P, "bnd", 6, [128, 2, 8], F32)

        def gates(ti):
            g_, gk = grt.next()
            P.dma("sp", g_[:], self.grd[ti * 128:(ti + 1) * 128, :], (), [gk])
            g4 = g_[:].rearrange("p (d k h) -> p d k h", d=2, k=2)
            t_, tk = gtmp.next()
            t4 = t_[:].rearrange("p (d k h) -> p d k h", d=2, k=2)
            sc, sk = gsc.next()
            P.tt("dve", t4[:, :, 0, :], g4[:, :, 0, :], ab[:, 1, :].rearrange("p (d h) -> p d h", d=2), ALU.add, [gk, "ab"], [tk])
            P.act(t4[:, :, 0, :], t4[:, :, 0, :], AF.Exp, [tk], [tk])
            P.act(t4[:, :, 0, :], t4[:, :, 0, :], AF.Ln, [tk], [tk], bias=1.0)
            P.tt("dve", t4[:, :, 0, :], t4[:, :, 0, :], negA[:].rearrange("p (d h) -> p d h", d=2), ALU.mult, [tk, "negA"], [tk])
            P.act(sc[:, 4, :].rearrange("p (d h) -> p d h", d=2), g4[:, :, 1, :], AF.Sigmoid, [gk], [sk])
            gg = t_[:].rearrange("p (d k h) -> p d k h", d=2, k=2)
            gcomp, gck = gtmp.next()
            P.cp("dve", gcomp[:, 0:8].rearrange("p (d h) -> p d h", d=2), gg[:, :, 0, :], [tk], [gck])
            ps, pk = psg.t[0], (psg.name, 0)
            P.mm(ps[:, 0:4], Lc[0][:], gcomp[:, 0:4], True, True, [Lc[0].name, gck], [pk])
            P.mm(ps[:, 4:8], Lc[1][:], gcomp[:, 4:8], True, True, [Lc[1].name, gck], [pk])
            P.mm(ps[:, 8:16], Ball[:], gcomp[:, 0:8], True, True, [Ball.name, gck], [pk])
            gps_sb, gpk = gtmp.next()
            P.cp("act", gps_sb[:], ps[:, 0:16], [pk], [gpk])
            P.cp("act", sc[:, 0, :], gps_sb[:, 0:8], [gpk], [sk])
            P.ts("dve", sc[:, 1, :], gps_sb[:, 0:8], -1.0, ALU.mult, r=[gpk], w=[sk])
            P.act(sc[:, 2, :], gps_sb[:, 0:8], AF.Exp, [gpk], [sk])
            P.tt("dve", sc[:, 5, :], gps_sb[:, 8:16], sc[:, 0, :], ALU.subtract, [gpk, sk], [sk])
            P.act(sc[:, 5, :], sc[:, 5, :], AF.Exp, [sk], [sk])
            P.stt(sc[:, 3, :], sc[:, 2, :], -1.0, sc[:, 4, :], ALU.mult, ALU.mult, [sk], [sk])
            P.cp("act", gcomp[:, 8:16], gps_sb[:, 8:16], [gpk], [gck])
            ps2, pk2 = psg.t[1], (psg.name, 1)
            for c in range(2):
                P.mm(ps2[:, c * 8:(c + 1) * 8], SEL[c][:], gcomp[:, 8:16], True, True, [SEL[c].name, gck], [pk2])
            b_, bk = bnd.next()
            P.act(b_[:].rearrange("p c e -> p (c e)"), ps2[:, 0:16], AF.Exp, [pk2], [bk])
            return sc, sk, b_, bk

        NCH = 8
        wk4 = [[P.sb("gwk%d_%d" % (ch, j), [128, 128], F32) for j in range(4)] for ch in range(NCH)]
        PAs = [[P.sb("PA%d_%d" % (ch, j), [128, 128], F32) for j in range(2)] for ch in range(NCH)]
        PTs = [[P.sb("PT%d_%d" % (ch, j), [128, 128], F32) for j in range(2)] for ch in range(NCH)]
        TTs = [P.sb("TT%d" % ch, [128, 128], F32) for ch in range(NCH)]
        lds = [P.sb("ld%d" % ch, [128, 3, 128], F32) for ch in range(NCH)]
        prep = [[P.sb("prep%d_%d" % (ch, j), [128, 9, 128], F32) for j in range(2)] for ch in range(NCH)]
        prep_i = [0] * NCH
        Sst = [[P.sb("S%d_%d" % (ch, j), [128, 128], F32) for j in range(2)] for ch in range(NCH)]
        S_i = [0] * NCH
        for ch in range(NCH):
            P.memset("pool", Sst[ch][0][:], 0.0, w=[("S", ch, 0)])
        rr_ = [P.sb("rr%d" % ch, [128, 128], F32) for ch in range(NCH)]
        uu = [P.sb("uu%d" % ch, [128, 128], F32) for ch in range(NCH)]
        osb = [P.sb("gosb%d" % ch, [128, 128], F32) for ch in range(NCH)]

        def prep_tile(ti, h, d, sc, sk):
            ch = d * 4 + h; c8 = ch
            j = prep_i[ch]; prep_i[ch] ^= 1
            pr = prep[ch][j]; prk = ("prep", ch, j)
            return pr, prk, prep_gen(ti, h, d, sc, sk, ch, pr, prk)

        free_ps = list(zip(psg.t, [(psg.name, j) for j in range(psg.n)]))

        def grab(n):
            while len(free_ps) < n:
                yield
            return [free_ps.pop(0) for _ in range(n)]

        def give(*tiles):
            free_ps.extend(tiles)

        def prep_gen(ti, h, d, sc, sk, ch, pr, prk):
            c8 = ch
            ld, ldk = lds[ch], ("ld", ch)
            for w_, cc in enumerate((h, 4 + h, 8 + h)):
                P.dma("sp", ld[:, w_, :], self.gT[cc, :, ti * 128:(ti + 1) * 128], (), [ldk])
            qT, kT, vT = ld[:, 0, :], ld[:, 1, :], ld[:, 2, :]
            gcol = lambda w_: sc[:, w_, c8:c8 + 1]
            (gcB, Dm, dT, EG) = wk4[ch]
            gcBk, Dmk, dTk, EGk = [("wk", ch, q) for q in range(4)]
            P.cp("pool", gcB[:], gcol(0).to_broadcast([128, 128]), [sk], [gcBk])
            yield
            (g_,) = yield from grab(1)
            Gps, Gk = g_
            P.mm(Gps[:], gcB[:], self.identf[:], True, True, [gcBk, "identf"], [Gk])
            yield
            P.stt(Dm[:], Gps[:], -1.0, MN[d][:], ALU.mult, ALU.add, [Gk, "MNf" if d == 0 else "MNb"], [Dmk])
            P.tt("dve", dT[:], Gps[:], MN[1 - d][:], ALU.add, [Gk, "MNf" if d == 1 else "MNb"], [dTk])
            P.act(EG[:], Gps[:], AF.Exp, [Gk, Dmk, dTk], [EGk])
            give(g_)
            yield
            P.act(Dm[:], Dm[:], AF.Exp, [Dmk, sk], [Dmk], bias=gcol(0))
            P.act(dT[:], dT[:], AF.Exp, [dTk, sk], [dTk], bias=gcol(1))
            yield
            P.tt("pool", Dm[:], Dm[:], ST[d][:], ALU.mult, [Dmk, ST[d].name], [Dmk])
            (g_,) = yield from grab(1)
            KKp, KKk = g_
            P.mm(KKp[:], kT, kT, True, True, [ldk], [KKk])
            yield
            pa = PAs[ch]; pt = PTs[ch]
            A_, Ak = pa[0], ("PA", ch, 0)
            P.stt(A_[:], KKp[:], gcol(4), Dm[:], ALU.mult, ALU.mult, [KKk, sk, Dmk], [Ak])
            give(g_)
            yield
            (g_,) = yield from grab(1)
            ATp, ATpk = g_
            P.tr(ATp[:], A_[:], self.identf[:], [Ak, "identf"], [ATpk])
            yield
            AT, ATk = pt[0], ("PT", ch, 0)
            P.cp("act", AT[:], ATp[:], [ATpk], [ATk])
            give(g_)
            yield
            TT, TTk = TTs[ch], ("TT", ch)
            P.tt("dve", TT[:], self.identf[:], AT[:], ALU.subtract, ["identf", ATk], [TTk])
            Pn, Pnk, PTn, PTnk = A_, Ak, AT, ATk
            for lev in range(5):
                gs = yield from grab(2 if lev < 4 else 1)
                p2, p2k = gs[0]
                P.mm(p2[:], PTn[:], Pn[:], True, True, [PTnk, Pnk], [p2k])
                if lev < 4:
                    p2t, p2tk = gs[1]
                    P.mm(p2t[:], Pn[:], PTn[:], True, True, [PTnk, Pnk], [p2tk])
                yield
                q_ = (lev + 1) % 2
                Pn2, Pn2k = pa[q_], ("PA", ch, q_)
                P.cp("act", Pn2[:], p2[:], [p2k], [Pn2k])
                if lev < 4:
                    PTn2, PTn2k = pt[q_], ("PT", ch, q_)
                    P.cp("dve", PTn2[:], p2t[:], [p2tk], [PTn2k])
                give(*gs)
                yield
                (g_,) = yield from grab(1)
                up, upk = g_
                P.mm(up[:], Pn2[:], TT[:], True, True, [Pn2k, TTk], [upk])
                yield
                P.tt("dve", TT[:], TT[:], up[:], ALU.add, [TTk, upk], [TTk])
                give(g_)
                Pn, Pnk = Pn2, Pn2k
                if lev < 4:
                    PTn, PTnk = PTn2, PTn2k
            yield
            for c in range(2):
                P.tt("pool", pr[:, c, :], TT[:], BM[c][:], ALU.mult, [TTk, BM[c].name], [prk])
            P.tt("pool", EG[:], EG[:], qT, ALU.mult, [EGk, ldk], [EGk])
            gs = yield from grab(3)
            (Pp, Ppk), (kp, kpk), (vp, vpk) = gs
            P.mm(Pp[:], kT, qT, True, True, [ldk], [Ppk])
            P.tr(kp[:], kT, self.identf[:], [ldk, "identf"], [kpk])
            P.tr(vp[:], vT, self.identf[:], [ldk, "identf"], [vpk])
            yield
            P.tt("dve", pr[:, 2, :], Pp[:], dT[:], ALU.mult, [Ppk, dTk], [prk])
            P.act(pr[:, 7, :], kp[:], AF.Copy, [kpk, sk], [prk], scale=gcol(5))
            P.act(pr[:, 8, :], vp[:], AF.Copy, [vpk, sk], [prk], scale=gcol(4))
            give(*gs)
            for c in range(2):
                P.tt("pool", pr[:, 3 + c, :], EG[:], CM[c][:], ALU.mult, [EGk, CM[c].name], [prk])
                P.tt("pool", pr[:, 5 + c, :], kT, CM[c][:], ALU.mult, [ldk, CM[c].name], [prk])
            yield

        def chain_gen(ti, h, d, pr, prk, sc, sk, b_, bk):
            ch = d * 4 + h; c8 = ch
            o_, ok = osb[ch], ("osb", ch)
            r_, rk = rr_[ch], ("rr", ch)
            u_, uk = uu[ch], ("uu", ch)
            order = (0, 1) if d == 0 else (1, 0)
            for n_, c in enumerate(order):
                si = S_i[ch]; S_i[ch] ^= 1
                S = Sst[ch][si]; Sk = ("S", ch, si)
                S2 = Sst[ch][si ^ 1]; S2k = ("S", ch, si ^ 1)
                (g_,) = yield from grab(1)
                ks, ksk = g_
                P.mm(ks[:], pr[:, 5 + c, :], S[:], True, True, [prk, Sk], [ksk])
                yield
                P.stt(r_[:], ks[:], sc[:, 3, c8:c8 + 1], pr[:, 8, :], ALU.mult, ALU.add, [ksk, sk, prk], [rk])
                give(g_)
                yield
                (g_,) = yield from grab(1)
                up, upk = g_
                P.mm(up[:], pr[:, c, :], r_[:], True, True, [prk, rk], [upk])
                yield
                P.cp("act", u_[:], up[:], [upk], [uk])
                give(g_)
                yield
                gs = yield from grab(2)
                (op_, opk), (ds, dsk) = gs
                P.mm(op_[:], pr[:, 3 + c, :], S[:], True, False, [prk, Sk], [opk])
                P.mm(op_[:], pr[:, 2, :], u_[:], False, True, [prk, uk], [opk])
                P.mm(ds[:], pr[:, 7, :], u_[:], True, True, [prk, uk], [dsk])
                yield
                P.stt(S2[:], S[:], b_[:, c, c8:c8 + 1], ds[:], ALU.mult, ALU.add, [Sk, bk, dsk], [S2k])
                if n_ == 0:
                    P.cp("act", o_[:], op_[:], [opk], [ok])
                else:
                    P.tt("dve", o_[:], o_[:], op_[:], ALU.add, [ok, opk], [ok])
                give(*gs)
                yield
            P.dma("sp", self.od[d, ti * 128:(ti + 1) * 128, h * 128:(h + 1) * 128], o_[:], [ok], ())

        def run_rr(gens):
            gens = list(gens)
            while gens:
                alive = []
                for g in gens:
                    try:
                        next(g); alive.append(g)
                    except StopIteration:
                        pass
                gens = alive

        fwd_order = list(range(NT))
        bwd_order = [1, 0] + list(range(NT - 1, 1, -1))

        def make_preps(s_):
            tf_, tb_ = fwd_order[s_], bwd_order[s_]
            gf = gates(tf_)
            gb = gates(tb_)
            out = []
            for h in range(4):
                out.append((tf_, h, 0, prep_tile(tf_, h, 0, gf[0], gf[1]), gf))
                out.append((tb_, h, 1, prep_tile(tb_, h, 1, gb[0], gb[1]), gb))
            return out

        cur = make_preps(0)
        run_rr([p[3][2] for p in cur])
        for s_ in range(NT):
            nxt = make_preps(s_ + 1) if s_ + 1 < NT else []
            chains = [chain_gen(ti, h, d, pr[0], pr[1], g[0], g[1], g[2], g[3]) for (ti, h, d, pr, g) in cur]
            run_rr([p[3][2] for p in nxt] + chains)
            cur = nxt
        P.release(m)

    def phase_gdn_out(self, l):
        P, I = self.P, self.I
        i = l // 2
        m = P.mark()
        gng = P.sb("gng", [128, 128], F32)
        P.dma("sp", gng[:], I["gdn_norm_g"][i:i + 1, :].to_broadcast([128, 128]), w=["gng"])
        of = Rot(P, "of", 2, [128, 512], F32)
        ob = Rot(P, "ob", 2, [128, 512], F32)
        zt = Rot(P, "zt", 2, [128, 512], F32)
        yt = Rot(P, "yt", 2, [128, 512], F32)
        for ti in range(NT):
            o1, k1 = of.next(); o2, k2 = ob.next(); z_, zk = zt.next()
            P.dma("sp", o1[:], self.od[0, ti * 128:(ti + 1) * 128, :], (), [k1])
            P.dma("sp", o2[:], self.od[1, ti * 128:(ti + 1) * 128, :], (), [k2])
            P.dma("sp", z_[:], self.zd[ti * 128:(ti + 1) * 128, :], (), [zk])
            P.tt("dve", o1[:], o1[:], o2[:], ALU.add, [k1, k2], [k1])
            P.act(z_[:], z_[:], AF.Silu, [zk], [zk])
            y_, yk = yt.next()
            for h in range(4):
                hs = slice(h * 128, (h + 1) * 128)
                r, rk = self.rstd(o1[:, hs], 128, k1)
                P.stt(y_[:, hs], o1[:, hs], r, gng[:], ALU.mult, ALU.mult, [k1, rk, "gng"], [yk])
            P.tt("dve", y_[:], y_[:], z_[:], ALU.mult, [yk, zk], [yk])
            P.dma("sp", self.attn[ti * 128:(ti + 1) * 128, 512:1024], y_[:], [yk], ())
        P.release(m)

    def phase_even(self, l, want_ctx):
        self.phase_even_proj(l)
        self.phase_na(l, want_ctx)
        self.phase_gdn_pre(l)
        self.phase_gdn(l)
        self.phase_gdn_out(l)
```

```python
import numpy as np
import concourse.bass as bass
import concourse.mybir as mybir
from concourse.bass_utils import run_bass_kernel_spmd

F32 = mybir.dt.float32
BF16 = mybir.dt.bfloat16
U32 = mybir.dt.uint32
AF = mybir.ActivationFunctionType
ALU = mybir.AluOpType
AX = mybir.AxisListType

D = 1024
SEQ = 8192
CTX = 256
T = SEQ + CTX
NT = T // 128
DEPTH = 4
EPS = 1e-6
ENGS = ("pe", "act", "dve", "pool", "sp")
DMA_SEMS_PER_Q = 24
NEG = -30000.0


class Prog:
    def __init__(self):
        self.nc = bass.Bass("TRN2", target_bir_lowering=False)
        nc = self.nc
        self.E = {"pe": nc.tensor, "act": nc.scalar, "dve": nc.vector, "pool": nc.gpsimd, "sp": nc.sync}
        self.cnt = {e: 0 for e in ENGS}
        self.waited = {e: {} for e in ENGS}
        self.last_w = {}
        self.readers = {}
        self.dma_rr = {e: 0 for e in ENGS}
        self.dma_tot = {}
        self.sems = {}
        self.ctxs = []
        self.n_inst = 0
        self._semctx = set()
        self.uid = 0

    def sb(self, name, shape, dt=F32):
        self.uid += 1; name = "%s_%d" % (name, self.uid)
        c = self.nc.sbuf_tensor(name, list(shape), dt)
        t = c.__enter__(); self.ctxs.append(c)
        return t

    def ps(self, name, shape, dt=F32):
        self.uid += 1; name = "%s_%d" % (name, self.uid)
        c = self.nc.psum_tensor(name, list(shape), dt)
        t = c.__enter__(); self.ctxs.append(c)
        return t

    def psv(self, name, shape, dt=F32):
        esz = 4 if dt == F32 else 2
        per = 1
        for d in shape[1:]:
            per *= d
        nb = (per * esz + 2047) // 2048
        t = self.ps(name, [128, nb * (2048 // esz)], dt)
        v = t[0:shape[0], 0:per]
        if len(shape) > 2:
            names = " ".join("d%d" % i for i in range(1, len(shape)))
            v = v.rearrange("p (%s) -> p %s" % (names, names), **{"d%d" % i: shape[i] for i in range(2, len(shape))})
        return v

    def dram(self, name, shape, dt=F32, kind="Internal"):
        return self.nc.dram_tensor(name, list(shape), dt, kind=kind).ap()

    def _sem(self, n):
        if n not in self.sems:
            c = self.nc.semaphore(n); self.sems[n] = c.__enter__(); self.ctxs.append(c); self._semctx.add(c)
        return self.sems[n]

    def _need(self, eng, sem, val):
        w = self.waited[eng]
        if w.get(sem, 0) >= val:
            return
        w[sem] = val
        self.E[eng].wait_ge(self._sem(sem), val)

    def _deps(self, eng, reads, writes):
        for k in reads:
            lw = self.last_w.get(k)
            if lw is not None:
                self._need(eng, *lw)
        for k in writes:
            lw = self.last_w.get(k)
            if lw is not None:
                self._need(eng, *lw)
            for r in self.readers.get(k, ()):
                self._need(eng, *r)

    def _commit(self, tok, reads, writes):
        for k in reads:
            self.readers.setdefault(k, []).append(tok)
        for k in writes:
            self.last_w[k] = tok
            self.readers[k] = []

    def op(self, eng, fn, reads=(), writes=()):
        self._deps(eng, reads, writes)
        self.cnt[eng] += 1
        tok = ("S_" + eng, self.cnt[eng])
        fn(self.E[eng]).then_inc(self._sem(tok[0]), 1)
        if eng == "pe":
            self.waited[eng][tok[0]] = tok[1]
        self._commit(tok, reads, writes)
        self.n_inst += 1

    def dma(self, q, out, in_, r=(), w=(), **kw):
        reads, writes = r, w
        self._deps(q, reads, writes)
        i = self.dma_rr[q]; self.dma_rr[q] = (i + 1) % DMA_SEMS_PER_Q
        sem = "D_%s_%d" % (q, i)
        prev = self.dma_tot.get(sem, 0)
        if prev:
            self._need(q, sem, prev)
        tot = prev + 16
        self.dma_tot[sem] = tot
        self.E[q].dma_start(out=out, in_=in_, **kw).then_inc(self._sem(sem), 16)
        self._commit((sem, tot), reads, writes)
        self.n_inst += 1

    def mark(self):
        return len(self.ctxs)

    def barrier(self):
        for x in ENGS:
            for e in ENGS:
                if self.cnt[e]:
                    self._need(x, "S_" + e, self.cnt[e])
            for sem, tot in self.dma_tot.items():
                self._need(x, sem, tot)

    def release(self, m):
        self.barrier()
        keep = []
        for c in reversed(self.ctxs[m:]):
            if c in self._semctx:
                keep.append(c)
            else:
                c.__exit__(None, None, None)
        self.ctxs = self.ctxs[:m] + list(reversed(keep))

    def finish(self, final_keys):
        for k in final_keys:
            lw = self.last_w.get(k)
            if lw is not None:
                self._need("sp", *lw)
        for c in reversed(self.ctxs):
            c.__exit__(None, None, None)
        return self.nc

    def mm(self, out, lhsT, rhs, start, stop, r=(), w=()):
        self.op("pe", lambda e: e.matmul(out, lhsT=lhsT, rhs=rhs, start=start, stop=stop), r, w)

    def tr(self, out, in_, ident, r=(), w=()):
        self.op("pe", lambda e: e.transpose(out=out, in_=in_, identity=ident), r, w)

    def act(self, out, in_, func, r=(), w=(), **kw):
        self.op("act", lambda e: e.activation(out=out, in_=in_, func=func, **kw), r, w)

    def tt(self, eng, out, in0, in1, op, r=(), w=()):
        self.op(eng, lambda e: e.tensor_tensor(out=out, in0=in0, in1=in1, op=op), r, w)

    def ts(self, eng, out, in0, s1, op0, s2=None, op1=None, r=(), w=(), **kw):
        if op1 is None:
            self.op(eng, lambda e: e.tensor_scalar(out=out, in0=in0, scalar1=s1, scalar2=None, op0=op0, **kw), r, w)
        else:
            self.op(eng, lambda e: e.tensor_scalar(out=out, in0=in0, scalar1=s1, scalar2=s2, op0=op0, op1=op1, **kw), r, w)

    def stt(self, out, in0, scalar, in1, op0, op1, r=(), w=()):
        self.op("dve", lambda e: e.scalar_tensor_tensor(out=out, in0=in0, scalar=scalar, in1=in1, op0=op0, op1=op1), r, w)

    def cp(self, eng, out, in_, r=(), w=()):
        if eng == "act":
            self.op("act", lambda e: e.copy(out=out, in_=in_), r, w)
        else:
            self.op(eng, lambda e: e.tensor_copy(out=out, in_=in_), r, w)

    def memset(self, eng, ap, val, w=()):
        self.op(eng, lambda e: e.memset(ap, val), (), w)


class Rot:
    def __init__(self, P, name, n, shape, dt, psum=False):
        P.uid += 1
        self.name = "%s#%d" % (name, P.uid); self.n = n; self.i = 0
        if psum:
            self.t = [P.psv("%s%d" % (name, j), shape, dt) for j in range(n)]
        else:
            self.t = [P.sb("%s%d" % (name, j), shape, dt) for j in range(n)]

    def next(self):
        j = self.i; self.i = (j + 1) % self.n
        return self.t[j], (self.name, j)


def bc_row(ap_row, n):
    return ap_row.to_broadcast([128, n])


class K:
    def __init__(self, cfg):
        self.cfg = cfg
        self.P = Prog()
        P = self.P
        dr = lambda n, s, dt=F32: P.dram(n, s, dt, kind="ExternalInput")
        self.I = I = {}
        I["xin"] = dr("xin", [T, D])
        I["cond2"] = dr("cond2", [128, 8, 2])
        I["ada_w"] = dr("ada_w", [DEPTH, D, 6 * D])
        I["ada_b"] = dr("ada_b", [DEPTH, 6 * D])
        I["norm1_g"] = dr("norm1_g", [DEPTH, D])
        I["norm2_g"] = dr("norm2_g", [DEPTH, D])
        I["final_g"] = dr("final_g", [1, D])
        I["mla_w_in"] = dr("mla_w_in", [2, D, 704])
        I["mla_q_g"] = dr("mla_q_g", [2, 384])
        I["mla_kv_g"] = dr("mla_kv_g", [2, 256])
        I["mla_w_uq"] = dr("mla_w_uq", [2, 384, 1536])
        I["mla_w_ukv"] = dr("mla_w_ukv", [2, 256, 2048])
        I["mla_w_out"] = dr("mla_w_out", [2, D, D])
        I["cosT"] = dr("cosT", [64, T])
        I["sinT"] = dr("sinT", [64, T])
        I["peer_w_q"] = dr("peer_w_q", [DEPTH, D, 2048])
        I["skT"] = dr("skT", [DEPTH, 128, 16, 128])
        I["peer_uT"] = dr("peer_uT", [DEPTH, D, 16384])
        I["peer_v"] = dr("peer_v", [DEPTH, 16384, D])
        I["even_w_in"] = dr("even_w_in", [2, D, 3600])
        I["even_w_out"] = dr("even_w_out", [2, D, D])
        I["conv_wT"] = dr("conv_wT", [2, 12, 128, 5])
        I["gdn_ab"] = dr("gdn_ab", [2, 2, 8])
        I["gdn_norm_g"] = dr("gdn_norm_g", [2, 128])
        I["rpbg"] = dr("rpbg", [2, 64, 15, 8, 64])
        I["namask"] = dr("namask", [64, 64])
        self.out = P.dram("out", [SEQ, D], F32, kind="ExternalOutput")
        self.xres = P.dram("xres", [T, D], F32)
        self.ada_s = P.dram("ada_s", [DEPTH, 2, 6 * D], F32)
        self.attn = P.dram("attn", [T, D], F32)
        self.h2T = P.dram("h2T", [D, T], BF16)
        self.UV = P.dram("UV", [128, 128, 2048], BF16)
        self.GTd = [P.dram("GTd0", [34, 128, 128, 128], BF16), P.dram("GTd1", [NT - 34, 128, 128, 128], BF16)]
        self.sel = P.dram("sel", [NT, 128, 3, 128], F32)
        self.dbg = {}
        for name, shape in cfg.get("dbg", {}).items():
            self.dbg[name] = P.dram("dbg_" + name, list(shape), F32, kind="ExternalOutput")
        self.consts()

    def consts(self):
        P = self.P
        self.identf = P.sb("identf", [128, 128], F32)
        self.ident = P.sb("ident", [128, 128], BF16)
        P.memset("pool", self.identf[:], 1.0, w=["identf"])
        P.op("pool", lambda e: e.affine_select(out=self.identf[:], in_=self.identf[:], pattern=[[-1, 128]],
                                               compare_op=ALU.is_equal, fill=0.0, base=0, channel_multiplier=1),
             ["identf"], ["identf"])
        P.cp("dve", self.ident[:], self.identf[:], ["identf"], ["ident"])
        self.iotab = P.sb("iotab", [128, 128], BF16)
        self.iotaf = P.sb("iotaf", [128, 128], F32)
        P.op("pool", lambda e: e.iota(self.iotaf[:], pattern=[[1, 128]], base=0, channel_multiplier=0,
                                      allow_small_or_imprecise_dtypes=True), (), ["iotaf"])
        P.cp("dve", self.iotab[:], self.iotaf[:], ["iotaf"], ["iotab"])
        self.modA = [P.sb("modA%d" % s, [128, D], F32) for s in range(2)]
        self.modS = [P.sb("modS%d" % s, [128, D], F32) for s in range(2)]
        self.modG = [P.sb("modG%d" % s, [128, D], F32) for s in range(2)]
        self.ng = P.sb("ng", [128, D], F32)
        self.xt = Rot(P, "xt", 2, [128, D], F32)
        self.hb = Rot(P, "hb", 2, [128, D], BF16)
        self.tmpf = Rot(P, "tmpf", 2, [128, D], F32)
        self.small = Rot(P, "small", 4, [128, 4], F32)
        self.junk = P.sb("junk", [128, D], F32)

    def phase_ada(self):
        P, I = self.P, self.I
        m = P.mark()
        cs = P.sb("cs", [128, 8, 2], F32)
        P.dma("sp", cs[:], I["cond2"][:, :, :], w=["cs"])
        P.act(cs[:], cs[:], AF.Silu, ["cs"], ["cs"])
        adab = P.sb("adab", [2, 6 * D], F32)
        arow = P.sb("arow", [2, 6 * D], F32)
        wt = Rot(P, "adaw", 2, [128, 8, 512], F32)
        pa = Rot(P, "pada", 2, [128, 512], F32, psum=True)
        for l in self.cfg["layers"]:
            P.dma("sp", adab[:], I["ada_b"][l:l + 1, :].to_broadcast([2, 6 * D]), w=["adab"])
            wv = I["ada_w"][l].rearrange("(kc p) n -> p kc n", p=128)
            for j in range(12):
                w, wk = wt.next()
                P.dma("sp", w[:], wv[:, :, j * 512:(j + 1) * 512], w=[wk])
                ps, pk = pa.next()
                for kc in range(8):
                    P.mm(ps[0:2, :], cs[:, kc, :], w[:, kc, :], kc == 0, kc == 7, ["cs", wk], [pk])
                P.tt("dve", arow[:, j * 512:(j + 1) * 512], ps[0:2, :], adab[:, j * 512:(j + 1) * 512], ALU.add,
                     [pk, "adab"], ["arow"])
            P.dma("sp", self.ada_s[l, :, :], arow[:], ["arow"], [("ada_s", l)])
        P.release(m)

    def load_AS(self, l, which, streams=(0, 1)):
        P, I = self.P, self.I
        off = (which - 1) * 3 * D
        ngd = I["norm1_g"] if which == 1 else I["norm2_g"]
        P.dma("sp", self.ng[:], ngd[l:l + 1, :].to_broadcast([128, D]), w=["ng"])
        for s in streams:
            row = self.ada_s[l, s:s + 1, :]
            P.dma("sp", self.modS[s][:], row[:, off:off + D].to_broadcast([128, D]), [("ada_s", l)], [("modS", s)])
            P.dma("sp", self.modA[s][:], row[:, off + D:off + 2 * D].to_broadcast([128, D]), [("ada_s", l)], [("modA", s)])
            P.stt(self.modA[s][:], self.modA[s][:], 1.0, self.ng[:], ALU.add, ALU.mult, [("modA", s), "ng"], [("modA", s)])

    def load_gate(self, l, which, streams=(0, 1)):
        P = self.P
        off = (which - 1) * 3 * D
        for s in streams:
            row = self.ada_s[l, s:s + 1, :]
            P.dma("sp", self.modG[s][:], row[:, off + 2 * D:off + 3 * D].to_broadcast([128, D]), [("ada_s", l)], [("modG", s)])

    def rstd(self, x_ap, n, xk, eps=EPS):
        P = self.P
        sm, sk = self.small.next()
        P.act(self.junk[:, 0:n], x_ap, AF.Square, [xk], ["junk", sk], accum_out=sm[:, 0:1])
        P.act(sm[:, 1:2], sm[:, 0:1], AF.Sqrt, [sk], [sk], scale=1.0 / n, bias=eps)
        P.op("dve", lambda e: e.reciprocal(out=sm[:, 2:3], in_=sm[:, 1:2]), [sk], [sk])
        return sm[:, 2:3], sk

    def norm_mod_T(self, x_ap, xk, s, hT_dst, hT_key, pst):
        P = self.P
        r, rk = self.rstd(x_ap, D, xk)
        tf, tk = self.tmpf.next()
        P.stt(tf[:], x_ap, r, self.modA[s][:], ALU.mult, ALU.mult, [xk, rk, ("modA", s)], [tk])
        hb, hk = self.hb.next()
        P.tt("dve", hb[:], tf[:], self.modS[s][:], ALU.add, [tk, ("modS", s)], [hk])
        ps, pk = pst.next()
        for kc in range(8):
            P.tr(ps[:, kc, :], hb[:, kc * 128:(kc + 1) * 128], self.ident[:], [hk, "ident"], [pk])
        P.cp("act", hT_dst, ps[:], [pk], [hT_key])


def blocks(want_ctx=True):
    b = [(0, 256)] if want_ctx else []
    return b + [(256 + 512 * i, 512) for i in range(16)]


def rot_cols(P, eng, dst, src, r, w):
    for (d0, s0, sign) in ((0, 16, -1.0), (16, 0, 1.0), (32, 48, -1.0), (48, 32, 1.0)):
        P.ts(eng, dst[:, :, d0:d0 + 16], src[:, :, s0:s0 + 16], sign, ALU.mult, r=r, w=w)


class KM(K):
    def phase_init(self):
        P = self.P
        src = self.I["xin"]
        for i in range(0, NT, 6):
            P.dma("sp", self.xres[i * 128:(i + 6) * 128, :], src[i * 128:(i + 6) * 128, :], (),
                  [("x", j) for j in range(i, i + 6)])

    def phase_mla_proj(self, l):
        P, I = self.P, self.I
        i = l // 2
        m = P.mark()
        self.QnT = P.dram("QnT%d" % l, [8, 128, T], BF16)
        self.QrT = P.dram("QrT%d" % l, [8, 64, T], BF16)
        self.KnT = P.dram("KnT%d" % l, [8, 128, T], BF16)
        self.KrT = P.dram("KrT%d" % l, [64, T], BF16)
        self.Vd = P.dram("Vd%d" % l, [T, D], BF16)
        Win = P.sb("Win", [128, 8, 704], BF16)
        P.dma("pool", Win[:], I["mla_w_in"][i].rearrange("(kc p) n -> p kc n", p=128), w=["Win"])
        WinRot = P.sb("WinRot", [128, 8, 64], BF16)
        rot_cols(P, "dve", WinRot[:, :, :], Win[:, :, 640:704], ["Win"], ["WinRot"])
        Wuq = P.sb("Wuq", [128, 3, 1536], BF16)
        P.dma("pool", Wuq[:], I["mla_w_uq"][i].rearrange("(kc p) n -> p kc n", p=128), w=["Wuq"])
        WuqRot = P.sb("WuqRot", [128, 3, 8, 64], BF16)
        for kc in range(3):
            src = Wuq[:, kc, :].rearrange("p (h x) -> p h x", x=192)[:, :, 128:192]
            rot_cols(P, "dve", WuqRot[:, kc, :, :], src, ["Wuq"], ["WuqRot"])
        Wukv = P.sb("Wukv", [128, 2, 2048], BF16)
        P.dma("pool", Wukv[:], I["mla_w_ukv"][i].rearrange("(kc p) n -> p kc n", p=128), w=["Wukv"])
        qg = P.sb("qg", [128, 384], F32)
        kvg = P.sb("kvg", [128, 256], F32)
        P.dma("sp", qg[:], I["mla_q_g"][i:i + 1, :].to_broadcast([128, 384]), w=["qg"])
        P.dma("sp", kvg[:], I["mla_kv_g"][i:i + 1, :].to_broadcast([128, 256]), w=["kvg"])
        self.load_AS(l, 1)

        pst = Rot(P, "pst", 2, [128, 8, 128], BF16, psum=True)
        psp = Rot(P, "psp", 1, [128, 1024], F32, psum=True)
        psu = Rot(P, "psu", 3, [128, 512], F32, psum=True)
        hTr = Rot(P, "hT", 2, [128, 8, 512], BF16)
        cTr = Rot(P, "cT", 2, [128, 5, 512], BF16)
        cqn = Rot(P, "cqn", 2, [128, 640], BF16)
        cst = Rot(P, "cst", 2, [64, 2, 512], F32)
        stQn = Rot(P, "stQn", 2, [128, 8, 512], BF16)
        stKn = Rot(P, "stKn", 2, [128, 8, 512], BF16)
        stQr = Rot(P, "stQr", 2, [64, 8, 512], BF16)
        stKr = Rot(P, "stKr", 2, [64, 512], BF16)
        rtmp = Rot(P, "rtmp", 2, [64, 2, 512], F32)
        vsb = Rot(P, "vsb", 2, [128, D], BF16)

        def rope_out(ps1, k1, ps2, k2, cs, ck, n, dst, dk):
            rt, rk = rtmp.next()
            P.tt("dve", rt[:, 0, 0:n], ps1[0:64, 0:n], cs[:, 0, 0:n], ALU.mult, [k1, ck], [rk])
            P.tt("dve", rt[:, 1, 0:n], ps2[0:64, 0:n], cs[:, 1, 0:n], ALU.mult, [k2, ck], [rk])
            P.tt("dve", dst, rt[:, 0, 0:n], rt[:, 1, 0:n], ALU.add, [rk], [dk])

        for (t0, n) in blocks(True):
            s = 0 if t0 >= CTX else 1
            nt = n // 128
            hT, hk = hTr.next()
            cT, ck_ = cTr.next()
            cs, csk = cst.next()
            P.dma("sp", cs[:, 0, 0:n], I["cosT"][:, t0:t0 + n], w=[csk])
            P.dma("sp", cs[:, 1, 0:n], I["sinT"][:, t0:t0 + n], w=[csk])
            for j in range(nt):
                ti = t0 // 128 + j
                xt, xk = self.xt.next()
                P.dma("sp", xt[:], self.xres[ti * 128:(ti + 1) * 128, :], [("x", ti)], [xk])
                self.norm_mod_T(xt[:], xk, s, hT[:, :, j * 128:(j + 1) * 128], hk, pst)
                pp, ppk = psp.next()
                for kc in range(8):
                    P.mm(pp[:, 0:512], hT[:, kc, j * 128:(j + 1) * 128], Win[:, kc, 0:512], kc == 0, kc == 7, [hk, "Win"], [ppk])
                for kc in range(8):
                    P.mm(pp[:, 512:640], hT[:, kc, j * 128:(j + 1) * 128], Win[:, kc, 512:640], kc == 0, kc == 7, [hk, "Win"], [ppk])
                r1, r1k = self.rstd(pp[:, 0:384], 384, ppk)
                r2, r2k = self.rstd(pp[:, 384:640], 256, ppk)
                cq, cqk = cqn.next()
                P.stt(cq[:, 0:384], pp[:, 0:384], r1, qg[:], ALU.mult, ALU.mult, [ppk, r1k, "qg"], [cqk])
                P.stt(cq[:, 384:640], pp[:, 384:640], r2, kvg[:], ALU.mult, ALU.mult, [ppk, r2k, "kvg"], [cqk])
                ps, pk = pst.next()
                for kc in range(5):
                    P.tr(ps[:, kc, :], cq[:, kc * 128:(kc + 1) * 128], self.ident[:], [cqk, "ident"], [pk])
                P.cp("act", cT[:, :, j * 128:(j + 1) * 128], ps[:, 0:5, :], [pk], [ck_])
            sQn, sQnk = stQn.next(); sKn, sKnk = stKn.next(); sQr, sQrk = stQr.next(); sKr, sKrk = stKr.next()
            for h in range(8):
                ps, pk = psu.next()
                for kc in range(3):
                    P.mm(ps[:, 0:n], Wuq[:, kc, h * 192:h * 192 + 128], cT[:, kc, 0:n], kc == 0, kc == 2, ["Wuq", ck_], [pk])
                P.cp("act", sQn[:, h, 0:n], ps[:, 0:n], [pk], [sQnk])
                ps, pk = psu.next()
                for kc in range(2):
                    P.mm(ps[:, 0:n], Wukv[:, kc, h * 256:h * 256 + 128], cT[:, 3 + kc, 0:n], kc == 0, kc == 1, ["Wukv", ck_], [pk])
                P.cp("act", sKn[:, h, 0:n], ps[:, 0:n], [pk], [sKnk])
                ps1, k1 = psu.next()
                for kc in range(3):
                    P.mm(ps1[0:64, 0:n], Wuq[:, kc, h * 192 + 128:h * 192 + 192], cT[:, kc, 0:n], kc == 0, kc == 2, ["Wuq", ck_], [k1])
                ps2, k2 = psu.next()
                for kc in range(3):
                    P.mm(ps2[0:64, 0:n], WuqRot[:, kc, h, :], cT[:, kc, 0:n], kc == 0, kc == 2, ["WuqRot", ck_], [k2])
                rope_out(ps1, k1, ps2, k2, cs, csk, n, sQr[:, h, 0:n], sQrk)
            ps1, k1 = psu.next()
            for kc in range(8):
                P.mm(ps1[0:64, 0:n], Win[:, kc, 640:704], hT[:, kc, 0:n], kc == 0, kc == 7, ["Win", hk], [k1])
            ps2, k2 = psu.next()
            for kc in range(8):
                P.mm(ps2[0:64, 0:n], WinRot[:, kc, :], hT[:, kc, 0:n], kc == 0, kc == 7, ["WinRot", hk], [k2])
            rope_out(ps1, k1, ps2, k2, cs, csk, n, sKr[:, 0:n], sKrk)
            bk = ("mlab", t0)
            P.dma("sp", self.QnT[:, :, t0:t0 + n].rearrange("h p t -> p h t"), sQn[:, :, 0:n], [sQnk], [("QnT", t0)])
            P.dma("sp", self.KnT[:, :, t0:t0 + n].rearrange("h p t -> p h t"), sKn[:, :, 0:n], [sKnk], [("KnT", t0)])
            P.dma("sp", self.QrT[:, :, t0:t0 + n].rearrange("h p t -> p h t"), sQr[:, :, 0:n], [sQrk], [("QrT", t0)])
            P.dma("sp", self.KrT[:, t0:t0 + n], sKr[:, 0:n], [sKrk], [("KrT", t0)])
            for j in range(nt):
                vs, vk = vsb.next()
                for hf in range(2):
                    ps, pk = psu.next()
                    for kc in range(2):
                        rhs = Wukv[:, kc, :].rearrange("p (h x) -> p h x", x=256)[:, 4 * hf:4 * hf + 4, 128:256]
                        P.mm(ps[:, :], cT[:, 3 + kc, j * 128:(j + 1) * 128], rhs, kc == 0, kc == 1, ["Wukv", ck_], [pk])
                    P.cp("act", vs[:, hf * 512:(hf + 1) * 512], ps[:, :], [pk], [vk])
                ti = t0 // 128 + j
                P.dma("sp", self.Vd[ti * 128:(ti + 1) * 128, :], vs[:], [vk], [("Vd", ti)])
        P.release(m)

    def phase_mla_attn(self, l, want_ctx):
        P = self.P
        m = P.mark()
        scale = 192.0 ** -0.5
        kn = Rot(P, "kn", 2, [128, T], BF16)
        va = Rot(P, "va", 2, [128, NT, 129], BF16)
        kr = P.sb("kr", [64, T], BF16)
        qn = Rot(P, "qn", 2, [128, 512], BF16)
        qr = Rot(P, "qr", 2, [64, 512], BF16)
        pT = Rot(P, "pT", 4, [128, 512], BF16)
        psS = Rot(P, "psS", 3, [128, 512], F32, psum=True)
        psO = P.ps("psO", [128, 4, 512], F32)
        osb = Rot(P, "osb", 2, [128, 4, 128], F32)
        for t in va.t:
            P.memset("pool", t[:, :, 128:129], 1.0, w=[(va.name, va.t.index(t))])
        allq = [("QnT", b[0]) for b in blocks(True)]
        P.dma("sp", kr[:], self.KrT[:, :], [("KrT", b[0]) for b in blocks(True)], ["kr"])
        for h in range(8):
            k_t, kk = kn.next()
            v_t, vk = va.next()
            P.dma("sp", k_t[:], self.KnT[h, :, :], [("KnT", b[0]) for b in blocks(True)], [kk])
            for q4 in range(0, NT, 11):
                P.dma("sp", v_t[:, q4:q4 + 11, 0:128],
                      self.Vd[q4 * 128:(q4 + 11) * 128, h * 128:(h + 1) * 128].rearrange("(kt p) d -> p kt d", p=128),
                      [("Vd", ti) for ti in range(q4, q4 + 11)], [vk])
            for (t0, n) in blocks(want_ctx):
                nkt = NT if t0 >= CTX else 2
                q_t, qk = qn.next()
                qr_t, qrk = qr.next()
                P.dma("sp", q_t[:, 0:n], self.QnT[h, :, t0:t0 + n], [("QnT", t0)], [qk])
                P.dma("sp", qr_t[:, 0:n], self.QrT[h, :, t0:t0 + n], [("QrT", t0)], [qrk])
                nj = n // 128
                def s_stage(kt):
                    ps, pk = psS.next()
                    P.mm(ps[:, 0:n], k_t[:, kt * 128:(kt + 1) * 128], q_t[:, 0:n], True, False, [kk, qk], [pk])
                    P.mm(ps[:, 0:n], kr[:, kt * 128:(kt + 1) * 128], qr_t[:, 0:n], False, True, ["kr", qrk], [pk])
                    p_t, ptk = pT.next()
                    P.act(p_t[:, 0:n], ps[:, 0:n], AF.Exp, [pk], [ptk], scale=scale)
                    return p_t, ptk

                def pv_stage(kt, p_t, ptk):
                    for j in range(nj):
                        P.mm(psO[:, j, 0:129], p_t[:, j * 128:(j + 1) * 128], v_t[:, kt, :], kt == 0, kt == nkt - 1,
                             [ptk, vk], [("psO", j)])

                q_ = [s_stage(0)]
                if nkt > 1:
                    q_.append(s_stage(1))
                for kt in range(nkt):
                    if kt + 2 < nkt:
                        q_.append(s_stage(kt + 2))
                    pv_stage(kt, *q_.pop(0))
                o_t, ok = osb.next()
                for j in range(nj):
                    sm, sk = self.small.next()
                    P.op("dve", lambda e, sm=sm, j=j: e.reciprocal(out=sm[:, 0:1], in_=psO[:, j, 128:129]), [("psO", j)], [sk])
                    P.ts("dve", o_t[:, j, :], psO[:, j, 0:128], sm[:, 0:1], ALU.mult, r=[("psO", j), sk], w=[ok])
                P.dma("sp", self.attn[t0:t0 + n, h * 128:(h + 1) * 128].rearrange("(j p) d -> p j d", p=128),
                      o_t[:, 0:nj, :], [ok], [("attn", t0, h)])
        P.release(m)

    def phase_outproj(self, l, wout_ap, want_ctx):
        P = self.P
        m = P.mark()
        Wout = P.sb("Wout", [128, 8, D], BF16)
        P.dma("pool", Wout[:], wout_ap.rearrange("(kc p) n -> p kc n", p=128), w=["Wout"])
        self.load_gate(l, 1)
        self.load_AS(l, 2)
        pst = Rot(P, "pst", 2, [128, 8, 128], BF16, psum=True)
        psy = Rot(P, "psy", 2, [128, D], F32, psum=True)
        at = Rot(P, "at", 2, [128, D], F32)
        ab = Rot(P, "ab", 2, [128, D], BF16)
        aT = Rot(P, "aT", 2, [128, 8, 128], BF16)
        xn = Rot(P, "xn", 2, [128, D], F32)
        h2s = Rot(P, "h2s", 2, [128, 8, 128], BF16)
        def op_a(ti):
            a_t, ak = at.next()
            P.dma("sp", a_t[:], self.attn[ti * 128:(ti + 1) * 128, :], (), [ak])
            b_t, bk = ab.next()
            P.cp("act", b_t[:], a_t[:], [ak], [bk])
            ps, pk = pst.next()
            for kc in range(8):
                P.tr(ps[:, kc, :], b_t[:, kc * 128:(kc + 1) * 128], self.ident[:], [bk, "ident"], [pk])
            aT_t, aTk = aT.next()
            P.cp("act", aT_t[:], ps[:], [pk], [aTk])
            xt, xk = self.xt.next()
            P.dma("sp", xt[:], self.xres[ti * 128:(ti + 1) * 128, :], (), [xk])
            return ti, aT_t, aTk, xt, xk

        def op_b(ti, aT_t, aTk, xt, xk):
            s = 0 if ti >= 2 else 1
            py, pyk = psy.next()
            for hf in range(2):
                for kc in range(8):
                    P.mm(py[:, hf * 512:(hf + 1) * 512], aT_t[:, kc, :], Wout[:, kc, hf * 512:(hf + 1) * 512], kc == 0, kc == 7,
                         [aTk, "Wout"], [pyk])
            tf, tk = self.tmpf.next()
            P.tt("dve", tf[:], py[:], self.modG[s][:], ALU.mult, [pyk, ("modG", s)], [tk])
            x_n, xnk = xn.next()
            P.tt("dve", x_n[:], tf[:], xt[:], ALU.add, [tk, xk], [xnk])
            P.dma("sp", self.xres[ti * 128:(ti + 1) * 128, :], x_n[:], [xnk], [("x", ti)])
            h_t, hk = h2s.next()
            self.norm_mod_T(x_n[:], xnk, s, h_t[:], hk, pst)
            P.dma("sp", self.h2T[:, ti * 128:(ti + 1) * 128].rearrange("(kc p) t -> p kc t", p=128), h_t[:], [hk], [("h2T", ti)])

        tiles = list(range(0 if want_ctx else 2, NT))
        prev = op_a(tiles[0])
        for ii in range(len(tiles)):
            nxt = op_a(tiles[ii + 1]) if ii + 1 < len(tiles) else None
            op_b(*prev)
            prev = nxt
        P.release(m)

    def phase_peer_prep(self, l):
        P, I = self.P, self.I
        uT = I["peer_uT"][l].rearrange("(kc p) e -> p kc e", p=128)
        for c in range(128):
            P.dma("pool", self.UV[c, :, 0:1024].rearrange("p (kc e) -> p kc e", e=128), uT[:, :, c * 128:(c + 1) * 128], (), [("UV", c)])
            P.dma("pool", self.UV[c, :, 1024:2048], I["peer_v"][l, c * 128:(c + 1) * 128, :], (), [("UV", c)])

    def phase_peer_sel(self, l, want_ctx):
        P, I = self.P, self.I
        m = P.mark()
        Wq = P.sb("Wq", [128, 8, 2048], BF16)
        P.dma("pool", Wq[:], I["peer_w_q"][l].rearrange("(kc p) n -> p kc n", p=128), w=["Wq"])
        skT = P.sb("skT", [128, 16, 128], F32)
        P.dma("sp", skT[:], I["skT"][l], w=["skT"])
        h2 = Rot(P, "h2", 2, [128, 8, 128], BF16)
        qTsR = Rot(P, "qTs", 2, [128, 16, 128], F32)
        s_sbR = Rot(P, "s_sb", 2, [128, 16, 128], F32)
        s_tmp = P.sb("s_tmp", [128, 16, 128], F32)
        stop_ = P.sb("stop", [128, 16, 16], F32)
        sidx = P.sb("sidx", [128, 16, 16], U32)
        sidf = P.sb("sidf", [128, 16, 16], F32)
        comb = P.sb("comb", [128, 8, 256], F32)
        comb2 = P.sb("comb2", [128, 8, 256], F32)
        ctop = P.sb("ctop", [128, 8, 16], F32)
        cidx = P.sb("cidx", [128, 8, 16], U32)
        cj = P.sb("cj", [128, 2, 8, 16], U32)
        cjf = P.sb("cjf", [128, 2, 8, 16], F32)
        oh = P.sb("oh", [128, 8, 16, 16], F32)
        i12 = P.sb("i12", [128, 3, 128], F32)
        i12T = Rot(P, "i12T", 2, [128, 3, 128], F32)
        allA = [("stopA", hp) for hp in range(16)]; allB = [("stopB", hp) for hp in range(16)]
        allI = [("sidxA", hp) for hp in range(16)] + [("sidxB", hp) for hp in range(16)]
        nmx = P.sb("nmx", [128, 8], F32)
        zs = P.sb("zs", [128, 8], F32)
        psA = Rot(P, "psA", 3, [128, 512], F32, psum=True)
        psS = P.ps("psS4", [128, 4, 512], F32)
        iota16 = self.iotaf[:, 0:16]
        for ti in range(0 if want_ctx else 2, NT):
            h_t, hk = h2.next()
            qTs, qTsk = qTsR.next()
            s_sb, ssk = s_sbR.next()
            P.dma("sp", h_t[:], self.h2T[:, ti * 128:(ti + 1) * 128].rearrange("(kc p) t -> p kc t", p=128), (), [hk])
            for hp4 in range(4):
                ps, pk = psA.next()
                for q in range(4):
                    hp = hp4 * 4 + q
                    for kc in range(8):
                        P.mm(ps[:, q * 128:(q + 1) * 128], Wq[:, kc, hp * 128:(hp + 1) * 128], h_t[:, kc, :], kc == 0, kc == 7,
                             ["Wq", hk], [pk])
                P.cp("act", qTs[:, hp4 * 4:hp4 * 4 + 4, :], ps[:, :].rearrange("p (a n) -> p a n", n=128), [pk], [qTsk])
            for hp in range(16):
                P.mm(psS[:, hp // 4, (hp % 4) * 128:(hp % 4 + 1) * 128], qTs[:, hp, :], skT[:, hp, :], True, True,
                     [qTsk, "skT"], [("psS", hp // 4)])
            for g in range(4):
                P.cp("act", s_sb[:, 4 * g:4 * g + 4, :], psS[:, g, :].rearrange("p (a n) -> p a n", n=128), [("psS", g)], [ssk])
            for hp in range(16):
                P.op("dve", lambda e, hp=hp: e.max(out=stop_[:, hp, 0:8], in_=s_sb[:, hp, :]), [ssk], [("stopA", hp)])
            for hp in range(16):
                P.op("dve", lambda e, hp=hp: e.match_replace(out=s_tmp[:, hp, :], in_to_replace=stop_[:, hp, 0:8], in_values=s_sb[:, hp, :],
                                                             imm_value=-1e30), [ssk, ("stopA", hp)], [("s_tmp", hp)])
            for hp in range(16):
                P.op("dve", lambda e, hp=hp: e.max(out=stop_[:, hp, 8:16], in_=s_tmp[:, hp, :]), [("s_tmp", hp)], [("stopB", hp)])
            for hp in range(16):
                P.op("dve", lambda e, hp=hp: e.max_index(out=sidx[:, hp, 0:8], in_max=stop_[:, hp, 0:8], in_values=s_sb[:, hp, :]),
                     [ssk, ("stopA", hp)], [("sidxA", hp)])
            for hp in range(16):
                P.op("dve", lambda e, hp=hp: e.max_index(out=sidx[:, hp, 8:16], in_max=stop_[:, hp, 8:16], in_values=s_sb[:, hp, :]),
                     [ssk, ("stopB", hp)], [("sidxB", hp)])
            P.cp("dve", sidf[:], sidx[:], allI, ["sidf"])
            st4 = stop_[:].rearrange("p (h two) j -> p h two j", two=2)
            in0 = st4[:, :, 0, :].unsqueeze(3).to_broadcast([128, 8, 16, 16])
            in1 = st4[:, :, 1, :].unsqueeze(2).to_broadcast([128, 8, 16, 16])
            P.tt("dve", comb[:].rearrange("p h (a b) -> p h a b", b=16), in0, in1, ALU.add, allA + allB, ["comb"])
            for h in range(8):
                P.op("dve", lambda e, h=h: e.max(out=ctop[:, h, 0:8], in_=comb[:, h, :]), ["comb"], [("ctopA", h)])
            for h in range(8):
                P.op("dve", lambda e, h=h: e.match_replace(out=comb2[:, h, :], in_to_replace=ctop[:, h, 0:8], in_values=comb[:, h, :],
                                                           imm_value=-1e30), ["comb", ("ctopA", h)], [("comb2", h)])
            for h in range(8):
                P.op("dve", lambda e, h=h: e.max(out=ctop[:, h, 8:16], in_=comb2[:, h, :]), [("comb2", h)], [("ctopB", h)])
            for h in range(8):
                P.op("dve", lambda e, h=h: e.max_index(out=cidx[:, h, 0:8], in_max=ctop[:, h, 0:8], in_values=comb[:, h, :]),
                     ["comb", ("ctopA", h)], [("cidxA", h)])
            for h in range(8):
                P.op("dve", lambda e, h=h: e.max_index(out=cidx[:, h, 8:16], in_max=ctop[:, h, 8:16], in_values=comb[:, h, :]),
                     ["comb", ("ctopB", h)], [("cidxB", h)])
            ctk = [("ctopA", h) for h in range(8)] + [("ctopB", h) for h in range(8)]
            cik = [("cidxA", h) for h in range(8)] + [("cidxB", h) for h in range(8)]
            P.ts("dve", cj[:, 0, :, :], cidx[:], 4, ALU.logical_shift_right, r=cik, w=["cj"])
            P.ts("dve", cj[:, 1, :, :], cidx[:], 15, ALU.bitwise_and, r=cik, w=["cj"])
            P.cp("dve", cjf[:], cj[:], ["cj"], ["cjf"])
            sd4 = sidf[:].rearrange("p (h two) j -> p h two j", two=2)
            for w_ in range(2):
                P.tt("dve", oh[:], cjf[:, w_, :, :].unsqueeze(3).to_broadcast([128, 8, 16, 16]),
                     iota16.unsqueeze(1).unsqueeze(1).to_broadcast([128, 8, 16, 16]), ALU.is_equal, ["cjf", "iotaf"], ["oh"])
                P.tt("dve", oh[:], oh[:], sd4[:, :, w_, :].unsqueeze(2).to_broadcast([128, 8, 16, 16]), ALU.mult, ["oh", "sidf"], ["oh"])
                P.op("dve", lambda e, w_=w_: e.tensor_reduce(out=i12[:, w_, :].rearrange("p (h k) -> p h k", k=16), in_=oh[:],
                                                             axis=AX.X, op=ALU.add), ["oh"], ["i12"])
            P.ts("dve", nmx[:], ctop[:, :, 0], -1.0, ALU.mult, r=ctk, w=["nmx"])
            for h in range(8):
                P.act(i12[:, 2, h * 16:(h + 1) * 16], ctop[:, h, :], AF.Exp, ctk + ["nmx"], [("i12g", h), ("zs", h)],
                      bias=nmx[:, h:h + 1], scale=1.0, accum_out=zs[:, h:h + 1])
            zk_ = [("zs", h) for h in range(8)]; gk_ = [("i12g", h) for h in range(8)]
            P.op("dve", lambda e: e.reciprocal(out=zs[:], in_=zs[:]), zk_, zk_)
            g3 = i12[:, 2, :].rearrange("p (h k) -> p h k", k=16)
            P.tt("dve", g3, g3, zs[:].unsqueeze(2).to_broadcast([128, 8, 16]), ALU.mult, ["i12"] + zk_ + gk_, ["i12"] + gk_)
            iT, iTk = i12T.next()
            for w_ in range(3):
                ps, pk = psA.next()
                P.tr(ps[:, 0:128], i12[:, w_, :], self.identf[:], ["i12", "identf"] + gk_, [pk])
                P.cp("act", iT[:, w_, :], ps[:, 0:128], [pk], [iTk])
            P.dma("sp", self.sel[ti], iT[:], [iTk], [("sel", ti)])
        P.release(m)

    def phase_peer_g(self, l, want_ctx):
        P = self.P
        m = P.mark()
        i12T = Rot(P, "i12T", 2, [128, 3, 128], F32)
        i12Tb = Rot(P, "i12Tb", 2, [128, 3, 128], BF16)
        Aoh = Rot(P, "Aoh", 2, [128, 64, 128], BF16)
        Boh = Rot(P, "Boh", 2, [128, 64, 128], BF16)
        GTs = Rot(P, "GTs", 2, [128, 128, 128], BF16)
        ni2r = Rot(P, "ni2", 2, [128, 128], F32)
        abt = Rot(P, "abt", 4, [128, 128], F32)
        NB_DVE = 64
        psA = Rot(P, "psG", 4, [128, 512], F32, psum=True)
        for ti in range(0 if want_ctx else 2, NT):
            iT, iTk = i12T.next()
            P.dma("sp", iT[:], self.sel[ti], (), [iTk])
            G_, Gk = GTs.next()
            ni2, nik = ni2r.next()
            P.ts("dve", ni2[:], iT[:, 1, :], -1.0, ALU.mult, r=[iTk], w=[nik])
            for g64 in range(2):
                A_, Ak = Aoh.next(); B_, Bk = Boh.next()
                for tt_ in range(64):
                    t = g64 * 64 + tt_
                    P.ts("dve", A_[:, tt_, :], self.iotab[:], iT[:, 0, t:t + 1], ALU.is_equal, iT[:, 2, t:t + 1], ALU.mult,
                         r=[iTk, "iotab"], w=[(Ak, tt_)])
                    if tt_ < NB_DVE:
                        P.ts("dve", B_[:, tt_, :], self.iotab[:], iT[:, 1, t:t + 1], ALU.is_equal, r=[iTk, "iotab"], w=[(Bk, tt_)])
                    else:
                        a_t, atk = abt.next()
                        P.act(a_t[:], self.iotaf[:], AF.Abs, ["iotaf", nik], [atk], bias=ni2[:, t:t + 1], scale=1.0)
                        P.act(B_[:, tt_, :], a_t[:], AF.Relu, [atk], [(Bk, tt_)], bias=1.0, scale=-1.0)
                for t4 in range(16):
                    ps, pk = psA.next()
                    for tq in range(4):
                        tt_ = t4 * 4 + tq
                        P.mm(ps[:, tq * 128:(tq + 1) * 128], B_[:, tt_, :], A_[:, tt_, :], True, True,
                             [(Ak, tt_), (Bk, tt_)], [pk])
                    c0 = g64 * 64 + t4 * 4
                    dst = G_[:, :, c0:c0 + 4].rearrange("p c t -> p t c")
                    P.cp("act", dst, ps[:, :].rearrange("p (t c) -> p t c", c=128), [pk], [Gk])
            P.dma("sp", self.GTd[ti // 34][ti % 34], G_[:], [Gk], ())
        P.release(m)

    def phase_peer(self, l, want_ctx):
        P, I = self.P, self.I
        m = P.mark()
        NJ = 3
        TB = 128 * NJ
        self.load_gate(l, 2)
        h2 = Rot(P, "h2", 2, [128, 8, TB], BF16)
        uv = Rot(P, "uv", 8, [128, 2048], BF16)
        gt = Rot(P, "gt", 3, [128, NJ, 8, 128], BF16)
        ga = Rot(P, "ga", 3, [128, TB], BF16)
        wt = Rot(P, "wt", 3, [128, TB], BF16)
        psA = Rot(P, "psA", 2, [128, 512], F32, psum=True)
        psV = P.ps("psV", [128, 2 * NJ, 512], F32)
        for b in range(T // TB):
            t0 = b * TB
            h_t, hk = h2.next()
            P.dma("sp", h_t[:], self.h2T[:, t0:t0 + TB].rearrange("(kc p) t -> p kc t", p=128), (), [hk])
            def stage_a(c):
                nonlocal g_t8, g8k
                if c % 8 == 0:
                    g_t8, g8k = gt.next()
                    for j in range(NJ):
                        ti = NJ * b + j
                        P.dma("sp", g_t8[:, j, :, :], self.GTd[ti // 34][ti % 34, :, c:c + 8, :], (), [g8k])
                u_t, uk = uv.next()
                P.dma("sp", u_t[:], self.UV[c], [("UV", c)], [uk])
                ps, pk = psA.next()
                for kc in range(8):
                    P.mm(ps[:, 0:TB], u_t[:, kc * 128:(kc + 1) * 128], h_t[:, kc, :], kc == 0, kc == 7, [uk, hk], [pk])
                g_t, gk = ga.next()
                P.act(g_t[:], ps[:, 0:TB], AF.Gelu, [pk], [gk])
                w_t, wk = wt.next()
                P.tt("dve", w_t[:].rearrange("p (j t) -> p j t", j=NJ), g_t[:].rearrange("p (j t) -> p j t", j=NJ),
                     g_t8[:, :, c % 8, :], ALU.mult, [gk, g8k], [wk])
                return w_t, wk, u_t, uk

            def stage_b(c, w_t, wk, u_t, uk):
                for j in range(NJ):
                    for hf in range(2):
                        P.mm(psV[:, j * 2 + hf, :], w_t[:, j * 128:(j + 1) * 128], u_t[:, 1024 + hf * 512:1024 + (hf + 1) * 512],
                             c == 0, c == 127, [wk, uk], [("psV", j * 2 + hf)])

            g_t8 = g8k = None
            prev = stage_a(0)
            for c in range(128):
                nxt = stage_a(c + 1) if c + 1 < 128 else None
                stage_b(c, *prev)
                prev = nxt
            for j in range(NJ):
                ti = t0 // 128 + j
                s_ = 0 if ti >= 2 else 1
                xt, xk = self.xt.next()
                P.dma("sp", xt[:], self.xres[ti * 128:(ti + 1) * 128, :], (), [xk])
                tf, tk = self.tmpf.next()
                P.tt("dve", tf[:], psV[:, 2 * j:2 * j + 2, :].rearrange("p a n -> p (a n)"), self.modG[s_][:], ALU.mult,
                     [("psV", 2 * j), ("psV", 2 * j + 1), ("modG", s_)], [tk])
                P.tt("dve", tf[:], tf[:], xt[:], ALU.add, [tk, xk], [tk])
                P.dma("sp", self.xres[ti * 128:(ti + 1) * 128, :], tf[:], [tk], [("x", ti)])
        P.release(m)

    def phase_final(self):
        P, I = self.P, self.I
        m = P.mark()
        fg = P.sb("fg", [128, D], F32)
        P.dma("sp", fg[:], I["final_g"][0:1, :].to_broadcast([128, D]), w=["fg"])
        ot = Rot(P, "ot", 2, [128, D], F32)
        for ti in range(2, NT):
            xt, xk = self.xt.next()
            P.dma("sp", xt[:], self.xres[ti * 128:(ti + 1) * 128, :], (), [xk])
            r, rk = self.rstd(xt[:], D, xk)
            o, ok = ot.next()
            P.stt(o[:], xt[:], r, fg[:], ALU.mult, ALU.mult, [xk, rk, "fg"], [ok])
            P.dma("sp", self.out[(ti - 2) * 128:(ti - 1) * 128, :], o[:], [ok], ["out"])
        P.release(m)


def build(cfg):
    k = KE(cfg)
    P = k.P
    if cfg.get("scopes"):
        for nm in [n for n in dir(k) if n.startswith("phase_") and n != "phase_even"]:
            def wrap(f, nm=nm):
                def g(*a, **kw):
                    with P.nc.named_scope(nm + "_" + str(a[0] if a else "")):
                        return f(*a, **kw)
                return g
            setattr(k, nm, wrap(getattr(k, nm)))
    layers = cfg["layers"]
    stop_after = cfg.get("stop_after")
    k.phase_init()
    k.phase_ada()
    P.barrier()

    def dump(name, src):
        if name in k.dbg:
            P.barrier()
            n = src.shape[0]
            for r0 in range(0, n, 1024):
                r1 = min(n, r0 + 1024)
                P.dma("sp", k.dbg[name][r0:r1, :], src[r0:r1, :], (), ["dbgout"])
            P.barrier()

    done = False
    for l in layers:
        want_ctx = l < DEPTH - 1
        k.phase_peer_prep(l)
        if l % 2 == 0:
            k.phase_even(l, want_ctx)
            wout = k.I["even_w_out"][l // 2]
        else:
            k.phase_mla_proj(l)
            k.phase_mla_attn(l, want_ctx)
            wout = k.I["mla_w_out"][l // 2]
        dump("attn%d" % l, k.attn)
        if stop_after == ("attn", l):
            done = True; break
        k.phase_outproj(l, wout, want_ctx)
        dump("xm%d" % l, k.xres)
        if stop_after == ("outproj", l):
            done = True; break
        k.phase_peer_sel(l, True)
        k.phase_peer_g(l, True)
        k.phase_peer(l, True)
        dump("x%d" % l, k.xres)
    if not done:
        k.phase_final()
    P.barrier()
    return P.finish([]), k


def rope_tables():
    half = 32
    inv = 10000.0 ** (-np.arange(0, half, 2, dtype=np.float32) / half)
    t = np.arange(SEQ)
    row = (t // 64).astype(np.float32); col = (t % 64).astype(np.float32)
    ar = row[:, None] * inv[None, :]; ac = col[:, None] * inv[None, :]
    ang = np.concatenate([ar, ar, ac, ac], axis=-1).astype(np.float32)
    cosT = np.ones((64, T), np.float32); sinT = np.zeros((64, T), np.float32)
    cosT[:, CTX:] = np.cos(ang).T; sinT[:, CTX:] = np.sin(ang).T
    return cosT, sinT


def na_tables(rpb):
    q = np.arange(64); kc = np.arange(64)
    dc = np.clip(kc[:, None] - q[None, :], -15, 15) + 15
    g = rpb[:, :, :, dc]
    g = np.ascontiguousarray(np.transpose(g, (0, 3, 2, 1, 4))).astype(np.float32)
    cstart = np.clip(q - 8, 0, 48)
    ok = (kc[:, None] >= cstart[None, :]) & (kc[:, None] < cstart[None, :] + 16)
    mask = np.where(ok, 0.0, NEG).astype(np.float32)
    return g, mask


def host_inputs(inp, b):
    f = lambda a: np.ascontiguousarray(np.asarray(a, dtype=np.float32))
    cosT, sinT = rope_tables()
    rpbg, namask = na_tables(np.asarray(inp["na_rpb"], np.float32))
    c2 = np.stack([np.asarray(inp["c"][b]).reshape(8, 128).T, np.asarray(inp["c_ctx"]).reshape(8, 128).T], axis=-1)
    m = {
        "xin": f(np.concatenate([inp["ctx"][b], inp["x"][b]], axis=0)),
        "cond2": f(c2),
        "cosT": cosT, "sinT": sinT, "rpbg": rpbg, "namask": namask,
        "final_g": f(np.asarray(inp["final_g"]).reshape(1, D)),
        "conv_wT": f(np.transpose(np.asarray(inp["even_conv_w"]).reshape(2, 5, 12, 128), (0, 2, 3, 1))),
        "gdn_ab": f(np.stack([np.asarray(inp["gdn_a_log"]).reshape(2, 8), np.asarray(inp["gdn_dt_bias"]).reshape(2, 8)], axis=1)),
    }
    return m


_SHARED = None


def shared_inputs(inp):
    f = lambda a: np.ascontiguousarray(np.asarray(a, dtype=np.float32))
    sk = np.asarray(inp["peer_sub_keys"], np.float32).reshape(DEPTH, 16, 128, 128)
    m = {k: f(inp[k]) for k in ("ada_w", "ada_b", "norm1_g", "norm2_g", "mla_w_in", "mla_q_g", "mla_kv_g", "mla_w_uq",
                                "mla_w_ukv", "mla_w_out", "peer_w_q", "peer_v", "even_w_in", "even_w_out", "gdn_norm_g")}
    m["skT"] = f(np.transpose(sk, (0, 3, 1, 2)))
    m["peer_uT"] = f(np.transpose(np.asarray(inp["peer_u"], np.float32), (0, 2, 1)))
    return m


def kernel(**inputs):
    cfg = {"layers": [0, 1, 2, 3]}
    nc, k = build(cfg)
    sh = shared_inputs(inputs)
    in_maps = []
    for b in range(8):
        m = dict(sh); m.update(host_inputs(inputs, b))
        in_maps.append(m)
    res = run_bass_kernel_spmd(nc, in_maps, core_ids=list(range(8)))
    return np.stack([np.asarray(r["out"], np.float32) for r in res.results], axis=0)


class KE(KM):
    def phase_even_proj(self, l):
        P, I = self.P, self.I
        i = l // 2
        m = P.mark()
        if not hasattr(self, "qaT"):
            self.qaT = P.dram("qaT", [8, 64, T], BF16)
            self.kaT = P.dram("kaT", [8, 64, T], BF16)
            self.va = P.dram("va", [T, 8, 65], BF16)
            self.gpre = P.dram("gpre", [12, 128, T], F32)
            self.zd = P.dram("zd", [T, 512], F32)
            self.grd = P.dram("grd", [T, 16], F32)
        Win = P.sb("WinE", [128, 8, 3600], BF16)
        wv = I["even_w_in"][i].rearrange("(kc p) n -> p kc n", p=128)
        for kc in range(8):
            P.dma("pool", Win[:, kc, :], wv[:, kc, :], w=["Win"])
        self.load_AS(l, 1)
        pst = Rot(P, "pst", 2, [128, 8, 128], BF16, psum=True)
        psu = Rot(P, "psu", 4, [128, 512], F32, psum=True)
        hTr = Rot(P, "hT", 2, [128, 8, 512], BF16)
        stq = Rot(P, "stq", 2, [64, 8, 512], BF16)
        stk = Rot(P, "stk", 2, [64, 8, 512], BF16)
        stg = Rot(P, "stg", 3, [128, 512], F32)
        vsb = Rot(P, "vsbE", 2, [128, 8, 65], BF16)
        for t_ in vsb.t:
            P.memset("pool", t_[:, :, 64:65], 1.0, w=[(vsb.name, vsb.t.index(t_))])
        zsb = Rot(P, "zsbE", 2, [128, 512], F32)
        gsb = Rot(P, "gsbE", 2, [128, 16], F32)
        for (t0, n) in blocks(True):
            s = 0 if t0 >= CTX else 1
            nt = n // 128
            hT, hk = hTr.next()
            for j in range(nt):
                ti = t0 // 128 + j
                xt, xk = self.xt.next()
                P.dma("sp", xt[:], self.xres[ti * 128:(ti + 1) * 128, :], (), [xk])
                self.norm_mod_T(xt[:], xk, s, hT[:, :, j * 128:(j + 1) * 128], hk, pst)
            sq, sqk = stq.next(); sk_, skk = stk.next()
            for h in range(8):
                for (dst, dk, c0) in ((sq, sqk, 0), (sk_, skk, 512)):
                    ps, pk = psu.next()
                    for kc in range(8):
                        P.mm(ps[0:64, 0:n], Win[:, kc, c0 + h * 64:c0 + (h + 1) * 64], hT[:, kc, 0:n], kc == 0, kc == 7, ["Win", hk], [pk])
                    P.cp("act", dst[:, h, 0:n], ps[0:64, 0:n], [pk], [dk])
            P.dma("sp", self.qaT[:, :, t0:t0 + n].rearrange("h p t -> p h t"), sq[:, :, 0:n], [sqk], ())
            P.dma("sp", self.kaT[:, :, t0:t0 + n].rearrange("h p t -> p h t"), sk_[:, :, 0:n], [skk], ())
            for c in range(12):
                ps, pk = psu.next()
                for kc in range(8):
                    P.mm(ps[:, 0:n], Win[:, kc, 1536 + c * 128:1536 + (c + 1) * 128], hT[:, kc, 0:n], kc == 0, kc == 7, ["Win", hk], [pk])
                sg, sgk = stg.next()
                P.cp("act" if c % 2 else "dve", sg[:, 0:n], ps[:, 0:n], [pk], [sgk])
                P.dma("sp", self.gpre[c, :, t0:t0 + n], sg[:, 0:n], [sgk], ())
            for j in range(nt):
                ti = t0 // 128 + j
                tsl = slice(j * 128, (j + 1) * 128)
                ps, pk = psu.next()
                for kc in range(8):
                    P.mm(ps[:, :], hT[:, kc, tsl], Win[:, kc, 1024:1536], kc == 0, kc == 7, ["Win", hk], [pk])
                v_, vk = vsb.next()
                P.cp("act", v_[:, :, 0:64], ps[:, :].rearrange("p (h d) -> p h d", d=64), [pk], [vk])
                P.dma("sp", self.va[ti * 128:(ti + 1) * 128, :, :], v_[:], [vk], ())
                ps, pk = psu.next()
                for kc in range(8):
                    P.mm(ps[:, :], hT[:, kc, tsl], Win[:, kc, 3072:3584], kc == 0, kc == 7, ["Win", hk], [pk])
                z_, zk = zsb.next()
                P.cp("dve", z_[:], ps[:, :], [pk], [zk])
                P.dma("sp", self.zd[ti * 128:(ti + 1) * 128, :], z_[:], [zk], ())
                ps, pk = psu.next()
                for kc in range(8):
                    P.mm(ps[:, 0:16], hT[:, kc, tsl], Win[:, kc, 3584:3600], kc == 0, kc == 7, ["Win", hk], [pk])
                g_, gk = gsb.next()
                P.cp("act", g_[:], ps[:, 0:16], [pk], [gk])
                P.dma("sp", self.grd[ti * 128:(ti + 1) * 128, :], g_[:], [gk], ())
        P.release(m)

    def phase_na(self, l, want_ctx):
        P, I = self.P, self.I
        i = l // 2
        m = P.mark()
        scale = 64.0 ** -0.5
        BB = P.sb("BB", [64, 15, 8, 64], F32)
        msk = P.sb("msk", [64, 64], F32)
        P.dma("sp", BB[:], I["rpbg"][i], w=["BB"])
        P.dma("sp", msk[:], I["namask"][:, :], w=["msk"])
        for dr in range(15):
            P.tt("pool", BB[:, dr, :, :], BB[:, dr, :, :], msk[:].unsqueeze(1).to_broadcast([64, 8, 64]), ALU.add, ["BB", "msk"], ["BB"])
            P.ts("pool", BB[:, dr, :, :], BB[:, dr, :, :], 1.0 / scale, ALU.mult, r=["BB"], w=["BB"])
        kcT = P.sb("kcT", [64, 8, 256], BF16)
        vcx = P.sb("vcx", [128, 2, 8, 65], BF16)
        P.dma("sp", kcT[:], self.kaT[:, :, 0:256].rearrange("h p t -> p h t"), w=["kcT"])
        P.dma("sp", vcx[:], self.va[0:256, :, :].rearrange("(kt p) h d -> p kt h d", p=128), (), ["vcx"])
        m2 = P.mark()
        kw = Rot(P, "kw", 2, [64, 8, 1024], BF16)
        vw = Rot(P, "vw", 2, [64, 16, 8, 65], BF16)
        qb = Rot(P, "qb", 2, [64, 8, 512], BF16)
        psS = Rot(P, "psS", 2, [64, 2, 8, 64], F32, psum=True)
        psC = Rot(P, "psC", 2, [128, 2, 2, 64], F32, psum=True)
        psO = Rot(P, "psO", 1, [128, 2, 65], F32, psum=True)
        stmp = Rot(P, "stmp", 2, [64, 2, 8, 64], F32)
        pb = Rot(P, "pb", 2, [64, 2, 8, 64], BF16)
        pcb = Rot(P, "pcb", 2, [128, 2, 2, 64], BF16)
        yo = Rot(P, "yo", 1, [64, 8, 8, 64], F32)
        for b8 in range(16):
            w0 = min(max(8 * b8 - 4, 0), 112)
            k_w, kk = kw.next(); v_w, vk = vw.next(); q_b, qk = qb.next()
            tk0 = CTX + w0 * 64
            P.dma("sp", k_w[:], self.kaT[:, :, tk0:tk0 + 1024].rearrange("h p t -> p h t"), (), [kk])
            P.dma("sp", v_w[:], self.va[tk0:tk0 + 1024, :, :].rearrange("(r p) h d -> p r h d", p=64), (), [vk])
            tq0 = CTX + b8 * 512
            P.dma("sp", q_b[:], self.qaT[:, :, tq0:tq0 + 512].rearrange("h p t -> p h t"), (), [qk])
            y_o, yk = yo.next()

            def na_a(rr, hp2):
                r = 8 * b8 + rr
                rs = min(max(r - 4, 0), 120)
                dr0 = rs - r + 7
                ps, pk = psS.next()
                pc, pck = psC.next()
                for hh in range(2):
                    h = 2 * hp2 + hh
                    for i8 in range(8):
                        kr = rs + i8 - w0
                        P.mm(ps[:, hh, i8, :], k_w[:, h, kr * 64:(kr + 1) * 64], q_b[:, h, rr * 64:(rr + 1) * 64], True, True,
                             [kk, qk], [pk])
                    for ct in range(2):
                        P.mm(pc[:, ct, hh, :], kcT[:, h, ct * 128:(ct + 1) * 128], q_b[:, h, rr * 64:(rr + 1) * 64], True, True,
                             ["kcT", qk], [pck])
                st, stk_ = stmp.next()
                bias = BB[:, dr0:dr0 + 8, 2 * hp2:2 * hp2 + 2, :].rearrange("p r h q -> p h r q")
                P.tt("dve", st[:], ps[:], bias, ALU.add, [pk, "BB"], [stk_])
                p_b, pbk = pb.next()
                P.act(p_b[:], st[:], AF.Exp, [stk_], [pbk], scale=scale)
                p_c, pcbk = pcb.next()
                P.act(p_c[:], pc[:], AF.Exp, [pck], [pcbk], scale=scale)
                return rr, hp2, rs, p_b, pbk, p_c, pcbk

            def na_b(rr, hp2, rs, p_b, pbk, p_c, pcbk):
                po, pok = psO.next()
                for hh in range(2):
                    h = 2 * hp2 + hh
                    for i8 in range(8):
                        kr = rs + i8 - w0
                        P.mm(po[0:64, hh, :], p_b[:, hh, i8, :], v_w[:, kr, h, :], i8 == 0, False, [pbk, vk], [pok])
                    for ct in range(2):
                        P.mm(po[0:64, hh, :], p_c[:, ct, hh, :], vcx[:, ct, h, :], False, ct == 1, [pcbk, "vcx"], [pok])
                sm, smk = self.small.next()
                P.op("dve", lambda e, sm=sm, po=po: e.reciprocal(out=sm[0:64, 0:2], in_=po[0:64, :, 64]), [pok], [smk])
                P.tt("dve", y_o[:, rr, 2 * hp2:2 * hp2 + 2, :], po[0:64, :, 0:64], sm[0:64, 0:2].unsqueeze(2).to_broadcast([64, 2, 64]),
                     ALU.mult, [pok, smk], [yk])

            items = [(rr, hp2) for rr in range(8) for hp2 in range(4)]
            prev = na_a(*items[0])
            for ii in range(len(items)):
                nxt = na_a(*items[ii + 1]) if ii + 1 < len(items) else None
                na_b(*prev)
                prev = nxt
            P.dma("sp", self.attn[tq0:tq0 + 512, 0:512].rearrange("(r p) (h d) -> p r h d", p=64, d=64), y_o[:], [yk], ())
        P.release(m2)
        if want_ctx:
            psO = Rot(P, "psO", 1, [128, 2, 65], F32, psum=True)
            qc = P.sb("qc", [64, 8, 256], BF16)
            P.dma("sp", qc[:], self.qaT[:, :, 0:256].rearrange("h p t -> p h t"), w=["qc"])
            psX = Rot(P, "psX", 1, [128, 2, 256], F32, psum=True)
            pxb = Rot(P, "pxb", 2, [128, 2, 256], BF16)
            yc = P.sb("yc", [128, 2, 8, 64], F32)
            for h in range(8):
                ps, pk = psX.next()
                for kt in range(2):
                    P.mm(ps[:, kt, :], kcT[:, h, kt * 128:(kt + 1) * 128], qc[:, h, :], True, True, ["kcT", "qc"], [pk])
                px, pxk = pxb.next()
                P.act(px[:], ps[:], AF.Exp, [pk], [pxk], scale=scale)
                po, pok = psO.next()
                for qt in range(2):
                    for kt in range(2):
                        P.mm(po[:, qt, :], px[:, kt, qt * 128:(qt + 1) * 128], vcx[:, kt, h, :], kt == 0, kt == 1, [pxk, "vcx"], [pok])
                sm, smk = self.small.next()
                P.op("dve", lambda e, sm=sm, po=po: e.reciprocal(out=sm[:, 0:2], in_=po[:, :, 64]), [pok], [smk])
                P.tt("dve", yc[:, :, h, :], po[:, :, 0:64], sm[:, 0:2].unsqueeze(2).to_broadcast([128, 2, 64]), ALU.mult, [pok, smk], ["yc"])
            P.dma("sp", self.attn[0:256, 0:512].rearrange("(qt p) (h d) -> p qt h d", p=128, d=64), yc[:], ["yc"], ())
        P.release(m)

    def phase_gdn_pre(self, l):
        P, I = self.P, self.I
        i = l // 2
        m = P.mark()
        if not hasattr(self, "gT"):
            self.gT = P.dram("gT", [12, 128, T], F32)
            self.od = P.dram("od", [2, T, 512], F32)
        cw = P.sb("cw", [128, 12, 5], F32)
        P.dma("sp", cw[:], I["conv_wT"][i].rearrange("c p k -> p c k"), w=["cw"])
        onesf = P.sb("onesf", [128, 128], F32)
        P.memset("pool", onesf[:], 1.0, w=["onesf"])
        NP = 2048
        xp = Rot(P, "xp", 2, [128, NP + 4], F32)
        acc = Rot(P, "acc", 2, [128, NP], F32)
        yv = Rot(P, "yv", 2, [128, NP], F32)
        sq = Rot(P, "sq", 2, [128, NP], F32)
        rn = Rot(P, "rn", 2, [128, 512], F32)
        psn = Rot(P, "psn", 2, [128, 512], F32, psum=True)
        pieces = [(0, 256, 0, 256)] + [(256 + NP * k, 256 + NP * (k + 1), 256, T) for k in range(SEQ // NP)]
        for c in range(12):
            for (a, b, lo_, hi_) in pieces:
                n = b - a
                x_, xk = xp.next()
                lo = max(a - 2, lo_); hi = min(b + 2, hi_)
                if lo > a - 2:
                    P.memset("pool", x_[:, 0:2], 0.0, w=[xk])
                if hi < b + 2:
                    P.memset("pool", x_[:, n + 2:n + 4], 0.0, w=[xk])
                P.dma("sp", x_[:, lo - (a - 2):hi - (a - 2)], self.gpre[c, :, lo:hi], (), [xk])
                a_, ak = acc.next()
                P.ts("dve", a_[:, 0:n], x_[:, 0:n], cw[:, c, 0:1], ALU.mult, r=[xk, "cw"], w=[ak])
                for k in range(1, 5):
                    P.stt(a_[:, 0:n], x_[:, k:k + n], cw[:, c, k:k + 1], a_[:, 0:n], ALU.mult, ALU.add, [xk, "cw", ak], [ak])
                y_, yk = yv.next()
                P.act(y_[:, 0:n], a_[:, 0:n], AF.Silu, [ak], [yk])
                if c < 8:
                    s_, sk = sq.next()
                    P.act(s_[:, 0:n], y_[:, 0:n], AF.Square, [yk], [sk])
                    for q0 in range(0, n, 512):
                        w_ = min(512, n - q0)
                        ps, pk = psn.next()
                        P.mm(ps[:, 0:w_], onesf[:], s_[:, q0:q0 + w_], True, True, ["onesf", sk], [pk])
                        r_, rk = rn.next()
                        mul = 128.0 if c < 4 else 1.0
                        P.act(r_[:, 0:w_], ps[:, 0:w_], AF.Sqrt, [pk], [rk], scale=mul, bias=mul * EPS)
                        P.op("dve", lambda e, r_=r_, w_=w_: e.reciprocal(out=r_[:, 0:w_], in_=r_[:, 0:w_]), [rk], [rk])
                        P.tt("dve", y_[:, q0:q0 + w_], y_[:, q0:q0 + w_], r_[:, 0:w_], ALU.mult, [yk, rk], [yk])
                P.dma("sp", self.gT[c, :, a:b], y_[:, 0:n], [yk], ())
        P.release(m)

    def phase_gdn(self, l):
        P, I = self.P, self.I
        i = l // 2
        m = P.mark()
        sbc = lambda n: P.sb(n, [128, 128], F32)
        Ball = sbc("Ball"); P.memset("pool", Ball[:], 0.0, w=["Ball"])
        P.memset("pool", Ball[0:64, 0:64], 1.0, w=["Ball"]); P.memset("pool", Ball[64:128, 64:128], 1.0, w=["Ball"])
        INf = sbc("INf"); INb = sbc("INb"); STf = sbc("STf"); STb = sbc("STb")
        for (dst, coef, cm, base) in ((INf, -1, 1, 0), (INb, 1, -1, 0), (STf, -1, 1, -1), (STb, 1, -1, -1)):
            P.op("pool", lambda e, dst=dst, coef=coef, cm=cm, base=base: e.affine_select(
                out=dst[:], in_=Ball[:], pattern=[[coef, 128]], compare_op=ALU.is_ge, fill=0.0, base=base, channel_multiplier=cm),
                ["Ball"], [dst.name])
        MNf = sbc("MNf"); MNb = sbc("MNb")
        P.ts("dve", MNf[:], INf[:], -1.0, ALU.add, -NEG, ALU.mult, r=[INf.name], w=["MNf"])
        P.ts("dve", MNb[:], INb[:], -1.0, ALU.add, -NEG, ALU.mult, r=[INb.name], w=["MNb"])
        CM = [sbc("CM0"), sbc("CM1")]
        BM = [sbc("BM0"), sbc("BM1")]
        SEL = [sbc("SEL0"), sbc("SEL1")]
        for c in range(2):
            for t_ in (CM[c], BM[c], SEL[c]):
                P.memset("pool", t_[:], 0.0, w=[t_.name])
            P.memset("pool", CM[c][:, c * 64:(c + 1) * 64], 1.0, w=[CM[c].name])
            P.memset("pool", BM[c][c * 64:(c + 1) * 64, c * 64:(c + 1) * 64], 1.0, w=[BM[c].name])
            P.memset("pool", SEL[c][c * 64:(c + 1) * 64, :], 1.0 / 64, w=[SEL[c].name])
        cnames = [Ball.name, INf.name, INb.name, STf.name, STb.name, "MNf", "MNb"] + [t_.name for t_ in CM + BM + SEL]
        MN = [MNf, MNb]; ST = [STf, STb]
        Lc = [INb, INf]
        ab = P.sb("ab", [128, 2, 8], F32)
        P.dma("sp", ab[:], I["gdn_ab"][i:i + 1, :, :].to_broadcast([128, 2, 8]), w=["ab"])
        negA = P.sb("negA", [128, 8], F32)
        P.act(negA[:], ab[:, 0, :], AF.Exp, ["ab"], ["negA"])
        P.ts("dve", negA[:], negA[:], -1.0, ALU.mult, r=["negA"], w=["negA"])

        psg = Rot(P, "psg", 8, [128, 128], F32, psum=True)
        grt = Rot(P, "grt", 2, [128, 16], F32)
        gtmp = Rot(P, "gtmp", 3, [128, 16], F32)
        gsc = Rot(P, "gsc", 6, [128, 6, 8], F32)
        bnd = Rot(P, "bnd", 6, [128, 2, 8], F32)

        def gates(ti):
            g_, gk = grt.next()
            P.dma("sp", g_[:], self.grd[ti * 128:(ti + 1) * 128, :], (), [gk])
            g4 = g_[:].rearrange("p (d k h) -> p d k h", d=2, k=2)
            t_, tk = gtmp.next()
            t4 = t_[:].rearrange("p (d k h) -> p d k h", d=2, k=2)
            sc, sk = gsc.next()
            P.tt("dve", t4[:, :, 0, :], g4[:, :, 0, :], ab[:, 1, :].rearrange("p (d h) -> p d h", d=2), ALU.add, [gk, "ab"], [tk])
            P.act(t4[:, :, 0, :], t4[:, :, 0, :], AF.Exp, [tk], [tk])
            P.act(t4[:, :, 0, :], t4[:, :, 0, :], AF.Ln, [tk], [tk], bias=1.0)
            P.tt("dve", t4[:, :, 0, :], t4[:, :, 0, :], negA[:].rearrange("p (d h) -> p d h", d=2), ALU.mult, [tk, "negA"], [tk])
            P.act(sc[:, 4, :].rearrange("p (d h) -> p d h", d=2), g4[:, :, 1, :], AF.Sigmoid, [gk], [sk])
            gg = t_[:].rearrange("p (d k h) -> p d k h", d=2, k=2)
            gcomp, gck = gtmp.next()
            P.cp("dve", gcomp[:, 0:8].rearrange("p (d h) -> p d h", d=2), gg[:, :, 0, :], [tk], [gck])
            ps, pk = psg.t[0], (psg.name, 0)
            P.mm(ps[:, 0:4], Lc[0][:], gcomp[:, 0:4], True, True, [Lc[0].name, gck], [pk])
            P.mm(ps[:, 4:8], Lc[1][:], gcomp[:, 4:8], True, True, [Lc[1].name, gck], [pk])
            P.mm(ps[:, 8:16], Ball[:], gcomp[:, 0:8], True, True, [Ball.name, gck], [pk])
            gps_sb, gpk = gtmp.next()
            P.cp("act", gps_sb[:], ps[:, 0:16], [pk], [gpk])
            P.cp("act", sc[:, 0, :], gps_sb[:, 0:8], [gpk], [sk])
            P.ts("dve", sc[:, 1, :], gps_sb[:, 0:8], -1.0, ALU.mult, r=[gpk], w=[sk])
            P.act(sc[:, 2, :], gps_sb[:, 0:8], AF.Exp, [gpk], [sk])
            P.tt("dve", sc[:, 5, :], gps_sb[:, 8:16], sc[:, 0, :], ALU.subtract, [gpk, sk], [sk])
            P.act(sc[:, 5, :], sc[:, 5, :], AF.Exp, [sk], [sk])
            P.stt(sc[:, 3, :], sc[:, 2, :], -1.0, sc[:, 4, :], ALU.mult, ALU.mult, [sk], [sk])
            P.cp("act", gcomp[:, 8:16], gps_sb[:, 8:16], [gpk], [gck])
            ps2, pk2 = psg.t[1], (psg.name, 1)
            for c in range(2):
                P.mm(ps2[:, c * 8:(c + 1) * 8], SEL[c][:], gcomp[:, 8:16], True, True, [SEL[c].name, gck], [pk2])
            b_, bk = bnd.next()
            P.act(b_[:].rearrange("p c e -> p (c e)"), ps2[:, 0:16], AF.Exp, [pk2], [bk])
            return sc, sk, b_, bk

        NCH = 8
        wk4 = [[P.sb("gwk%d_%d" % (ch, j), [128, 128], F32) for j in range(4)] for ch in range(NCH)]
        PAs = [[P.sb("PA%d_%d" % (ch, j), [128, 128], F32) for j in range(2)] for ch in range(NCH)]
        PTs = [[P.sb("PT%d_%d" % (ch, j), [128, 128], F32) for j in range(2)] for ch in range(NCH)]
        TTs = [P.sb("TT%d" % ch, [128, 128], F32) for ch in range(NCH)]
        lds = [P.sb("ld%d" % ch, [128, 3, 128], F32) for ch in range(NCH)]
        prep = [[P.sb("prep%d_%d" % (ch, j), [128, 9, 128], F32) for j in range(2)] for ch in range(NCH)]
        prep_i = [0] * NCH
        Sst = [[P.sb("S%d_%d" % (ch, j), [128, 128], F32) for j in range(2)] for ch in range(NCH)]
        S_i = [0] * NCH
        for ch in range(NCH):
            P.memset("pool", Sst[ch][0][:], 0.0, w=[("S", ch, 0)])
        rr_ = [P.sb("rr%d" % ch, [128, 128], F32) for ch in range(NCH)]
        uu = [P.sb("uu%d" % ch, [128, 128], F32) for ch in range(NCH)]
        osb = [P.sb("gosb%d" % ch, [128, 128], F32) for ch in range(NCH)]

        def prep_tile(ti, h, d, sc, sk):
            ch = d * 4 + h; c8 = ch
            j = prep_i[ch]; prep_i[ch] ^= 1
            pr = prep[ch][j]; prk = ("prep", ch, j)
            return pr, prk, prep_gen(ti, h, d, sc, sk, ch, pr, prk)

        free_ps = list(zip(psg.t, [(psg.name, j) for j in range(psg.n)]))

        def grab(n):
            while len(free_ps) < n:
                yield
            return [free_ps.pop(0) for _ in range(n)]

        def give(*tiles):
            free_ps.extend(tiles)

        def prep_gen(ti, h, d, sc, sk, ch, pr, prk):
            c8 = ch
            ld, ldk = lds[ch], ("ld", ch)
            for w_, cc in enumerate((h, 4 + h, 8 + h)):
                P.dma("sp", ld[:, w_, :], self.gT[cc, :, ti * 128:(ti + 1) * 128], (), [ldk])
            qT, kT, vT = ld[:, 0, :], ld[:, 1, :], ld[:, 2, :]
            gcol = lambda w_: sc[:, w_, c8:c8 + 1]
            (gcB, Dm, dT, EG) = wk4[ch]
            gcBk, Dmk, dTk, EGk = [("wk", ch, q) for q in range(4)]
            P.cp("pool", gcB[:], gcol(0).to_broadcast([128, 128]), [sk], [gcBk])
            yield
            (g_,) = yield from grab(1)
            Gps, Gk = g_
            P.mm(Gps[:], gcB[:], self.identf[:], True, True, [gcBk, "identf"], [Gk])
            yield
            P.stt(Dm[:], Gps[:], -1.0, MN[d][:], ALU.mult, ALU.add, [Gk, "MNf" if d == 0 else "MNb"], [Dmk])
            P.tt("dve", dT[:], Gps[:], MN[1 - d][:], ALU.add, [Gk, "MNf" if d == 1 else "MNb"], [dTk])
            P.act(EG[:], Gps[:], AF.Exp, [Gk, Dmk, dTk], [EGk])
            give(g_)
            yield
            P.act(Dm[:], Dm[:], AF.Exp, [Dmk, sk], [Dmk], bias=gcol(0))
            P.act(dT[:], dT[:], AF.Exp, [dTk, sk], [dTk], bias=gcol(1))
            yield
            P.tt("pool", Dm[:], Dm[:], ST[d][:], ALU.mult, [Dmk, ST[d].name], [Dmk])
            (g_,) = yield from grab(1)
            KKp, KKk = g_
            P.mm(KKp[:], kT, kT, True, True, [ldk], [KKk])
            yield
            pa = PAs[ch]; pt = PTs[ch]
            A_, Ak = pa[0], ("PA", ch, 0)
            P.stt(A_[:], KKp[:], gcol(4), Dm[:], ALU.mult, ALU.mult, [KKk, sk, Dmk], [Ak])
            give(g_)
            yield
            (g_,) = yield from grab(1)
            ATp, ATpk = g_
            P.tr(ATp[:], A_[:], self.identf[:], [Ak, "identf"], [ATpk])
            yield
            AT, ATk = pt[0], ("PT", ch, 0)
            P.cp("act", AT[:], ATp[:], [ATpk], [ATk])
            give(g_)
            yield
            TT, TTk = TTs[ch], ("TT", ch)
            P.tt("dve", TT[:], self.identf[:], AT[:], ALU.subtract, ["identf", ATk], [TTk])
            Pn, Pnk, PTn, PTnk = A_, Ak, AT, ATk
            for lev in range(5):
                gs = yield from grab(2 if lev < 4 else 1)
                p2, p2k = gs[0]
                P.mm(p2[:], PTn[:], Pn[:], True, True, [PTnk, Pnk], [p2k])
                if lev < 4:
                    p2t, p2tk = gs[1]
                    P.mm(p2t[:], Pn[:], PTn[:], True, True, [PTnk, Pnk], [p2tk])
                yield
                q_ = (lev + 1) % 2
                Pn2, Pn2k = pa[q_], ("PA", ch, q_)
                P.cp("act", Pn2[:], p2[:], [p2k], [Pn2k])
                if lev < 4:
                    PTn2, PTn2k = pt[q_], ("PT", ch, q_)
                    P.cp("dve", PTn2[:], p2t[:], [p2tk], [PTn2k])
                give(*gs)
                yield
                (g_,) = yield from grab(1)
                up, upk = g_
                P.mm(up[:], Pn2[:], TT[:], True, True, [Pn2k, TTk], [upk])
                yield
                P.tt("dve", TT[:], TT[:], up[:], ALU.add, [TTk, upk], [TTk])
                give(g_)
                Pn, Pnk = Pn2, Pn2k
                if lev < 4:
                    PTn, PTnk = PTn2, PTn2k
            yield
            for c in range(2):
                P.tt("pool", pr[:, c, :], TT[:], BM[c][:], ALU.mult, [TTk, BM[c].name], [prk])
            P.tt("pool", EG[:], EG[:], qT, ALU.mult, [EGk, ldk], [EGk])
            gs = yield from grab(3)
            (Pp, Ppk), (kp, kpk), (vp, vpk) = gs
            P.mm(Pp[:], kT, qT, True, True, [ldk], [Ppk])
            P.tr(kp[:], kT, self.identf[:], [ldk, "identf"], [kpk])
            P.tr(vp[:], vT, self.identf[:], [ldk, "identf"], [vpk])
            yield
            P.tt("dve", pr[:, 2, :], Pp[:], dT[:], ALU.mult, [Ppk, dTk], [prk])
            P.act(pr[:, 7, :], kp[:], AF.Copy, [kpk, sk], [prk], scale=gcol(5))
            P.act(pr[:, 8, :], vp[:], AF.Copy, [vpk, sk], [prk], scale=gcol(4))
            give(*gs)
            for c in range(2):
                P.tt("pool", pr[:, 3 + c, :], EG[:], CM[c][:], ALU.mult, [EGk, CM[c].name], [prk])
                P.tt("pool", pr[:, 5 + c, :], kT, CM[c][:], ALU.mult, [ldk, CM[c].name], [prk])
            yield

        def chain_gen(ti, h, d, pr, prk, sc, sk, b_, bk):
            ch = d * 4 + h; c8 = ch
            o_, ok = osb[ch], ("osb", ch)
            r_, rk = rr_[ch], ("rr", ch)
            u_, uk = uu[ch], ("uu", ch)
            order = (0, 1) if d == 0 else (1, 0)
            for n_, c in enumerate(order):
                si = S_i[ch]; S_i[ch] ^= 1
                S = Sst[ch][si]; Sk = ("S", ch, si)
                S2 = Sst[ch][si ^ 1]; S2k = ("S", ch, si ^ 1)
                (g_,) = yield from grab(1)
                ks, ksk = g_
                P.mm(ks[:], pr[:, 5 + c, :], S[:], True, True, [prk, Sk], [ksk])
                yield
                P.stt(r_[:], ks[:], sc[:, 3, c8:c8 + 1], pr[:, 8, :], ALU.mult, ALU.add, [ksk, sk, prk], [rk])
                give(g_)
                yield
                (g_,) = yield from grab(1)
                up, upk = g_
                P.mm(up[:], pr[:, c, :], r_[:], True, True, [prk, rk], [upk])
                yield
                P.cp("act", u_[:], up[:], [upk], [uk])
                give(g_)
                yield
                gs = yield from grab(2)
                (op_, opk), (ds, dsk) = gs
                P.mm(op_[:], pr[:, 3 + c, :], S[:], True, False, [prk, Sk], [opk])
                P.mm(op_[:], pr[:, 2, :], u_[:], False, True, [prk, uk], [opk])
                P.mm(ds[:], pr[:, 7, :], u_[:], True, True, [prk, uk], [dsk])
                yield
                P.stt(S2[:], S[:], b_[:, c, c8:c8 + 1], ds[:], ALU.mult, ALU.add, [Sk, bk, dsk], [S2k])
                if n_ == 0:
                    P.cp("act", o_[:], op_[:], [opk], [ok])
                else:
                    P.tt("dve", o_[:], o_[:], op_[:], ALU.add, [ok, opk], [ok])
                give(*gs)
                yield
            P.dma("sp", self.od[d, ti * 128:(ti + 1) * 128, h * 128:(h + 1) * 128], o_[:], [ok], ())

        def run_rr(gens):
            gens = list(gens)
            while gens:
                alive = []
                for g in gens:
                    try:
                        next(g); alive.append(g)
                    except StopIteration:
                        pass
                gens = alive

        fwd_order = list(range(NT))
        bwd_order = [1, 0] + list(range(NT - 1, 1, -1))

        def make_preps(s_):
            tf_, tb_ = fwd_order[s_], bwd_order[s_]
            gf = gates(tf_)
            gb = gates(tb_)
            out = []
            for h in range(4):
                out.append((tf_, h, 0, prep_tile(tf_, h, 0, gf[0], gf[1]), gf))
                out.append((tb_, h, 1, prep_tile(tb_, h, 1, gb[0], gb[1]), gb))
            return out

        cur = make_preps(0)
        run_rr([p[3][2] for p in cur])
        for s_ in range(NT):
            nxt = make_preps(s_ + 1) if s_ + 1 < NT else []
            chains = [chain_gen(ti, h, d, pr[0], pr[1], g[0], g[1], g[2], g[3]) for (ti, h, d, pr, g) in cur]
            run_rr([p[3][2] for p in nxt] + chains)
            cur = nxt
        P.release(m)

    def phase_gdn_out(self, l):
        P, I = self.P, self.I
        i = l // 2
        m = P.mark()
        gng = P.sb("gng", [128, 128], F32)
        P.dma("sp", gng[:], I["gdn_norm_g"][i:i + 1, :].to_broadcast([128, 128]), w=["gng"])
        of = Rot(P, "of", 2, [128, 512], F32)
        ob = Rot(P, "ob", 2, [128, 512], F32)
        zt = Rot(P, "zt", 2, [128, 512], F32)
        yt = Rot(P, "yt", 2, [128, 512], F32)
        for ti in range(NT):
            o1, k1 = of.next(); o2, k2 = ob.next(); z_, zk = zt.next()
            P.dma("sp", o1[:], self.od[0, ti * 128:(ti + 1) * 128, :], (), [k1])
            P.dma("sp", o2[:], self.od[1, ti * 128:(ti + 1) * 128, :], (), [k2])
            P.dma("sp", z_[:], self.zd[ti * 128:(ti + 1) * 128, :], (), [zk])
            P.tt("dve", o1[:], o1[:], o2[:], ALU.add, [k1, k2], [k1])
            P.act(z_[:], z_[:], AF.Silu, [zk], [zk])
            y_, yk = yt.next()
            for h in range(4):
                hs = slice(h * 128, (h + 1) * 128)
                r, rk = self.rstd(o1[:, hs], 128, k1)
                P.stt(y_[:, hs], o1[:, hs], r, gng[:], ALU.mult, ALU.mult, [k1, rk, "gng"], [yk])
            P.tt("dve", y_[:], y_[:], z_[:], ALU.mult, [yk, zk], [yk])
            P.dma("sp", self.attn[ti * 128:(ti + 1) * 128, 512:1024], y_[:], [yk], ())
        P.release(m)

    def phase_even(self, l, want_ctx):
        self.phase_even_proj(l)
        self.phase_na(l, want_ctx)
        self.phase_gdn_pre(l)
        self.phase_gdn(l)
        self.phase_gdn_out(l)
```

```python
import numpy as np
import concourse.bass as bass
import concourse.mybir as mybir
from concourse.bass_utils import run_bass_kernel_spmd

F32 = mybir.dt.float32
BF16 = mybir.dt.bfloat16
U32 = mybir.dt.uint32
AF = mybir.ActivationFunctionType
ALU = mybir.AluOpType
AX = mybir.AxisListType

D = 1024
SEQ = 8192
CTX = 256
T = SEQ + CTX
NT = T // 128
DEPTH = 4
EPS = 1e-6
ENGS = ("pe", "act", "dve", "pool", "sp")
DMA_SEMS_PER_Q = 24
NEG = -30000.0


class Prog:
    def __init__(self):
        self.nc = bass.Bass("TRN2", target_bir_lowering=False)
        nc = self.nc
        self.E = {"pe": nc.tensor, "act": nc.scalar, "dve": nc.vector, "pool": nc.gpsimd, "sp": nc.sync}
        self.cnt = {e: 0 for e in ENGS}
        self.waited = {e: {} for e in ENGS}
        self.last_w = {}
        self.readers = {}
        self.dma_rr = {e: 0 for e in ENGS}
        self.dma_tot = {}
        self.sems = {}
        self.ctxs = []
        self.n_inst = 0
        self._semctx = set()
        self.uid = 0

    def sb(self, name, shape, dt=F32):
        self.uid += 1; name = "%s_%d" % (name, self.uid)
        c = self.nc.sbuf_tensor(name, list(shape), dt)
        t = c.__enter__(); self.ctxs.append(c)
        return t

    def ps(self, name, shape, dt=F32):
        self.uid += 1; name = "%s_%d" % (name, self.uid)
        c = self.nc.psum_tensor(name, list(shape), dt)
        t = c.__enter__(); self.ctxs.append(c)
        return t

    def psv(self, name, shape, dt=F32):
        esz = 4 if dt == F32 else 2
        per = 1
        for d in shape[1:]:
            per *= d
        nb = (per * esz + 2047) // 2048
        t = self.ps(name, [128, nb * (2048 // esz)], dt)
        v = t[0:shape[0], 0:per]
        if len(shape) > 2:
            names = " ".join("d%d" % i for i in range(1, len(shape)))
            v = v.rearrange("p (%s) -> p %s" % (names, names), **{"d%d" % i: shape[i] for i in range(2, len(shape))})
        return v

    def dram(self, name, shape, dt=F32, kind="Internal"):
        return self.nc.dram_tensor(name, list(shape), dt, kind=kind).ap()

    def _sem(self, n):
        if n not in self.sems:
            c = self.nc.semaphore(n); self.sems[n] = c.__enter__(); self.ctxs.append(c); self._semctx.add(c)
        return self.sems[n]

    def _need(self, eng, sem, val):
        w = self.waited[eng]
        if w.get(sem, 0) >= val:
            return
        w[sem] = val
        self.E[eng].wait_ge(self._sem(sem), val)

    def _deps(self, eng, reads, writes):
        for k in reads:
            lw = self.last_w.get(k)
            if lw is not None:
                self._need(eng, *lw)
        for k in writes:
            lw = self.last_w.get(k)
            if lw is not None:
                self._need(eng, *lw)
            for r in self.readers.get(k, ()):
                self._need(eng, *r)

    def _commit(self, tok, reads, writes):
        for k in reads:
            self.readers.setdefault(k, []).append(tok)
        for k in writes:
            self.last_w[k] = tok
            self.readers[k] = []

    def op(self, eng, fn, reads=(), writes=()):
        self._deps(eng, reads, writes)
        self.cnt[eng] += 1
        tok = ("S_" + eng, self.cnt[eng])
        fn(self.E[eng]).then_inc(self._sem(tok[0]), 1)
        if eng == "pe":
            self.waited[eng][tok[0]] = tok[1]
        self._commit(tok, reads, writes)
        self.n_inst += 1

    def dma(self, q, out, in_, r=(), w=(), **kw):
        reads, writes = r, w
        self._deps(q, reads, writes)
        i = self.dma_rr[q]; self.dma_rr[q] = (i + 1) % DMA_SEMS_PER_Q
        sem = "D_%s_%d" % (q, i)
        prev = self.dma_tot.get(sem, 0)
        if prev:
            self._need(q, sem, prev)
        tot = prev + 16
        self.dma_tot[sem] = tot
        self.E[q].dma_start(out=out, in_=in_, **kw).then_inc(self._sem(sem), 16)
        self._commit((sem, tot), reads, writes)
        self.n_inst += 1

    def mark(self):
        return len(self.ctxs)

    def barrier(self):
        for x in ENGS:
            for e in ENGS:
                if self.cnt[e]:
                    self._need(x, "S_" + e, self.cnt[e])
            for sem, tot in self.dma_tot.items():
                self._need(x, sem, tot)

    def release(self, m):
        self.barrier()
        keep = []
        for c in reversed(self.ctxs[m:]):
            if c in self._semctx:
                keep.append(c)
            else:
                c.__exit__(None, None, None)
        self.ctxs = self.ctxs[:m] + list(reversed(keep))

    def finish(self, final_keys):
        for k in final_keys:
            lw = self.last_w.get(k)
            if lw is not None:
                self._need("sp", *lw)
        for c in reversed(self.ctxs):
            c.__exit__(None, None, None)
        return self.nc

    def mm(self, out, lhsT, rhs, start, stop, r=(), w=()):
        self.op("pe", lambda e: e.matmul(out, lhsT=lhsT, rhs=rhs, start=start, stop=stop), r, w)

    def tr(self, out, in_, ident, r=(), w=()):
        self.op("pe", lambda e: e.transpose(out=out, in_=in_, identity=ident), r, w)

    def act(self, out, in_, func, r=(), w=(), **kw):
        self.op("act", lambda e: e.activation(out=out, in_=in_, func=func, **kw), r, w)

    def tt(self, eng, out, in0, in1, op, r=(), w=()):
        self.op(eng, lambda e: e.tensor_tensor(out=out, in0=in0, in1=in1, op=op), r, w)

    def ts(self, eng, out, in0, s1, op0, s2=None, op1=None, r=(), w=(), **kw):
        if op1 is None:
            self.op(eng, lambda e: e.tensor_scalar(out=out, in0=in0, scalar1=s1, scalar2=None, op0=op0, **kw), r, w)
        else:
            self.op(eng, lambda e: e.tensor_scalar(out=out, in0=in0, scalar1=s1, scalar2=s2, op0=op0, op1=op1, **kw), r, w)

    def stt(self, out, in0, scalar, in1, op0, op1, r=(), w=()):
        self.op("dve", lambda e: e.scalar_tensor_tensor(out=out, in0=in0, scalar=scalar, in1=in1, op0=op0, op1=op1), r, w)

    def cp(self, eng, out, in_, r=(), w=()):
        if eng == "act":
            self.op("act", lambda e: e.copy(out=out, in_=in_), r, w)
        else:
            self.op(eng, lambda e: e.tensor_copy(out=out, in_=in_), r, w)

    def memset(self, eng, ap, val, w=()):
        self.op(eng, lambda e: e.memset(ap, val), (), w)


class Rot:
    def __init__(self, P, name, n, shape, dt, psum=False):
        P.uid += 1
        self.name = "%s#%d" % (name, P.uid); self.n = n; self.i = 0
        if psum:
            self.t = [P.psv("%s%d" % (name, j), shape, dt) for j in range(n)]
        else:
            self.t = [P.sb("%s%d" % (name, j), shape, dt) for j in range(n)]

    def next(self):
        j = self.i; self.i = (j + 1) % self.n
        return self.t[j], (self.name, j)


def bc_row(ap_row, n):
    return ap_row.to_broadcast([128, n])


class K:
    def __init__(self, cfg):
        self.cfg = cfg
        self.P = Prog()
        P = self.P
        dr = lambda n, s, dt=F32: P.dram(n, s, dt, kind="ExternalInput")
        self.I = I = {}
        I["xin"] = dr("xin", [T, D])
        I["cond2"] = dr("cond2", [128, 8, 2])
        I["ada_w"] = dr("ada_w", [DEPTH, D, 6 * D])
        I["ada_b"] = dr("ada_b", [DEPTH, 6 * D])
        I["norm1_g"] = dr("norm1_g", [DEPTH, D])
        I["norm2_g"] = dr("norm2_g", [DEPTH, D])
        I["final_g"] = dr("final_g", [1, D])
        I["mla_w_in"] = dr("mla_w_in", [2, D, 704])
        I["mla_q_g"] = dr("mla_q_g", [2, 384])
        I["mla_kv_g"] = dr("mla_kv_g", [2, 256])
        I["mla_w_uq"] = dr("mla_w_uq", [2, 384, 1536])
        I["mla_w_ukv"] = dr("mla_w_ukv", [2, 256, 2048])
        I["mla_w_out"] = dr("mla_w_out", [2, D, D])
        I["cosT"] = dr("cosT", [64, T])
        I["sinT"] = dr("sinT", [64, T])
        I["peer_w_q"] = dr("peer_w_q", [DEPTH, D, 2048])
        I["skT"] = dr("skT", [DEPTH, 128, 16, 128])
        I["peer_uT"] = dr("peer_uT", [DEPTH, D, 16384])
        I["peer_v"] = dr("peer_v", [DEPTH, 16384, D])
        I["even_w_in"] = dr("even_w_in", [2, D, 3600])
        I["even_w_out"] = dr("even_w_out", [2, D, D])
        I["conv_wT"] = dr("conv_wT", [2, 12, 128, 5])
        I["gdn_ab"] = dr("gdn_ab", [2, 2, 8])
        I["gdn_norm_g"] = dr("gdn_norm_g", [2, 128])
        I["rpbg"] = dr("rpbg", [2, 64, 15, 8, 64])
        I["namask"] = dr("namask", [64, 64])
        self.out = P.dram("out", [SEQ, D], F32, kind="ExternalOutput")
        self.xres = P.dram("xres", [T, D], F32)
        self.ada_s = P.dram("ada_s", [DEPTH, 2, 6 * D], F32)
        self.attn = P.dram("attn", [T, D], F32)
        self.h2T = P.dram("h2T", [D, T], BF16)
        self.UV = P.dram("UV", [128, 128, 2048], BF16)
        self.GTd = [P.dram("GTd0", [34, 128, 128, 128], BF16), P.dram("GTd1", [NT - 34, 128, 128, 128], BF16)]
        self.sel = P.dram("sel", [NT, 128, 3, 128], F32)
        self.dbg = {}
        for name, shape in cfg.get("dbg", {}).items():
            self.dbg[name] = P.dram("dbg_" + name, list(shape), F32, kind="ExternalOutput")
        self.consts()

    def consts(self):
        P = self.P
        self.identf = P.sb("identf", [128, 128], F32)
        self.ident = P.sb("ident", [128, 128], BF16)
        P.memset("pool", self.identf[:], 1.0, w=["identf"])
        P.op("pool", lambda e: e.affine_select(out=self.identf[:], in_=self.identf[:], pattern=[[-1, 128]],
                                               compare_op=ALU.is_equal, fill=0.0, base=0, channel_multiplier=1),
             ["identf"], ["identf"])
        P.cp("dve", self.ident[:], self.identf[:], ["identf"], ["ident"])
        self.iotab = P.sb("iotab", [128, 128], BF16)
        self.iotaf = P.sb("iotaf", [128, 128], F32)
        P.op("pool", lambda e: e.iota(self.iotaf[:], pattern=[[1, 128]], base=0, channel_multiplier=0,
                                      allow_small_or_imprecise_dtypes=True), (), ["iotaf"])
        P.cp("dve", self.iotab[:], self.iotaf[:], ["iotaf"], ["iotab"])
        self.modA = [P.sb("modA%d" % s, [128, D], F32) for s in range(2)]
        self.modS = [P.sb("modS%d" % s, [128, D], F32) for s in range(2)]
        self.modG = [P.sb("modG%d" % s, [128, D], F32) for s in range(2)]
        self.ng = P.sb("ng", [128, D], F32)
        self.xt = Rot(P, "xt", 2, [128, D], F32)
        self.hb = Rot(P, "hb", 2, [128, D], BF16)
        self.tmpf = Rot(P, "tmpf", 2, [128, D], F32)
        self.small = Rot(P, "small", 4, [128, 4], F32)
        self.junk = P.sb("junk", [128, D], F32)

    def phase_ada(self):
        P, I = self.P, self.I
        m = P.mark()
        cs = P.sb("cs", [128, 8, 2], F32)
        P.dma("sp", cs[:], I["cond2"][:, :, :], w=["cs"])
        P.act(cs[:], cs[:], AF.Silu, ["cs"], ["cs"])
        adab = P.sb("adab", [2, 6 * D], F32)
        arow = P.sb("arow", [2, 6 * D], F32)
        wt = Rot(P, "adaw", 2, [128, 8, 512], F32)
        pa = Rot(P, "pada", 2, [128, 512], F32, psum=True)
        for l in self.cfg["layers"]:
            P.dma("sp", adab[:], I["ada_b"][l:l + 1, :].to_broadcast([2, 6 * D]), w=["adab"])
            wv = I["ada_w"][l].rearrange("(kc p) n -> p kc n", p=128)
            for j in range(12):
                w, wk = wt.next()
                P.dma("sp", w[:], wv[:, :, j * 512:(j + 1) * 512], w=[wk])
                ps, pk = pa.next()
                for kc in range(8):
                    P.mm(ps[0:2, :], cs[:, kc, :], w[:, kc, :], kc == 0, kc == 7, ["cs", wk], [pk])
                P.tt("dve", arow[:, j * 512:(j + 1) * 512], ps[0:2, :], adab[:, j * 512:(j + 1) * 512], ALU.add,
                     [pk, "adab"], ["arow"])
            P.dma("sp", self.ada_s[l, :, :], arow[:], ["arow"], [("ada_s", l)])
        P.release(m)

    def load_AS(self, l, which, streams=(0, 1)):
        P, I = self.P, self.I
        off = (which - 1) * 3 * D
        ngd = I["norm1_g"] if which == 1 else I["norm2_g"]
        P.dma("sp", self.ng[:], ngd[l:l + 1, :].to_broadcast([128, D]), w=["ng"])
        for s in streams:
            row = self.ada_s[l, s:s + 1, :]
            P.dma("sp", self.modS[s][:], row[:, off:off + D].to_broadcast([128, D]), [("ada_s", l)], [("modS", s)])
            P.dma("sp", self.modA[s][:], row[:, off + D:off + 2 * D].to_broadcast([128, D]), [("ada_s", l)], [("modA", s)])
            P.stt(self.modA[s][:], self.modA[s][:], 1.0, self.ng[:], ALU.add, ALU.mult, [("modA", s), "ng"], [("modA", s)])

    def load_gate(self, l, which, streams=(0, 1)):
        P = self.P
        off = (which - 1) * 3 * D
        for s in streams:
            row = self.ada_s[l, s:s + 1, :]
            P.dma("sp", self.modG[s][:], row[:, off + 2 * D:off + 3 * D].to_broadcast([128, D]), [("ada_s", l)], [("modG", s)])

    def rstd(self, x_ap, n, xk, eps=EPS):
        P = self.P
        sm, sk = self.small.next()
        P.act(self.junk[:, 0:n], x_ap, AF.Square, [xk], ["junk", sk], accum_out=sm[:, 0:1])
        P.act(sm[:, 1:2], sm[:, 0:1], AF.Sqrt, [sk], [sk], scale=1.0 / n, bias=eps)
        P.op("dve", lambda e: e.reciprocal(out=sm[:, 2:3], in_=sm[:, 1:2]), [sk], [sk])
        return sm[:, 2:3], sk

    def norm_mod_T(self, x_ap, xk, s, hT_dst, hT_key, pst):
        P = self.P
        r, rk = self.rstd(x_ap, D, xk)
        tf, tk = self.tmpf.next()
        P.stt(tf[:], x_ap, r, self.modA[s][:], ALU.mult, ALU.mult, [xk, rk, ("modA", s)], [tk])
        hb, hk = self.hb.next()
        P.tt("dve", hb[:], tf[:], self.modS[s][:], ALU.add, [tk, ("modS", s)], [hk])
        ps, pk = pst.next()
        for kc in range(8):
            P.tr(ps[:, kc, :], hb[:, kc * 128:(kc + 1) * 128], self.ident[:], [hk, "ident"], [pk])
        P.cp("act", hT_dst, ps[:], [pk], [hT_key])


def blocks(want_ctx=True):
    b = [(0, 256)] if want_ctx else []
    return b + [(256 + 512 * i, 512) for i in range(16)]


def rot_cols(P, eng, dst, src, r, w):
    for (d0, s0, sign) in ((0, 16, -1.0), (16, 0, 1.0), (32, 48, -1.0), (48, 32, 1.0)):
        P.ts(eng, dst[:, :, d0:d0 + 16], src[:, :, s0:s0 + 16], sign, ALU.mult, r=r, w=w)


class KM(K):
    def phase_init(self):
        P = self.P
        src = self.I["xin"]
        for i in range(0, NT, 6):
            P.dma("sp", self.xres[i * 128:(i + 6) * 128, :], src[i * 128:(i + 6) * 128, :], (),
                  [("x", j) for j in range(i, i + 6)])

    def phase_mla_proj(self, l):
        P, I = self.P, self.I
        i = l // 2
        m = P.mark()
        self.QnT = P.dram("QnT%d" % l, [8, 128, T], BF16)
        self.QrT = P.dram("QrT%d" % l, [8, 64, T], BF16)
        self.KnT = P.dram("KnT%d" % l, [8, 128, T], BF16)
        self.KrT = P.dram("KrT%d" % l, [64, T], BF16)
        self.Vd = P.dram("Vd%d" % l, [T, D], BF16)
        Win = P.sb("Win", [128, 8, 704], BF16)
        P.dma("pool", Win[:], I["mla_w_in"][i].rearrange("(kc p) n -> p kc n", p=128), w=["Win"])
        WinRot = P.sb("WinRot", [128, 8, 64], BF16)
        rot_cols(P, "dve", WinRot[:, :, :], Win[:, :, 640:704], ["Win"], ["WinRot"])
        Wuq = P.sb("Wuq", [128, 3, 1536], BF16)
        P.dma("pool", Wuq[:], I["mla_w_uq"][i].rearrange("(kc p) n -> p kc n", p=128), w=["Wuq"])
        WuqRot = P.sb("WuqRot", [128, 3, 8, 64], BF16)
        for kc in range(3):
            src = Wuq[:, kc, :].rearrange("p (h x) -> p h x", x=192)[:, :, 128:192]
            rot_cols(P, "dve", WuqRot[:, kc, :, :], src, ["Wuq"], ["WuqRot"])
        Wukv = P.sb("Wukv", [128, 2, 2048], BF16)
        P.dma("pool", Wukv[:], I["mla_w_ukv"][i].rearrange("(kc p) n -> p kc n", p=128), w=["Wukv"])
        qg = P.sb("qg", [128, 384], F32)
        kvg = P.sb("kvg", [128, 256], F32)
        P.dma("sp", qg[:], I["mla_q_g"][i:i + 1, :].to_broadcast([128, 384]), w=["qg"])
        P.dma("sp", kvg[:], I["mla_kv_g"][i:i + 1, :].to_broadcast([128, 256]), w=["kvg"])
        self.load_AS(l, 1)

        pst = Rot(P, "pst", 2, [128, 8, 128], BF16, psum=True)
        psp = Rot(P, "psp", 1, [128, 1024], F32, psum=True)
        psu = Rot(P, "psu", 3, [128, 512], F32, psum=True)
        hTr = Rot(P, "hT", 2, [128, 8, 512], BF16)
        cTr = Rot(P, "cT", 2, [128, 5, 512], BF16)
        cqn = Rot(P, "cqn", 2, [128, 640], BF16)
        cst = Rot(P, "cst", 2, [64, 2, 512], F32)
        stQn = Rot(P, "stQn", 2, [128, 8, 512], BF16)
        stKn = Rot(P, "stKn", 2, [128, 8, 512], BF16)
        stQr = Rot(P, "stQr", 2, [64, 8, 512], BF16)
        stKr = Rot(P, "stKr", 2, [64, 512], BF16)
        rtmp = Rot(P, "rtmp", 2, [64, 2, 512], F32)
        vsb = Rot(P, "vsb", 2, [128, D], BF16)

        def rope_out(ps1, k1, ps2, k2, cs, ck, n, dst, dk):
            rt, rk = rtmp.next()
            P.tt("dve", rt[:, 0, 0:n], ps1[0:64, 0:n], cs[:, 0, 0:n], ALU.mult, [k1, ck], [rk])
            P.tt("dve", rt[:, 1, 0:n], ps2[0:64, 0:n], cs[:, 1, 0:n], ALU.mult, [k2, ck], [rk])
            P.tt("dve", dst, rt[:, 0, 0:n], rt[:, 1, 0:n], ALU.add, [rk], [dk])

        for (t0, n) in blocks(True):
            s = 0 if t0 >= CTX else 1
            nt = n // 128
            hT, hk = hTr.next()
            cT, ck_ = cTr.next()
            cs, csk = cst.next()
            P.dma("sp", cs[:, 0, 0:n], I["cosT"][:, t0:t0 + n], w=[csk])
            P.dma("sp", cs[:, 1, 0:n], I["sinT"][:, t0:t0 + n], w=[csk])
            for j in range(nt):
                ti = t0 // 128 + j
                xt, xk = self.xt.next()
                P.dma("sp", xt[:], self.xres[ti * 128:(ti + 1) * 128, :], [("x", ti)], [xk])
                self.norm_mod_T(xt[:], xk, s, hT[:, :, j * 128:(j + 1) * 128], hk, pst)
                pp, ppk = psp.next()
                for kc in range(8):
                    P.mm(pp[:, 0:512], hT[:, kc, j * 128:(j + 1) * 128], Win[:, kc, 0:512], kc == 0, kc == 7, [hk, "Win"], [ppk])
                for kc in range(8):
                    P.mm(pp[:, 512:640], hT[:, kc, j * 128:(j + 1) * 128], Win[:, kc, 512:640], kc == 0, kc == 7, [hk, "Win"], [ppk])
                r1, r1k = self.rstd(pp[:, 0:384], 384, ppk)
                r2, r2k = self.rstd(pp[:, 384:640], 256, ppk)
                cq, cqk = cqn.next()
                P.stt(cq[:, 0:384], pp[:, 0:384], r1, qg[:], ALU.mult, ALU.mult, [ppk, r1k, "qg"], [cqk])
                P.stt(cq[:, 384:640], pp[:, 384:640], r2, kvg[:], ALU.mult, ALU.mult, [ppk, r2k, "kvg"], [cqk])
                ps, pk = pst.next()
                for kc in range(5):
                    P.tr(ps[:, kc, :], cq[:, kc * 128:(kc + 1) * 128], self.ident[:], [cqk, "ident"], [pk])
                P.cp("act", cT[:, :, j * 128:(j + 1) * 128], ps[:, 0:5, :], [pk], [ck_])
            sQn, sQnk = stQn.next(); sKn, sKnk = stKn.next(); sQr, sQrk = stQr.next(); sKr, sKrk = stKr.next()
            for h in range(8):
                ps, pk = psu.next()
                for kc in range(3):
                    P.mm(ps[:, 0:n], Wuq[:, kc, h * 192:h * 192 + 128], cT[:, kc, 0:n], kc == 0, kc == 2, ["Wuq", ck_], [pk])
                P.cp("act", sQn[:, h, 0:n], ps[:, 0:n], [pk], [sQnk])
                ps, pk = psu.next()
                for kc in range(2):
                    P.mm(ps[:, 0:n], Wukv[:, kc, h * 256:h * 256 + 128], cT[:, 3 + kc, 0:n], kc == 0, kc == 1, ["Wukv", ck_], [pk])
                P.cp("act", sKn[:, h, 0:n], ps[:, 0:n], [pk], [sKnk])
                ps1, k1 = psu.next()
                for kc in range(3):
                    P.mm(ps1[0:64, 0:n], Wuq[:, kc, h * 192 + 128:h * 192 + 192], cT[:, kc, 0:n], kc == 0, kc == 2, ["Wuq", ck_], [k1])
                ps2, k2 = psu.next()
                for kc in range(3):
                    P.mm(ps2[0:64, 0:n], WuqRot[:, kc, h, :], cT[:, kc, 0:n], kc == 0, kc == 2, ["WuqRot", ck_], [k2])
                rope_out(ps1, k1, ps2, k2, cs, csk, n, sQr[:, h, 0:n], sQrk)
            ps1, k1 = psu.next()
            for kc in range(8):
                P.mm(ps1[0:64, 0:n], Win[:, kc, 640:704], hT[:, kc, 0:n], kc == 0, kc == 7, ["Win", hk], [k1])
            ps2, k2 = psu.next()
            for kc in range(8):
                P.mm(ps2[0:64, 0:n], WinRot[:, kc, :], hT[:, kc, 0:n], kc == 0, kc == 7, ["WinRot", hk], [k2])
            rope_out(ps1, k1, ps2, k2, cs, csk, n, sKr[:, 0:n], sKrk)
            bk = ("mlab", t0)
            P.dma("sp", self.QnT[:, :, t0:t0 + n].rearrange("h p t -> p h t"), sQn[:, :, 0:n], [sQnk], [("QnT", t0)])
            P.dma("sp", self.KnT[:, :, t0:t0 + n].rearrange("h p t -> p h t"), sKn[:, :, 0:n], [sKnk], [("KnT", t0)])
            P.dma("sp", self.QrT[:, :, t0:t0 + n].rearrange("h p t -> p h t"), sQr[:, :, 0:n], [sQrk], [("QrT", t0)])
            P.dma("sp", self.KrT[:, t0:t0 + n], sKr[:, 0:n], [sKrk], [("KrT", t0)])
            for j in range(nt):
                vs, vk = vsb.next()
                for hf in range(2):
                    ps, pk = psu.next()
                    for kc in range(2):
                        rhs = Wukv[:, kc, :].rearrange("p (h x) -> p h x", x=256)[:, 4 * hf:4 * hf + 4, 128:256]
                        P.mm(ps[:, :], cT[:, 3 + kc, j * 128:(j + 1) * 128], rhs, kc == 0, kc == 1, ["Wukv", ck_], [pk])
                    P.cp("act", vs[:, hf * 512:(hf + 1) * 512], ps[:, :], [pk], [vk])
                ti = t0 // 128 + j
                P.dma("sp", self.Vd[ti * 128:(ti + 1) * 128, :], vs[:], [vk], [("Vd", ti)])
        P.release(m)

    def phase_mla_attn(self, l, want_ctx):
        P = self.P
        m = P.mark()
        scale = 192.0 ** -0.5
        kn = Rot(P, "kn", 2, [128, T], BF16)
        va = Rot(P, "va", 2, [128, NT, 129], BF16)
        kr = P.sb("kr", [64, T], BF16)
        qn = Rot(P, "qn", 2, [128, 512], BF16)
        qr = Rot(P, "qr", 2, [64, 512], BF16)
        pT = Rot(P, "pT", 4, [128, 512], BF16)
        psS = Rot(P, "psS", 3, [128, 512], F32, psum=True)
        psOT = Rot(P, "psOT", 2, [128, 512], F32, psum=True)
        psR = Rot(P, "psR", 2, [128, 512], F32, psum=True)
        onesb = P.sb("onesb", [128, 128], BF16)
        P.memset("pool", onesb[:], 1.0, w=["onesb"])
        rinv = Rot(P, "rinv", 2, [128, 512], F32)
        osb = Rot(P, "osb", 2, [128, 512], BF16)
        if not hasattr(self, "attnT"):
            self.attnT = P.dram("attnT", [D, T], BF16)
        for t in va.t:
            P.memset("pool", t[:, :, 128:129], 1.0, w=[(va.name, va.t.index(t))])
        allq = [("QnT", b[0]) for b in blocks(True)]
        P.dma("sp", kr[:], self.KrT[:, :], [("KrT", b[0]) for b in blocks(True)], ["kr"])
        for h in range(8):
            k_t, kk = kn.next()
            v_t, vk = va.next()
            P.dma("sp", k_t[:], self.KnT[h, :, :], [("KnT", b[0]) for b in blocks(True)], [kk])
            for q4 in range(0, NT, 11):
                P.dma("sp", v_t[:, q4:q4 + 11, 0:128],
                      self.Vd[q4 * 128:(q4 + 11) * 128, h * 128:(h + 1) * 128].rearrange("(kt p) d -> p kt d", p=128),
                      [("Vd", ti) for ti in range(q4, q4 + 11)], [vk])
            for (t0, n) in blocks(want_ctx):
                nkt = NT if t0 >= CTX else 2
                q_t, qk = qn.next()
                qr_t, qrk = qr.next()
                P.dma("sp", q_t[:, 0:n], self.QnT[h, :, t0:t0 + n], [("QnT", t0)], [qk])
                P.dma("sp", qr_t[:, 0:n], self.QrT[h, :, t0:t0 + n], [("QrT", t0)], [qrk])
                nj = n // 128
                def s_stage(kt):
                    ps, pk = psS.next()
                    P.mm(ps[:, 0:n], k_t[:, kt * 128:(kt + 1) * 128], q_t[:, 0:n], True, False, [kk, qk], [pk])
                    P.mm(ps[:, 0:n], kr[:, kt * 128:(kt + 1) * 128], qr_t[:, 0:n], False, True, ["kr", qrk], [pk])
                    p_t, ptk = pT.next()
                    P.act(p_t[:, 0:n], ps[:, 0:n], AF.Exp, [pk], [ptk], scale=scale)
                    return p_t, ptk

                oT, oTk = psOT.next()
                rT, rTk = psR.next()

                def pv_stage(kt, p_t, ptk):
                    P.mm(oT[:, 0:n], v_t[:, kt, 0:128], p_t[:, 0:n], kt == 0, kt == nkt - 1, [ptk, vk], [oTk])
                    P.mm(rT[:, 0:n], onesb[:], p_t[:, 0:n], kt == 0, kt == nkt - 1, [ptk, "onesb"], [rTk])

                q_ = [s_stage(0)]
                if nkt > 1:
                    q_.append(s_stage(1))
                for kt in range(nkt):
                    if kt + 2 < nkt:
                        q_.append(s_stage(kt + 2))
                    pv_stage(kt, *q_.pop(0))
                ri, rik = rinv.next()
                P.op("dve", lambda e, ri=ri, rT=rT, n=n: e.reciprocal(out=ri[:, 0:n], in_=rT[:, 0:n]), [rTk], [rik])
                o_t, ok = osb.next()
                P.tt("dve", o_t[:, 0:n], oT[:, 0:n], ri[:, 0:n], ALU.mult, [oTk, rik], [ok])
                P.dma("sp", self.attnT[h * 128:(h + 1) * 128, t0:t0 + n], o_t[:, 0:n], [ok], ())
        P.release(m)

    def phase_outproj(self, l, wout_ap, want_ctx, featmajor=False):
        P = self.P
        m = P.mark()
        Wout = P.sb("Wout", [128, 8, D], BF16)
        P.dma("pool", Wout[:], wout_ap.rearrange("(kc p) n -> p kc n", p=128), w=["Wout"])
        self.load_gate(l, 1)
        self.load_AS(l, 2)
        pst = Rot(P, "pst", 2, [128, 8, 128], BF16, psum=True)
        psy = Rot(P, "psy", 2, [128, D], F32, psum=True)
        at = Rot(P, "at", 2, [128, D], F32)
        ab = Rot(P, "ab", 2, [128, D], BF16)
        aT = Rot(P, "aT", 2, [128, 8, 128], BF16)
        xn = Rot(P, "xn", 2, [128, D], F32)
        h2s = Rot(P, "h2s", 2, [128, 8, 128], BF16)
        def op_a(ti):
            if featmajor:
                aT_t, aTk = aT.next()
                P.dma("sp", aT_t[:], self.attnT[:, ti * 128:(ti + 1) * 128].rearrange("(kc p) t -> p kc t", p=128), (), [aTk])
                xt, xk = self.xt.next()
                P.dma("sp", xt[:], self.xres[ti * 128:(ti + 1) * 128, :], (), [xk])
                return ti, aT_t, aTk, xt, xk
            a_t, ak = at.next()
            P.dma("sp", a_t[:], self.attn[ti * 128:(ti + 1) * 128, :], (), [ak])
            b_t, bk = ab.next()
            P.cp("act", b_t[:], a_t[:], [ak], [bk])
            ps, pk = pst.next()
            for kc in range(8):
                P.tr(ps[:, kc, :], b_t[:, kc * 128:(kc + 1) * 128], self.ident[:], [bk, "ident"], [pk])
            aT_t, aTk = aT.next()
            P.cp("act", aT_t[:], ps[:], [pk], [aTk])
            xt, xk = self.xt.next()
            P.dma("sp", xt[:], self.xres[ti * 128:(ti + 1) * 128, :], (), [xk])
            return ti, aT_t, aTk, xt, xk

        def op_b(ti, aT_t, aTk, xt, xk):
            s = 0 if ti >= 2 else 1
            py, pyk = psy.next()
            for hf in range(2):
                for kc in range(8):
                    P.mm(py[:, hf * 512:(hf + 1) * 512], aT_t[:, kc, :], Wout[:, kc, hf * 512:(hf + 1) * 512], kc == 0, kc == 7,
                         [aTk, "Wout"], [pyk])
            tf, tk = self.tmpf.next()
            P.tt("dve", tf[:], py[:], self.modG[s][:], ALU.mult, [pyk, ("modG", s)], [tk])
            x_n, xnk = xn.next()
            P.tt("dve", x_n[:], tf[:], xt[:], ALU.add, [tk, xk], [xnk])
            P.dma("sp", self.xres[ti * 128:(ti + 1) * 128, :], x_n[:], [xnk], [("x", ti)])
            h_t, hk = h2s.next()
            self.norm_mod_T(x_n[:], xnk, s, h_t[:], hk, pst)
            P.dma("sp", self.h2T[:, ti * 128:(ti + 1) * 128].rearrange("(kc p) t -> p kc t", p=128), h_t[:], [hk], [("h2T", ti)])

        tiles = list(range(0 if want_ctx else 2, NT))
        prev = op_a(tiles[0])
        for ii in range(len(tiles)):
            nxt = op_a(tiles[ii + 1]) if ii + 1 < len(tiles) else None
            op_b(*prev)
            prev = nxt
        P.release(m)

    def phase_peer_prep(self, l):
        P, I = self.P, self.I
        uT = I["peer_uT"][l].rearrange("(kc p) e -> p kc e", p=128)
        for c in range(128):
            P.dma("pool", self.UV[c, :, 0:1024].rearrange("p (kc e) -> p kc e", e=128), uT[:, :, c * 128:(c + 1) * 128], (), [("UV", c)])
            P.dma("pool", self.UV[c, :, 1024:2048], I["peer_v"][l, c * 128:(c + 1) * 128, :], (), [("UV", c)])

    def phase_peer_sel(self, l, want_ctx):
        P, I = self.P, self.I
        m = P.mark()
        Wq = P.sb("Wq", [128, 8, 2048], BF16)
        P.dma("pool", Wq[:], I["peer_w_q"][l].rearrange("(kc p) n -> p kc n", p=128), w=["Wq"])
        skT = P.sb("skT", [128, 16, 128], F32)
        P.dma("sp", skT[:], I["skT"][l], w=["skT"])
        h2 = Rot(P, "h2", 2, [128, 8, 128], BF16)
        qTsR = Rot(P, "qTs", 2, [128, 16, 128], F32)
        s_sbR = Rot(P, "s_sb", 2, [128, 16, 128], F32)
        s_tmp = P.sb("s_tmp", [128, 16, 128], F32)
        stop_ = P.sb("stop", [128, 16, 16], F32)
        sidx = P.sb("sidx", [128, 16, 16], U32)
        sidf = P.sb("sidf", [128, 16, 16], F32)
        comb = P.sb("comb", [128, 8, 256], F32)
        comb2 = P.sb("comb2", [128, 8, 256], F32)
        ctop = P.sb("ctop", [128, 8, 16], F32)
        cidx = P.sb("cidx", [128, 8, 16], U32)
        cj = P.sb("cj", [128, 2, 8, 16], U32)
        cjf = P.sb("cjf", [128, 2, 8, 16], F32)
        oh = P.sb("oh", [128, 8, 16, 16], F32)
        i12 = P.sb("i12", [128, 3, 128], F32)
        i12T = Rot(P, "i12T", 2, [128, 3, 128], F32)
        allA = [("stopA", hp) for hp in range(16)]; allB = [("stopB", hp) for hp in range(16)]
        allI = [("sidxA", hp) for hp in range(16)] + [("sidxB", hp) for hp in range(16)]
        nmx = P.sb("nmx", [128, 8], F32)
        zs = P.sb("zs", [128, 8], F32)
        psA = Rot(P, "psA", 3, [128, 512], F32, psum=True)
        psS = P.ps("psS4", [128, 4, 512], F32)
        iota16 = self.iotaf[:, 0:16]
        for ti in range(0 if want_ctx else 2, NT):
            h_t, hk = h2.next()
            qTs, qTsk = qTsR.next()
            s_sb, ssk = s_sbR.next()
            P.dma("sp", h_t[:], self.h2T[:, ti * 128:(ti + 1) * 128].rearrange("(kc p) t -> p kc t", p=128), (), [hk])
            for hp4 in range(4):
                ps, pk = psA.next()
                for q in range(4):
                    hp = hp4 * 4 + q
                    for kc in range(8):
                        P.mm(ps[:, q * 128:(q + 1) * 128], Wq[:, kc, hp * 128:(hp + 1) * 128], h_t[:, kc, :], kc == 0, kc == 7,
                             ["Wq", hk], [pk])
                P.cp("act", qTs[:, hp4 * 4:hp4 * 4 + 4, :], ps[:, :].rearrange("p (a n) -> p a n", n=128), [pk], [qTsk])
            for hp in range(16):
                P.mm(psS[:, hp // 4, (hp % 4) * 128:(hp % 4 + 1) * 128], qTs[:, hp, :], skT[:, hp, :], True, True,
                     [qTsk, "skT"], [("psS", hp // 4)])
            for g in range(4):
                P.cp("act", s_sb[:, 4 * g:4 * g + 4, :], psS[:, g, :].rearrange("p (a n) -> p a n", n=128), [("psS", g)], [ssk])
            for hp in range(16):
                P.op("dve", lambda e, hp=hp: e.max(out=stop_[:, hp, 0:8], in_=s_sb[:, hp, :]), [ssk], [("stopA", hp)])
            for hp in range(16):
                P.op("dve", lambda e, hp=hp: e.match_replace(out=s_tmp[:, hp, :], in_to_replace=stop_[:, hp, 0:8], in_values=s_sb[:, hp, :],
                                                             imm_value=-1e30), [ssk, ("stopA", hp)], [("s_tmp", hp)])
            for hp in range(16):
                P.op("dve", lambda e, hp=hp: e.max(out=stop_[:, hp, 8:16], in_=s_tmp[:, hp, :]), [("s_tmp", hp)], [("stopB", hp)])
            for hp in range(16):
                P.op("dve", lambda e, hp=hp: e.max_index(out=sidx[:, hp, 0:8], in_max=stop_[:, hp, 0:8], in_values=s_sb[:, hp, :]),
                     [ssk, ("stopA", hp)], [("sidxA", hp)])
            for hp in range(16):
                P.op("dve", lambda e, hp=hp: e.max_index(out=sidx[:, hp, 8:16], in_max=stop_[:, hp, 8:16], in_values=s_sb[:, hp, :]),
                     [ssk, ("stopB", hp)], [("sidxB", hp)])
            P.cp("dve", sidf[:], sidx[:], allI, ["sidf"])
            st4 = stop_[:].rearrange("p (h two) j -> p h two j", two=2)
            in0 = st4[:, :, 0, :].unsqueeze(3).to_broadcast([128, 8, 16, 16])
            in1 = st4[:, :, 1, :].unsqueeze(2).to_broadcast([128, 8, 16, 16])
            P.tt("dve", comb[:].rearrange("p h (a b) -> p h a b", b=16), in0, in1, ALU.add, allA + allB, ["comb"])
            for h in range(8):
                P.op("dve", lambda e, h=h: e.max(out=ctop[:, h, 0:8], in_=comb[:, h, :]), ["comb"], [("ctopA", h)])
            for h in range(8):
                P.op("dve", lambda e, h=h: e.match_replace(out=comb2[:, h, :], in_to_replace=ctop[:, h, 0:8], in_values=comb[:, h, :],
                                                           imm_value=-1e30), ["comb", ("ctopA", h)], [("comb2", h)])
            for h in range(8):
                P.op("dve", lambda e, h=h: e.max(out=ctop[:, h, 8:16], in_=comb2[:, h, :]), [("comb2", h)], [("ctopB", h)])
            for h in range(8):
                P.op("dve", lambda e, h=h: e.max_index(out=cidx[:, h, 0:8], in_max=ctop[:, h, 0:8], in_values=comb[:, h, :]),
                     ["comb", ("ctopA", h)], [("cidxA", h)])
            for h in range(8):
                P.op("dve", lambda e, h=h: e.max_index(out=cidx[:, h, 8:16], in_max=ctop[:, h, 8:16], in_values=comb[:, h, :]),
                     ["comb", ("ctopB", h)], [("cidxB", h)])
            ctk = [("ctopA", h) for h in range(8)] + [("ctopB", h) for h in range(8)]
            cik = [("cidxA", h) for h in range(8)] + [("cidxB", h) for h in range(8)]
            P.ts("dve", cj[:, 0, :, :], cidx[:], 4, ALU.logical_shift_right, r=cik, w=["cj"])
            P.ts("dve", cj[:, 1, :, :], cidx[:], 15, ALU.bitwise_and, r=cik, w=["cj"])
            P.cp("dve", cjf[:], cj[:], ["cj"], ["cjf"])
            sd4 = sidf[:].rearrange("p (h two) j -> p h two j", two=2)
            for w_ in range(2):
                P.tt("dve", oh[:], cjf[:, w_, :, :].unsqueeze(3).to_broadcast([128, 8, 16, 16]),
                     iota16.unsqueeze(1).unsqueeze(1).to_broadcast([128, 8, 16, 16]), ALU.is_equal, ["cjf", "iotaf"], ["oh"])
                P.tt("dve", oh[:], oh[:], sd4[:, :, w_, :].unsqueeze(2).to_broadcast([128, 8, 16, 16]), ALU.mult, ["oh", "sidf"], ["oh"])
                P.op("dve", lambda e, w_=w_: e.tensor_reduce(out=i12[:, w_, :].rearrange("p (h k) -> p h k", k=16), in_=oh[:],
                                                             axis=AX.X, op=ALU.add), ["oh"], ["i12"])
            P.ts("dve", nmx[:], ctop[:, :, 0], -1.0, ALU.mult, r=ctk, w=["nmx"])
            for h in range(8):
                P.act(i12[:, 2, h * 16:(h + 1) * 16], ctop[:, h, :], AF.Exp, ctk + ["nmx"], [("i12g", h), ("zs", h)],
                      bias=nmx[:, h:h + 1], scale=1.0, accum_out=zs[:, h:h + 1])
            zk_ = [("zs", h) for h in range(8)]; gk_ = [("i12g", h) for h in range(8)]
            P.op("dve", lambda e: e.reciprocal(out=zs[:], in_=zs[:]), zk_, zk_)
            g3 = i12[:, 2, :].rearrange("p (h k) -> p h k", k=16)
            P.tt("dve", g3, g3, zs[:].unsqueeze(2).to_broadcast([128, 8, 16]), ALU.mult, ["i12"] + zk_ + gk_, ["i12"] + gk_)
            iT, iTk = i12T.next()
            for w_ in range(3):
                ps, pk = psA.next()
                P.tr(ps[:, 0:128], i12[:, w_, :], self.identf[:], ["i12", "identf"] + gk_, [pk])
                P.cp("act", iT[:, w_, :], ps[:, 0:128], [pk], [iTk])
            P.dma("sp", self.sel[ti], iT[:], [iTk], [("sel", ti)])
        P.release(m)

    def phase_peer_g(self, l, want_ctx):
        P = self.P
        m = P.mark()
        i12T = Rot(P, "i12T", 2, [128, 3, 128], F32)
        i12Tb = Rot(P, "i12Tb", 2, [128, 3, 128], BF16)
        Aoh = Rot(P, "Aoh", 2, [128, 64, 128], BF16)
        Boh = Rot(P, "Boh", 2, [128, 64, 128], BF16)
        GTs = Rot(P, "GTs", 2, [128, 128, 128], BF16)
        ni2r = Rot(P, "ni2", 2, [128, 128], F32)
        abt = Rot(P, "abt", 4, [128, 128], F32)
        NB_DVE = 64
        psA = Rot(P, "psG", 4, [128, 512], F32, psum=True)
        for ti in range(0 if want_ctx else 2, NT):
            iT, iTk = i12T.next()
            P.dma("sp", iT[:], self.sel[ti], (), [iTk])
            G_, Gk = GTs.next()
            ni2, nik = ni2r.next()
            P.ts("dve", ni2[:], iT[:, 1, :], -1.0, ALU.mult, r=[iTk], w=[nik])
            for g64 in range(2):
                A_, Ak = Aoh.next(); B_, Bk = Boh.next()
                for tt_ in range(64):
                    t = g64 * 64 + tt_
                    P.ts("dve", A_[:, tt_, :], self.iotab[:], iT[:, 0, t:t + 1], ALU.is_equal, iT[:, 2, t:t + 1], ALU.mult,
                         r=[iTk, "iotab"], w=[(Ak, tt_)])
                    if tt_ < NB_DVE:
                        P.ts("dve", B_[:, tt_, :], self.iotab[:], iT[:, 1, t:t + 1], ALU.is_equal, r=[iTk, "iotab"], w=[(Bk, tt_)])
                    else:
                        a_t, atk = abt.next()
                        P.act(a_t[:], self.iotaf[:], AF.Abs, ["iotaf", nik], [atk], bias=ni2[:, t:t + 1], scale=1.0)
                        P.act(B_[:, tt_, :], a_t[:], AF.Relu, [atk], [(Bk, tt_)], bias=1.0, scale=-1.0)
                for t4 in range(16):
                    ps, pk = psA.next()
                    for tq in range(4):
                        tt_ = t4 * 4 + tq
                        P.mm(ps[:, tq * 128:(tq + 1) * 128], B_[:, tt_, :], A_[:, tt_, :], True, True,
                             [(Ak, tt_), (Bk, tt_)], [pk])
                    c0 = g64 * 64 + t4 * 4
                    dst = G_[:, :, c0:c0 + 4].rearrange("p c t -> p t c")
                    P.cp("act", dst, ps[:, :].rearrange("p (t c) -> p t c", c=128), [pk], [Gk])
            P.dma("sp", self.GTd[ti // 34][ti % 34], G_[:], [Gk], ())
        P.release(m)

    def phase_peer(self, l, want_ctx):
        P, I = self.P, self.I
        m = P.mark()
        NJ = 3
        TB = 128 * NJ
        self.load_gate(l, 2)
        h2 = Rot(P, "h2", 2, [128, 8, TB], BF16)
        uv = Rot(P, "uv", 8, [128, 2048], BF16)
        gt = Rot(P, "gt", 3, [128, NJ, 8, 128], BF16)
        ga = Rot(P, "ga", 3, [128, TB], BF16)
        wt = Rot(P, "wt", 3, [128, TB], BF16)
        psA = Rot(P, "psA", 2, [128, 512], F32, psum=True)
        psV = P.ps("psV", [128, 2 * NJ, 512], F32)
        for b in range(T // TB):
            t0 = b * TB
            h_t, hk = h2.next()
            P.dma("sp", h_t[:], self.h2T[:, t0:t0 + TB].rearrange("(kc p) t -> p kc t", p=128), (), [hk])
            def stage_a(c):
                nonlocal g_t8, g8k
                if c % 8 == 0:
                    g_t8, g8k = gt.next()
                    for j in range(NJ):
                        ti = NJ * b + j
                        P.dma("sp", g_t8[:, j, :, :], self.GTd[ti // 34][ti % 34, :, c:c + 8, :], (), [g8k])
                u_t, uk = uv.next()
                P.dma("sp", u_t[:], self.UV[c], [("UV", c)], [uk])
                ps, pk = psA.next()
                for kc in range(8):
                    P.mm(ps[:, 0:TB], u_t[:, kc * 128:(kc + 1) * 128], h_t[:, kc, :], kc == 0, kc == 7, [uk, hk], [pk])
                g_t, gk = ga.next()
                P.act(g_t[:], ps[:, 0:TB], AF.Gelu, [pk], [gk])
                w_t, wk = wt.next()
                P.tt("dve", w_t[:].rearrange("p (j t) -> p j t", j=NJ), g_t[:].rearrange("p (j t) -> p j t", j=NJ),
                     g_t8[:, :, c % 8, :], ALU.mult, [gk, g8k], [wk])
                return w_t, wk, u_t, uk

            def stage_b(c, w_t, wk, u_t, uk):
                for j in range(NJ):
                    for hf in range(2):
                        P.mm(psV[:, j * 2 + hf, :], w_t[:, j * 128:(j + 1) * 128], u_t[:, 1024 + hf * 512:1024 + (hf + 1) * 512],
                             c == 0, c == 127, [wk, uk], [("psV", j * 2 + hf)])

            g_t8 = g8k = None
            prev = stage_a(0)
            for c in range(128):
                nxt = stage_a(c + 1) if c + 1 < 128 else None
                stage_b(c, *prev)
                prev = nxt
            for j in range(NJ):
                ti = t0 // 128 + j
                s_ = 0 if ti >= 2 else 1
                xt, xk = self.xt.next()
                P.dma("sp", xt[:], self.xres[ti * 128:(ti + 1) * 128, :], (), [xk])
                tf, tk = self.tmpf.next()
                P.tt("dve", tf[:], psV[:, 2 * j:2 * j + 2, :].rearrange("p a n -> p (a n)"), self.modG[s_][:], ALU.mult,
                     [("psV", 2 * j), ("psV", 2 * j + 1), ("modG", s_)], [tk])
                P.tt("dve", tf[:], tf[:], xt[:], ALU.add, [tk, xk], [tk])
                P.dma("sp", self.xres[ti * 128:(ti + 1) * 128, :], tf[:], [tk], [("x", ti)])
        P.release(m)

    def phase_final(self):
        P, I = self.P, self.I
        m = P.mark()
        fg = P.sb("fg", [128, D], F32)
        P.dma("sp", fg[:], I["final_g"][0:1, :].to_broadcast([128, D]), w=["fg"])
        ot = Rot(P, "ot", 2, [128, D], F32)
        for ti in range(2, NT):
            xt, xk = self.xt.next()
            P.dma("sp", xt[:], self.xres[ti * 128:(ti + 1) * 128, :], (), [xk])
            r, rk = self.rstd(xt[:], D, xk)
            o, ok = ot.next()
            P.stt(o[:], xt[:], r, fg[:], ALU.mult, ALU.mult, [xk, rk, "fg"], [ok])
            P.dma("sp", self.out[(ti - 2) * 128:(ti - 1) * 128, :], o[:], [ok], ["out"])
        P.release(m)


def build(cfg):
    k = KE(cfg)
    P = k.P
    if cfg.get("scopes"):
        for nm in [n for n in dir(k) if n.startswith("phase_") and n != "phase_even"]:
            def wrap(f, nm=nm):
                def g(*a, **kw):
                    with P.nc.named_scope(nm + "_" + str(a[0] if a else "")):
                        return f(*a, **kw)
                return g
            setattr(k, nm, wrap(getattr(k, nm)))
    layers = cfg["layers"]
    stop_after = cfg.get("stop_after")
    k.phase_init()
    k.phase_ada()
    P.barrier()

    def dump(name, src):
        if name in k.dbg:
            P.barrier()
            n = src.shape[0]
            for r0 in range(0, n, 1024):
                r1 = min(n, r0 + 1024)
                P.dma("sp", k.dbg[name][r0:r1, :], src[r0:r1, :], (), ["dbgout"])
            P.barrier()

    done = False
    for l in layers:
        want_ctx = l < DEPTH - 1
        k.phase_peer_prep(l)
        if l % 2 == 0:
            k.phase_even(l, want_ctx)
            wout = k.I["even_w_out"][l // 2]
        else:
            k.phase_mla_proj(l)
            k.phase_mla_attn(l, want_ctx)
            wout = k.I["mla_w_out"][l // 2]
        dump("attn%d" % l, k.attn)
        if stop_after == ("attn", l):
            done = True; break
        k.phase_outproj(l, wout, want_ctx, featmajor=(l % 2 == 1))
        dump("xm%d" % l, k.xres)
        if stop_after == ("outproj", l):
            done = True; break
        k.phase_peer_sel(l, True)
        k.phase_peer_g(l, True)
        k.phase_peer(l, True)
        dump("x%d" % l, k.xres)
    if not done:
        k.phase_final()
    P.barrier()
    return P.finish([]), k


def rope_tables():
    half = 32
    inv = 10000.0 ** (-np.arange(0, half, 2, dtype=np.float32) / half)
    t = np.arange(SEQ)
    row = (t // 64).astype(np.float32); col = (t % 64).astype(np.float32)
    ar = row[:, None] * inv[None, :]; ac = col[:, None] * inv[None, :]
    ang = np.concatenate([ar, ar, ac, ac], axis=-1).astype(np.float32)
    cosT = np.ones((64, T), np.float32); sinT = np.zeros((64, T), np.float32)
    cosT[:, CTX:] = np.cos(ang).T; sinT[:, CTX:] = np.sin(ang).T
    return cosT, sinT


def na_tables(rpb):
    q = np.arange(64); kc = np.arange(64)
    dc = np.clip(kc[:, None] - q[None, :], -15, 15) + 15
    g = rpb[:, :, :, dc]
    g = np.ascontiguousarray(np.transpose(g, (0, 3, 2, 1, 4))).astype(np.float32)
    cstart = np.clip(q - 8, 0, 48)
    ok = (kc[:, None] >= cstart[None, :]) & (kc[:, None] < cstart[None, :] + 16)
    mask = np.where(ok, 0.0, NEG).astype(np.float32)
    return g, mask


def host_inputs(inp, b):
    f = lambda a: np.ascontiguousarray(np.asarray(a, dtype=np.float32))
    cosT, sinT = rope_tables()
    rpbg, namask = na_tables(np.asarray(inp["na_rpb"], np.float32))
    c2 = np.stack([np.asarray(inp["c"][b]).reshape(8, 128).T, np.asarray(inp["c_ctx"]).reshape(8, 128).T], axis=-1)
    m = {
        "xin": f(np.concatenate([inp["ctx"][b], inp["x"][b]], axis=0)),
        "cond2": f(c2),
        "cosT": cosT, "sinT": sinT, "rpbg": rpbg, "namask": namask,
        "final_g": f(np.asarray(inp["final_g"]).reshape(1, D)),
        "conv_wT": f(np.transpose(np.asarray(inp["even_conv_w"]).reshape(2, 5, 12, 128), (0, 2, 3, 1))),
        "gdn_ab": f(np.stack([np.asarray(inp["gdn_a_log"]).reshape(2, 8), np.asarray(inp["gdn_dt_bias"]).reshape(2, 8)], axis=1)),
    }
    return m


_SHARED = None


def shared_inputs(inp):
    f = lambda a: np.ascontiguousarray(np.asarray(a, dtype=np.float32))
    sk = np.asarray(inp["peer_sub_keys"], np.float32).reshape(DEPTH, 16, 128, 128)
    m = {k: f(inp[k]) for k in ("ada_w", "ada_b", "norm1_g", "norm2_g", "mla_w_in", "mla_q_g", "mla_kv_g", "mla_w_uq",
                                "mla_w_ukv", "mla_w_out", "peer_w_q", "peer_v", "even_w_in", "even_w_out", "gdn_norm_g")}
    m["skT"] = f(np.transpose(sk, (0, 3, 1, 2)))
    m["peer_uT"] = f(np.transpose(np.asarray(inp["peer_u"], np.float32), (0, 2, 1)))
    return m


def kernel(**inputs):
    cfg = {"layers": [0, 1, 2, 3]}
    nc, k = build(cfg)
    sh = shared_inputs(inputs)
    in_maps = []
    for b in range(8):
        m = dict(sh); m.update(host_inputs(inputs, b))
        in_maps.append(m)
    res = run_bass_kernel_spmd(nc, in_maps, core_ids=list(range(8)))
    return np.stack([np.asarray(r["out"], np.float32) for r in res.results], axis=0)


class KE(KM):
    def phase_even_proj(self, l):
        P, I = self.P, self.I
        i = l // 2
        m = P.mark()
        if not hasattr(self, "qaT"):
            self.qaT = P.dram("qaT", [8, 64, T], BF16)
            self.kaT = P.dram("kaT", [8, 64, T], BF16)
            self.va = P.dram("va", [T, 8, 65], BF16)
            self.gpre = P.dram("gpre", [12, 128, T], F32)
            self.zd = P.dram("zd", [T, 512], F32)
            self.grd = P.dram("grd", [T, 16], F32)
        Win = P.sb("WinE", [128, 8, 3600], BF16)
        wv = I["even_w_in"][i].rearrange("(kc p) n -> p kc n", p=128)
        for kc in range(8):
            P.dma("pool", Win[:, kc, :], wv[:, kc, :], w=["Win"])
        self.load_AS(l, 1)
        pst = Rot(P, "pst", 2, [128, 8, 128], BF16, psum=True)
        psu = Rot(P, "psu", 4, [128, 512], F32, psum=True)
        hTr = Rot(P, "hT", 2, [128, 8, 512], BF16)
        stq = Rot(P, "stq", 2, [64, 8, 512], BF16)
        stk = Rot(P, "stk", 2, [64, 8, 512], BF16)
        stg = Rot(P, "stg", 3, [128, 512], F32)
        vsb = Rot(P, "vsbE", 2, [128, 8, 65], BF16)
        for t_ in vsb.t:
            P.memset("pool", t_[:, :, 64:65], 1.0, w=[(vsb.name, vsb.t.index(t_))])
        zsb = Rot(P, "zsbE", 2, [128, 512], F32)
        gsb = Rot(P, "gsbE", 2, [128, 16], F32)
        for (t0, n) in blocks(True):
            s = 0 if t0 >= CTX else 1
            nt = n // 128
            hT, hk = hTr.next()
            for j in range(nt):
                ti = t0 // 128 + j
                xt, xk = self.xt.next()
                P.dma("sp", xt[:], self.xres[ti * 128:(ti + 1) * 128, :], (), [xk])
                self.norm_mod_T(xt[:], xk, s, hT[:, :, j * 128:(j + 1) * 128], hk, pst)
            sq, sqk = stq.next(); sk_, skk = stk.next()
            for h in range(8):
                for (dst, dk, c0) in ((sq, sqk, 0), (sk_, skk, 512)):
                    ps, pk = psu.next()
                    for kc in range(8):
                        P.mm(ps[0:64, 0:n], Win[:, kc, c0 + h * 64:c0 + (h + 1) * 64], hT[:, kc, 0:n], kc == 0, kc == 7, ["Win", hk], [pk])
                    P.cp("act", dst[:, h, 0:n], ps[0:64, 0:n], [pk], [dk])
            P.dma("sp", self.qaT[:, :, t0:t0 + n].rearrange("h p t -> p h t"), sq[:, :, 0:n], [sqk], ())
            P.dma("sp", self.kaT[:, :, t0:t0 + n].rearrange("h p t -> p h t"), sk_[:, :, 0:n], [skk], ())
            for c in range(12):
                ps, pk = psu.next()
                for kc in range(8):
                    P.mm(ps[:, 0:n], Win[:, kc, 1536 + c * 128:1536 + (c + 1) * 128], hT[:, kc, 0:n], kc == 0, kc == 7, ["Win", hk], [pk])
                sg, sgk = stg.next()
                P.cp("act" if c % 2 else "dve", sg[:, 0:n], ps[:, 0:n], [pk], [sgk])
                P.dma("sp", self.gpre[c, :, t0:t0 + n], sg[:, 0:n], [sgk], ())
            for j in range(nt):
                ti = t0 // 128 + j
                tsl = slice(j * 128, (j + 1) * 128)
                ps, pk = psu.next()
                for kc in range(8):
                    P.mm(ps[:, :], hT[:, kc, tsl], Win[:, kc, 1024:1536], kc == 0, kc == 7, ["Win", hk], [pk])
                v_, vk = vsb.next()
                P.cp("act", v_[:, :, 0:64], ps[:, :].rearrange("p (h d) -> p h d", d=64), [pk], [vk])
                P.dma("sp", self.va[ti * 128:(ti + 1) * 128, :, :], v_[:], [vk], ())
                ps, pk = psu.next()
                for kc in range(8):
                    P.mm(ps[:, :], hT[:, kc, tsl], Win[:, kc, 3072:3584], kc == 0, kc == 7, ["Win", hk], [pk])
                z_, zk = zsb.next()
                P.cp("dve", z_[:], ps[:, :], [pk], [zk])
                P.dma("sp", self.zd[ti * 128:(ti + 1) * 128, :], z_[:], [zk], ())
                ps, pk = psu.next()
                for kc in range(8):
                    P.mm(ps[:, 0:16], hT[:, kc, tsl], Win[:, kc, 3584:3600], kc == 0, kc == 7, ["Win", hk], [pk])
                g_, gk = gsb.next()
                P.cp("act", g_[:], ps[:, 0:16], [pk], [gk])
                P.dma("sp", self.grd[ti * 128:(ti + 1) * 128, :], g_[:], [gk], ())
        P.release(m)

    def phase_na(self, l, want_ctx):
        P, I = self.P, self.I
        i = l // 2
        m = P.mark()
        scale = 64.0 ** -0.5
        BB = P.sb("BB", [64, 15, 8, 64], F32)
        msk = P.sb("msk", [64, 64], F32)
        P.dma("sp", BB[:], I["rpbg"][i], w=["BB"])
        P.dma("sp", msk[:], I["namask"][:, :], w=["msk"])
        for dr in range(15):
            P.tt("pool", BB[:, dr, :, :], BB[:, dr, :, :], msk[:].unsqueeze(1).to_broadcast([64, 8, 64]), ALU.add, ["BB", "msk"], ["BB"])
            P.ts("pool", BB[:, dr, :, :], BB[:, dr, :, :], 1.0 / scale, ALU.mult, r=["BB"], w=["BB"])
        kcT = P.sb("kcT", [64, 8, 256], BF16)
        vcx = P.sb("vcx", [128, 2, 8, 65], BF16)
        P.dma("sp", kcT[:], self.kaT[:, :, 0:256].rearrange("h p t -> p h t"), w=["kcT"])
        P.dma("sp", vcx[:], self.va[0:256, :, :].rearrange("(kt p) h d -> p kt h d", p=128), (), ["vcx"])
        m2 = P.mark()
        kw = Rot(P, "kw", 2, [64, 8, 1024], BF16)
        vw = Rot(P, "vw", 2, [64, 16, 8, 65], BF16)
        qb = Rot(P, "qb", 2, [64, 8, 512], BF16)
        psS = Rot(P, "psS", 2, [64, 2, 8, 64], F32, psum=True)
        psC = Rot(P, "psC", 2, [128, 2, 2, 64], F32, psum=True)
        psO = Rot(P, "psO", 1, [128, 2, 65], F32, psum=True)
        stmp = Rot(P, "stmp", 2, [64, 2, 8, 64], F32)
        pb = Rot(P, "pb", 2, [64, 2, 8, 64], BF16)
        pcb = Rot(P, "pcb", 2, [128, 2, 2, 64], BF16)
        yo = Rot(P, "yo", 1, [64, 8, 8, 64], F32)
        for b8 in range(16):
            w0 = min(max(8 * b8 - 4, 0), 112)
            k_w, kk = kw.next(); v_w, vk = vw.next(); q_b, qk = qb.next()
            tk0 = CTX + w0 * 64
            P.dma("sp", k_w[:], self.kaT[:, :, tk0:tk0 + 1024].rearrange("h p t -> p h t"), (), [kk])
            P.dma("sp", v_w[:], self.va[tk0:tk0 + 1024, :, :].rearrange("(r p) h d -> p r h d", p=64), (), [vk])
            tq0 = CTX + b8 * 512
            P.dma("sp", q_b[:], self.qaT[:, :, tq0:tq0 + 512].rearrange("h p t -> p h t"), (), [qk])
            y_o, yk = yo.next()

            def na_a(rr, hp2):
                r = 8 * b8 + rr
                rs = min(max(r - 4, 0), 120)
                dr0 = rs - r + 7
                ps, pk = psS.next()
                pc, pck = psC.next()
                for hh in range(2):
                    h = 2 * hp2 + hh
                    for i8 in range(8):
                        kr = rs + i8 - w0
                        P.mm(ps[:, hh, i8, :], k_w[:, h, kr * 64:(kr + 1) * 64], q_b[:, h, rr * 64:(rr + 1) * 64], True, True,
                             [kk, qk], [pk])
                    for ct in range(2):
                        P.mm(pc[:, ct, hh, :], kcT[:, h, ct * 128:(ct + 1) * 128], q_b[:, h, rr * 64:(rr + 1) * 64], True, True,
                             ["kcT", qk], [pck])
                st, stk_ = stmp.next()
                bias = BB[:, dr0:dr0 + 8, 2 * hp2:2 * hp2 + 2, :].rearrange("p r h q -> p h r q")
                P.tt("dve", st[:], ps[:], bias, ALU.add, [pk, "BB"], [stk_])
                p_b, pbk = pb.next()
                P.act(p_b[:], st[:], AF.Exp, [stk_], [pbk], scale=scale)
                p_c, pcbk = pcb.next()
                P.act(p_c[:], pc[:], AF.Exp, [pck], [pcbk], scale=scale)
                return rr, hp2, rs, p_b, pbk, p_c, pcbk

            def na_b(rr, hp2, rs, p_b, pbk, p_c, pcbk):
                po, pok = psO.next()
                for hh in range(2):
                    h = 2 * hp2 + hh
                    for i8 in range(8):
                        kr = rs + i8 - w0
                        P.mm(po[0:64, hh, :], p_b[:, hh, i8, :], v_w[:, kr, h, :], i8 == 0, False, [pbk, vk], [pok])
                    for ct in range(2):
                        P.mm(po[0:64, hh, :], p_c[:, ct, hh, :], vcx[:, ct, h, :], False, ct == 1, [pcbk, "vcx"], [pok])
                sm, smk = self.small.next()
                P.op("dve", lambda e, sm=sm, po=po: e.reciprocal(out=sm[0:64, 0:2], in_=po[0:64, :, 64]), [pok], [smk])
                P.tt("dve", y_o[:, rr, 2 * hp2:2 * hp2 + 2, :], po[0:64, :, 0:64], sm[0:64, 0:2].unsqueeze(2).to_broadcast([64, 2, 64]),
                     ALU.mult, [pok, smk], [yk])

            items = [(rr, hp2) for rr in range(8) for hp2 in range(4)]
            prev = na_a(*items[0])
            for ii in range(len(items)):
                nxt = na_a(*items[ii + 1]) if ii + 1 < len(items) else None
                na_b(*prev)
                prev = nxt
            P.dma("sp", self.attn[tq0:tq0 + 512, 0:512].rearrange("(r p) (h d) -> p r h d", p=64, d=64), y_o[:], [yk], ())
        P.release(m2)
        if want_ctx:
            psO = Rot(P, "psO", 1, [128, 2, 65], F32, psum=True)
            qc = P.sb("qc", [64, 8, 256], BF16)
            P.dma("sp", qc[:], self.qaT[:, :, 0:256].rearrange("h p t -> p h t"), w=["qc"])
            psX = Rot(P, "psX", 1, [128, 2, 256], F32, psum=True)
            pxb = Rot(P, "pxb", 2, [128, 2, 256], BF16)
            yc = P.sb("yc", [128, 2, 8, 64], F32)
            for h in range(8):
                ps, pk = psX.next()
                for kt in range(2):
                    P.mm(ps[:, kt, :], kcT[:, h, kt * 128:(kt + 1) * 128], qc[:, h, :], True, True, ["kcT", "qc"], [pk])
                px, pxk = pxb.next()
                P.act(px[:], ps[:], AF.Exp, [pk], [pxk], scale=scale)
                po, pok = psO.next()
                for qt in range(2):
                    for kt in range(2):
                        P.mm(po[:, qt, :], px[:, kt, qt * 128:(qt + 1) * 128], vcx[:, kt, h, :], kt == 0, kt == 1, [pxk, "vcx"], [pok])
                sm, smk = self.small.next()
                P.op("dve", lambda e, sm=sm, po=po: e.reciprocal(out=sm[:, 0:2], in_=po[:, :, 64]), [pok], [smk])
                P.tt("dve", yc[:, :, h, :], po[:, :, 0:64], sm[:, 0:2].unsqueeze(2).to_broadcast([128, 2, 64]), ALU.mult, [pok, smk], ["yc"])
            P.dma("sp", self.attn[0:256, 0:512].rearrange("(qt p) (h d) -> p qt h d", p=128, d=64), yc[:], ["yc"], ())
        P.release(m)

    def phase_gdn_pre(self, l):
        P, I = self.P, self.I
        i = l // 2
        m = P.mark()
        if not hasattr(self, "gT"):
            self.gT = P.dram("gT", [12, 128, T], F32)
            self.od = P.dram("od", [2, T, 512], F32)
        cw = P.sb("cw", [128, 12, 5], F32)
        P.dma("sp", cw[:], I["conv_wT"][i].rearrange("c p k -> p c k"), w=["cw"])
        onesf = P.sb("onesf", [128, 128], F32)
        P.memset("pool", onesf[:], 1.0, w=["onesf"])
        NP = 2048
        xp = Rot(P, "xp", 2, [128, NP + 4], F32)
        acc = Rot(P, "acc", 2, [128, NP], F32)
        yv = Rot(P, "yv", 2, [128, NP], F32)
        sq = Rot(P, "sq", 2, [128, NP], F32)
        rn = Rot(P, "rn", 2, [128, 512], F32)
        psn = Rot(P, "psn", 2, [128, 512], F32, psum=True)
        pieces = [(0, 256, 0, 256)] + [(256 + NP * k, 256 + NP * (k + 1), 256, T) for k in range(SEQ // NP)]
        for c in range(12):
            for (a, b, lo_, hi_) in pieces:
                n = b - a
                x_, xk = xp.next()
                lo = max(a - 2, lo_); hi = min(b + 2, hi_)
                if lo > a - 2:
                    P.memset("pool", x_[:, 0:2], 0.0, w=[xk])
                if hi < b + 2:
                    P.memset("pool", x_[:, n + 2:n + 4], 0.0, w=[xk])
                P.dma("sp", x_[:, lo - (a - 2):hi - (a - 2)], self.gpre[c, :, lo:hi], (), [xk])
                a_, ak = acc.next()
                P.ts("dve", a_[:, 0:n], x_[:, 0:n], cw[:, c, 0:1], ALU.mult, r=[xk, "cw"], w=[ak])
                for k in range(1, 5):
                    P.stt(a_[:, 0:n], x_[:, k:k + n], cw[:, c, k:k + 1], a_[:, 0:n], ALU.mult, ALU.add, [xk, "cw", ak], [ak])
                y_, yk = yv.next()
                P.act(y_[:, 0:n], a_[:, 0:n], AF.Silu, [ak], [yk])
                if c < 8:
                    s_, sk = sq.next()
                    P.act(s_[:, 0:n], y_[:, 0:n], AF.Square, [yk], [sk])
                    for q0 in range(0, n, 512):
                        w_ = min(512, n - q0)
                        ps, pk = psn.next()
                        P.mm(ps[:, 0:w_], onesf[:], s_[:, q0:q0 + w_], True, True, ["onesf", sk], [pk])
                        r_, rk = rn.next()
                        mul = 128.0 if c < 4 else 1.0
                        P.act(r_[:, 0:w_], ps[:, 0:w_], AF.Sqrt, [pk], [rk], scale=mul, bias=mul * EPS)
                        P.op("dve", lambda e, r_=r_, w_=w_: e.reciprocal(out=r_[:, 0:w_], in_=r_[:, 0:w_]), [rk], [rk])
                        P.tt("dve", y_[:, q0:q0 + w_], y_[:, q0:q0 + w_], r_[:, 0:w_], ALU.mult, [yk, rk], [yk])
                P.dma("sp", self.gT[c, :, a:b], y_[:, 0:n], [yk], ())
        P.release(m)

    def phase_gdn(self, l):
        P, I = self.P, self.I
        i = l // 2
        m = P.mark()
        sbc = lambda n: P.sb(n, [128, 128], F32)
        Ball = sbc("Ball"); P.memset("pool", Ball[:], 0.0, w=["Ball"])
        P.memset("pool", Ball[0:64, 0:64], 1.0, w=["Ball"]); P.memset("pool", Ball[64:128, 64:128], 1.0, w=["Ball"])
        INf = sbc("INf"); INb = sbc("INb"); STf = sbc("STf"); STb = sbc("STb")
        for (dst, coef, cm, base) in ((INf, -1, 1, 0), (INb, 1, -1, 0), (STf, -1, 1, -1), (STb, 1, -1, -1)):
            P.op("pool", lambda e, dst=dst, coef=coef, cm=cm, base=base: e.affine_select(
                out=dst[:], in_=Ball[:], pattern=[[coef, 128]], compare_op=ALU.is_ge, fill=0.0, base=base, channel_multiplier=cm),
                ["Ball"], [dst.name])
        MNf = sbc("MNf"); MNb = sbc("MNb")
        P.ts("dve", MNf[:], INf[:], -1.0, ALU.add, -NEG, ALU.mult, r=[INf.name], w=["MNf"])
        P.ts("dve", MNb[:], INb[:], -1.0, ALU.add, -NEG, ALU.mult, r=[INb.name], w=["MNb"])
        CM = [sbc("CM0"), sbc("CM1")]
        BM = [sbc("BM0"), sbc("BM1")]
        SEL = [sbc("SEL0"), sbc("SEL1")]
        for c in range(2):
            for t_ in (CM[c], BM[c], SEL[c]):
                P.memset("pool", t_[:], 0.0, w=[t_.name])
            P.memset("pool", CM[c][:, c * 64:(c + 1) * 64], 1.0, w=[CM[c].name])
            P.memset("pool", BM[c][c * 64:(c + 1) * 64, c * 64:(c + 1) * 64], 1.0, w=[BM[c].name])
            P.memset("pool", SEL[c][c * 64:(c + 1) * 64, :], 1.0 / 64, w=[SEL[c].name])
        cnames = [Ball.name, INf.name, INb.name, STf.name, STb.name, "MNf", "MNb"] + [t_.name for t_ in CM + BM + SEL]
        MN = [MNf, MNb]; ST = [STf, STb]
        Lc = [INb, INf]
        ab = P.sb("ab", [128, 2, 8], F32)
        P.dma("sp", ab[:], I["gdn_ab"][i:i + 1, :, :].to_broadcast([128, 2, 8]), w=["ab"])
        negA = P.sb("negA", [128, 8], F32)
        P.act(negA[:], ab[:, 0, :], AF.Exp, ["ab"], ["negA"])
        P.ts("dve", negA[:], negA[:], -1.0, ALU.mult, r=["negA"], w=["negA"])

        psg = Rot(P, "psg", 8, [128, 128], F32, psum=True)
        grt = Rot(P, "grt", 2, [128, 16], F32)
        gtmp = Rot(P, "gtmp", 3, [128, 16], F32)
        gsc = Rot(P, "gsc", 6, [128, 6, 8], F32)
        bnd = Rot(P, "bnd", 6, [128, 2, 8], F32)

        def gates(ti):
            g_, gk = grt.next()
            P.dma("sp", g_[:], self.grd[ti * 128:(ti + 1) * 128, :], (), [gk])
            g4 = g_[:].rearrange("p (d k h) -> p d k h", d=2, k=2)
            t_, tk = gtmp.next()
            t4 = t_[:].rearrange("p (d k h) -> p d k h", d=2, k=2)
            sc, sk = gsc.next()
            P.tt("dve", t4[:, :, 0, :], g4[:, :, 0, :], ab[:, 1, :].rearrange("p (d h) -> p d h", d=2), ALU.add, [gk, "ab"], [tk])
            P.act(t4[:, :, 0, :], t4[:, :, 0, :], AF.Exp, [tk], [tk])
            P.act(t4[:, :, 0, :], t4[:, :, 0, :], AF.Ln, [tk], [tk], bias=1.0)
            P.tt("dve", t4[:, :, 0, :], t4[:, :, 0, :], negA[:].rearrange("p (d h) -> p d h", d=2), ALU.mult, [tk, "negA"], [tk])
            P.act(sc[:, 4, :].rearrange("p (d h) -> p d h", d=2), g4[:, :, 1, :], AF.Sigmoid, [gk], [sk])
            gg = t_[:].rearrange("p (d k h) -> p d k h", d=2, k=2)
            gcomp, gck = gtmp.next()
            P.cp("dve", gcomp[:, 0:8].rearrange("p (d h) -> p d h", d=2), gg[:, :, 0, :], [tk], [gck])
            ps, pk = psg.t[0], (psg.name, 0)
            P.mm(ps[:, 0:4], Lc[0][:], gcomp[:, 0:4], True, True, [Lc[0].name, gck], [pk])
            P.mm(ps[:, 4:8], Lc[1][:], gcomp[:, 4:8], True, True, [Lc[1].name, gck], [pk])
            P.mm(ps[:, 8:16], Ball[:], gcomp[:, 0:8], True, True, [Ball.name, gck], [pk])
            gps_sb, gpk = gtmp.next()
            P.cp("act", gps_sb[:], ps[:, 0:16], [pk], [gpk])
            P.cp("act", sc[:, 0, :], gps_sb[:, 0:8], [gpk], [sk])
            P.ts("dve", sc[:, 1, :], gps_sb[:, 0:8], -1.0, ALU.mult, r=[gpk], w=[sk])
            P.act(sc[:, 2, :], gps_sb[:, 0:8], AF.Exp, [gpk], [sk])
            P.tt("dve", sc[:, 5, :], gps_sb[:, 8:16], sc[:, 0, :], ALU.subtract, [gpk, sk], [sk])
            P.act(sc[:, 5, :], sc[:, 5, :], AF.Exp, [sk], [sk])
            P.stt(sc[:, 3, :], sc[:, 2, :], -1.0, sc[:, 4, :], ALU.mult, ALU.mult, [sk], [sk])
            P.cp("act", gcomp[:, 8:16], gps_sb[:, 8:16], [gpk], [gck])
            ps2, pk2 = psg.t[1], (psg.name, 1)
            for c in range(2):
                P.mm(ps2[:, c * 8:(c + 1) * 8], SEL[c][:], gcomp[:, 8:16], True, True, [SEL[c].name, gck], [pk2])
            b_, bk = bnd.next()
            P.act(b_[:].rearrange("p c e -> p (c e)"), ps2[:, 0:16], AF.Exp, [pk2], [bk])
            return sc, sk, b_, bk

        NCH = 8
        wk4 = [[P.sb("gwk%d_%d" % (ch, j), [128, 128], F32) for j in range(4)] for ch in range(NCH)]
        PAs = [[P.sb("PA%d_%d" % (ch, j), [128, 128], F32) for j in range(2)] for ch in range(NCH)]
        PTs = [[P.sb("PT%d_%d" % (ch, j), [128, 128], F32) for j in range(2)] for ch in range(NCH)]
        TTs = [P.sb("TT%d" % ch, [128, 128], F32) for ch in range(NCH)]
        lds = [P.sb("ld%d" % ch, [128, 3, 128], F32) for ch in range(NCH)]
        prep = [[P.sb("prep%d_%d" % (ch, j), [128, 9, 128], F32) for j in range(2)] for ch in range(NCH)]
        prep_i = [0] * NCH
        Sst = [[P.sb("S%d_%d" % (ch, j), [128, 128], F32) for j in range(2)] for ch in range(NCH)]
        S_i = [0] * NCH
        for ch in range(NCH):
            P.memset("pool", Sst[ch][0][:], 0.0, w=[("S", ch, 0)])
        rr_ = [P.sb("rr%d" % ch, [128, 128], F32) for ch in range(NCH)]
        uu = [P.sb("uu%d" % ch, [128, 128], F32) for ch in range(NCH)]
        osb = [P.sb("gosb%d" % ch, [128, 128], F32) for ch in range(NCH)]

        def prep_tile(ti, h, d, sc, sk):
            ch = d * 4 + h; c8 = ch
            j = prep_i[ch]; prep_i[ch] ^= 1
            pr = prep[ch][j]; prk = ("prep", ch, j)
            return pr, prk, prep_gen(ti, h, d, sc, sk, ch, pr, prk)

        free_ps = list(zip(psg.t, [(psg.name, j) for j in range(psg.n)]))

        def grab(n):
            while len(free_ps) < n:
                yield
            return [free_ps.pop(0) for _ in range(n)]

        def give(*tiles):
            free_ps.extend(tiles)

        def prep_gen(ti, h, d, sc, sk, ch, pr, prk):
            c8 = ch
            ld, ldk = lds[ch], ("ld", ch)
            for w_, cc in enumerate((h, 4 + h, 8 + h)):
                P.dma("sp", ld[:, w_, :], self.gT[cc, :, ti * 128:(ti + 1) * 128], (), [ldk])
            qT, kT, vT = ld[:, 0, :], ld[:, 1, :], ld[:, 2, :]
            gcol = lambda w_: sc[:, w_, c8:c8 + 1]
            (gcB, Dm, dT, EG) = wk4[ch]
            gcBk, Dmk, dTk, EGk = [("wk", ch, q) for q in range(4)]
            P.cp("pool", gcB[:], gcol(0).to_broadcast([128, 128]), [sk], [gcBk])
            yield
            (g_,) = yield from grab(1)
            Gps, Gk = g_
            P.mm(Gps[:], gcB[:], self.identf[:], True, True, [gcBk, "identf"], [Gk])
            yield
            P.stt(Dm[:], Gps[:], -1.0, MN[d][:], ALU.mult, ALU.add, [Gk, "MNf" if d == 0 else "MNb"], [Dmk])
            P.tt("dve", dT[:], Gps[:], MN[1 - d][:], ALU.add, [Gk, "MNf" if d == 1 else "MNb"], [dTk])
            P.act(EG[:], Gps[:], AF.Exp, [Gk, Dmk, dTk], [EGk])
            give(g_)
            yield
            P.act(Dm[:], Dm[:], AF.Exp, [Dmk, sk], [Dmk], bias=gcol(0))
            P.act(dT[:], dT[:], AF.Exp, [dTk, sk], [dTk], bias=gcol(1))
            yield
            P.tt("pool", Dm[:], Dm[:], ST[d][:], ALU.mult, [Dmk, ST[d].name], [Dmk])
            (g_,) = yield from grab(1)
            KKp, KKk = g_
            P.mm(KKp[:], kT, kT, True, True, [ldk], [KKk])
            yield
            pa = PAs[ch]; pt = PTs[ch]
            A_, Ak = pa[0], ("PA", ch, 0)
            P.stt(A_[:], KKp[:], gcol(4), Dm[:], ALU.mult, ALU.mult, [KKk, sk, Dmk], [Ak])
            give(g_)
            yield
            (g_,) = yield from grab(1)
            ATp, ATpk = g_
            P.tr(ATp[:], A_[:], self.identf[:], [Ak, "identf"], [ATpk])
            yield
            AT, ATk = pt[0], ("PT", ch, 0)
            P.cp("act", AT[:], ATp[:], [ATpk], [ATk])
            give(g_)
            yield
            TT, TTk = TTs[ch], ("TT", ch)
            P.tt("dve", TT[:], self.identf[:], AT[:], ALU.subtract, ["identf", ATk], [TTk])
            Pn, Pnk, PTn, PTnk = A_, Ak, AT, ATk
            for lev in range(5):
                gs = yield from grab(2 if lev < 4 else 1)
                p2, p2k = gs[0]
                P.mm(p2[:], PTn[:], Pn[:], True, True, [PTnk, Pnk], [p2k])
                if lev < 4:
                    p2t, p2tk = gs[1]
                    P.mm(p2t[:], Pn[:], PTn[:], True, True, [PTnk, Pnk], [p2tk])
                yield
                q_ = (lev + 1) % 2
                Pn2, Pn2k = pa[q_], ("PA", ch, q_)
                P.cp("act", Pn2[:], p2[:], [p2k], [Pn2k])
                if lev < 4:
                    PTn2, PTn2k = pt[q_], ("PT", ch, q_)
                    P.cp("dve", PTn2[:], p2t[:], [p2tk], [PTn2k])
                give(*gs)
                yield
                (g_,) = yield from grab(1)
                up, upk = g_
                P.mm(up[:], Pn2[:], TT[:], True, True, [Pn2k, TTk], [upk])
                yield
                P.tt("dve", TT[:], TT[:], up[:], ALU.add, [TTk, upk], [TTk])
                give(g_)
                Pn, Pnk = Pn2, Pn2k
                if lev < 4:
                    PTn, PTnk = PTn2, PTn2k
            yield
            for c in range(2):
                P.tt("pool", pr[:, c, :], TT[:], BM[c][:], ALU.mult, [TTk, BM[c].name], [prk])
            P.tt("pool", EG[:], EG[:], qT, ALU.mult, [EGk, ldk], [EGk])
            gs = yield from grab(3)
            (Pp, Ppk), (kp, kpk), (vp, vpk) = gs
            P.mm(Pp[:], kT, qT, True, True, [ldk], [Ppk])
            P.tr(kp[:], kT, self.identf[:], [ldk, "identf"], [kpk])
            P.tr(vp[:], vT, self.identf[:], [ldk, "identf"], [vpk])
            yield
            P.tt("dve", pr[:, 2, :], Pp[:], dT[:], ALU.mult, [Ppk, dTk], [prk])
            P.act(pr[:, 7, :], kp[:], AF.Copy, [kpk, sk], [prk], scale=gcol(5))
            P.act(pr[:, 8, :], vp[:], AF.Copy, [vpk, sk], [prk], scale=gcol(4))
            give(*gs)
            for c in range(2):
                P.tt("pool", pr[:, 3 + c, :], EG[:], CM[c][:], ALU.mult, [EGk, CM[c].name], [prk])
                P.tt("pool", pr[:, 5 + c, :], kT, CM[c][:], ALU.mult, [ldk, CM[c].name], [prk])
            yield

        def chain_gen(ti, h, d, pr, prk, sc, sk, b_, bk):
            ch = d * 4 + h; c8 = ch
            o_, ok = osb[ch], ("osb", ch)
            r_, rk = rr_[ch], ("rr", ch)
            u_, uk = uu[ch], ("uu", ch)
            order = (0, 1) if d == 0 else (1, 0)
            for n_, c in enumerate(order):
                si = S_i[ch]; S_i[ch] ^= 1
                S = Sst[ch][si]; Sk = ("S", ch, si)
                S2 = Sst[ch][si ^ 1]; S2k = ("S", ch, si ^ 1)
                (g_,) = yield from grab(1)
                ks, ksk = g_
                P.mm(ks[:], pr[:, 5 + c, :], S[:], True, True, [prk, Sk], [ksk])
                yield
                P.stt(r_[:], ks[:], sc[:, 3, c8:c8 + 1], pr[:, 8, :], ALU.mult, ALU.add, [ksk, sk, prk], [rk])
                give(g_)
                yield
                (g_,) = yield from grab(1)
                up, upk = g_
                P.mm(up[:], pr[:, c, :], r_[:], True, True, [prk, rk], [upk])
                yield
                P.cp("act", u_[:], up[:], [upk], [uk])
                give(g_)
                yield
                gs = yield from grab(2)
                (op_, opk), (ds, dsk) = gs
                P.mm(op_[:], pr[:, 3 + c, :], S[:], True, False, [prk, Sk], [opk])
                P.mm(op_[:], pr[:, 2, :], u_[:], False, True, [prk, uk], [opk])
                P.mm(ds[:], pr[:, 7, :], u_[:], True, True, [prk, uk], [dsk])
                yield
                P.stt(S2[:], S[:], b_[:, c, c8:c8 + 1], ds[:], ALU.mult, ALU.add, [Sk, bk, dsk], [S2k])
                if n_ == 0:
                    P.cp("act", o_[:], op_[:], [opk], [ok])
                else:
                    P.tt("dve", o_[:], o_[:], op_[:], ALU.add, [ok, opk], [ok])
                give(*gs)
                yield
            P.dma("sp", self.od[d, ti * 128:(ti + 1) * 128, h * 128:(h + 1) * 128], o_[:], [ok], ())

        def run_rr(gens):
            gens = list(gens)
            while gens:
                alive = []
                for g in gens:
                    try:
                        next(g); alive.append(g)
                    except StopIteration:
                        pass
                gens = alive

        fwd_order = list(range(NT))
        bwd_order = [1, 0] + list(range(NT - 1, 1, -1))

        def make_preps(s_):
            tf_, tb_ = fwd_order[s_], bwd_order[s_]
            gf = gates(tf_)
            gb = gates(tb_)
            out = []
            for h in range(4):
                out.append((tf_, h, 0, prep_tile(tf_, h, 0, gf[0], gf[1]), gf))
                out.append((tb_, h, 1, prep_tile(tb_, h, 1, gb[0], gb[1]), gb))
            return out

        cur = make_preps(0)
        run_rr([p[3][2] for p in cur])
        for s_ in range(NT):
            nxt = make_preps(s_ + 1) if s_ + 1 < NT else []
            chains = [chain_gen(ti, h, d, pr[0], pr[1], g[0], g[1], g[2], g[3]) for (ti, h, d, pr, g) in cur]
            run_rr([p[3][2] for p in nxt] + chains)
            cur = nxt
        P.release(m)

    def phase_gdn_out(self, l):
        P, I = self.P, self.I
        i = l // 2
        m = P.mark()
        gng = P.sb("gng", [128, 128], F32)
        P.dma("sp", gng[:], I["gdn_norm_g"][i:i + 1, :].to_broadcast([128, 128]), w=["gng"])
        of = Rot(P, "of", 2, [128, 512], F32)
        ob = Rot(P, "ob", 2, [128, 512], F32)
        zt = Rot(P, "zt", 2, [128, 512], F32)
        yt = Rot(P, "yt", 2, [128, 512], F32)
        for ti in range(NT):
            o1, k1 = of.next(); o2, k2 = ob.next(); z_, zk = zt.next()
            P.dma("sp", o1[:], self.od[0, ti * 128:(ti + 1) * 128, :], (), [k1])
            P.dma("sp", o2[:], self.od[1, ti * 128:(ti + 1) * 128, :], (), [k2])
            P.dma("sp", z_[:], self.zd[ti * 128:(ti + 1) * 128, :], (), [zk])
            P.tt("dve", o1[:], o1[:], o2[:], ALU.add, [k1, k2], [k1])
            P.act(z_[:], z_[:], AF.Silu, [zk], [zk])
            y_, yk = yt.next()
            for h in range(4):
                hs = slice(h * 128, (h + 1) * 128)
                r, rk = self.rstd(o1[:, hs], 128, k1)
                P.stt(y_[:, hs], o1[:, hs], r, gng[:], ALU.mult, ALU.mult, [k1, rk, "gng"], [yk])
            P.tt("dve", y_[:], y_[:], z_[:], ALU.mult, [yk, zk], [yk])
            P.dma("sp", self.attn[ti * 128:(ti + 1) * 128, 512:1024], y_[:], [yk], ())
        P.release(m)

    def phase_even(self, l, want_ctx):
        self.phase_even_proj(l)
        self.phase_na(l, want_ctx)
        self.phase_gdn_pre(l)
        self.phase_gdn(l)
        self.phase_gdn_out(l)
```
